# Optimizing a Trainium2 kernel written in Bass

```python
import math
import jax, jax.numpy as jnp
from jax import lax
import numpy as np

D_MODEL = 1024
BATCH = 16
SEQ = 2048
DEPTH = 1

MEM_LEN = 256
EPS = 1e-6

MIX_WIDTH = D_MODEL
ML_WIDTH = MIX_WIDTH // 2
DA_WIDTH = MIX_WIDTH - ML_WIDTH
ML_HEADS = 4
ML_HEAD_DIM = ML_WIDTH // ML_HEADS
ML_CHUNK = 64
CONV_WIDTH = 4
DA_HEADS = 4
DA_VDIM = DA_WIDTH // DA_HEADS
DA_QK_DIM = DA_VDIM // 2
Q_BLOCK = 128

CA_HEADS = 4
CA_HEAD_DIM = D_MODEL // CA_HEADS

PEER_HEADS = 8
PEER_KEYS = 128
PEER_EXPERTS = PEER_KEYS * PEER_KEYS
PEER_TOPK = 16
PEER_DKEY = 128
PEER_HALF = PEER_DKEY // 2
PEER_BLOCK = 128

IN_SPLITS = (2 * ML_WIDTH,
             ML_WIDTH,
             ML_WIDTH,
             ML_HEADS,
             ML_HEADS,
             2 * DA_HEADS * DA_QK_DIM,
             2 * DA_HEADS * DA_QK_DIM,
             DA_WIDTH)
IN_WIDTH = sum(IN_SPLITS)
SPLIT_IDX = tuple(int(c) for c in np.cumsum(IN_SPLITS)[:-1])

kernel_name = "hymba_mlstm_diffattn_peer_layer"


def rmsnorm(x, g):
    xf = x.astype(jnp.float32)
    y = xf * lax.rsqrt(jnp.mean(xf * xf, axis=-1, keepdims=True) + EPS)
    return (y * g.astype(jnp.float32)).astype(x.dtype)


def alibi_slopes(n_heads):
    return jnp.asarray(np.array([2.0 ** (-8.0 * (i + 1) / n_heads) for i in range(n_heads)], np.float32))


def causal_depthwise_conv(x, w):
    c = x.shape[-1]
    return lax.conv_general_dilated(x, w[:, None, :].astype(x.dtype), window_strides=(1,),
                                    padding=[(CONV_WIDTH - 1, 0)],
                                    dimension_numbers=('NWC', 'WIO', 'NWC'),
                                    feature_group_count=c)


def mlstm_chunkwise(q, k, v, ig, lf):
    b_, s_, h_, d_ = q.shape
    nc = s_ // ML_CHUNK
    k = k * (d_ ** -0.5)

    def chunks4(a):
        return a.reshape(b_, nc, ML_CHUNK, h_, d_).transpose(1, 0, 3, 2, 4)

    def chunks3(a):
        return a.reshape(b_, nc, ML_CHUNK, h_).transpose(1, 0, 3, 2)

    causal = jnp.tril(jnp.ones((ML_CHUNK, ML_CHUNK), dtype=bool))

    def step(carry, inp):
        c_st, n_st, m_st = carry
        qc, kc, vc, ic, fc = inp
        bcum = jnp.cumsum(fc, axis=-1)
        log_d = bcum[..., :, None] - bcum[..., None, :] + ic[..., None, :]
        log_d = jnp.where(causal, log_d, -jnp.inf)
        m_inter = bcum + m_st[..., None]
        m_t = jnp.maximum(m_inter, jnp.max(log_d, axis=-1))
        dmat = jnp.exp(log_d - m_t[..., None])
        sc = jnp.einsum('bhld,bhsd->bhls', qc, kc) * dmat
        inter = jnp.exp(m_inter - m_t)
        num = jnp.einsum('bhls,bhse->bhle', sc, vc) + inter[..., None] * jnp.einsum('bhld,bhde->bhle', qc, c_st)
        den = jnp.sum(sc, axis=-1) + inter * jnp.einsum('bhld,bhd->bhl', qc, n_st)
        h = num / jnp.maximum(jnp.abs(den), jnp.exp(-m_t))[..., None]
        g = bcum[..., -1:] - bcum + ic
        m_next = jnp.maximum(bcum[..., -1] + m_st, jnp.max(g, axis=-1))
        decay = jnp.exp(bcum[..., -1] + m_st - m_next)
        wgt = jnp.exp(g - m_next[..., None])
        c_next = decay[..., None, None] * c_st + jnp.einsum('bhs,bhsd,bhse->bhde', wgt, kc, vc)
        n_next = decay[..., None] * n_st + jnp.einsum('bhs,bhsd->bhd', wgt, kc)
        return (c_next, n_next, m_next), h

    init = (jnp.zeros((b_, h_, d_, d_), jnp.float32),
            jnp.zeros((b_, h_, d_), jnp.float32),
            jnp.zeros((b_, h_), jnp.float32))
    _, hs = lax.scan(step, init, (chunks4(q), chunks4(k), chunks4(v), chunks3(ig), chunks3(lf)))
    return hs.transpose(1, 0, 3, 2, 4).reshape(b_, s_, h_, d_)


def diff_attention(q, k, v, lam, lam_init, norm_g):
    b_, s_, h_, _, d_ = q.shape
    nb = s_ // Q_BLOCK
    qb = q.reshape(b_, nb, Q_BLOCK, h_, 2, d_).transpose(1, 0, 2, 3, 4, 5)
    starts = jnp.arange(nb, dtype=jnp.int32) * Q_BLOCK
    kpos = jnp.arange(s_, dtype=jnp.int32)
    slopes = alibi_slopes(h_)
    scale = d_ ** -0.5

    def block(args):
        qblk, start = args
        qpos = start + jnp.arange(Q_BLOCK, dtype=jnp.int32)
        dist = (qpos[:, None] - kpos[None, :]).astype(jnp.float32)
        sc = jnp.einsum('bqhcd,bkhcd->bhcqk', qblk, k).astype(jnp.float32) * scale
        sc = sc - slopes[None, :, None, None, None] * dist
        sc = jnp.where(dist >= 0, sc, -jnp.inf)
        p = jax.nn.softmax(sc, axis=-1)
        a = p[:, :, 0] - lam * p[:, :, 1]
        return jnp.einsum('bhqk,bkhe->bqhe', a.astype(v.dtype), v)

    o = lax.map(block, (qb, starts))
    o = o.transpose(1, 0, 2, 3, 4).reshape(b_, s_, h_, 2 * d_)
    return rmsnorm(o, norm_g.reshape(h_, 2 * d_)) * (1.0 - lam_init)


def parallel_mixer(h, w_in, conv_w, b_igate, b_fgate, ml_norm_g, lambda_q1, lambda_k1,
                   lambda_q2, lambda_k2, da_norm_g, w_out, layer_idx):
    b_, s_, _ = h.shape
    f32 = jnp.float32
    z = h @ w_in
    ml_qk, ml_v, ml_o, ml_i, ml_f, da_q, da_k, da_v = jnp.split(z, SPLIT_IDX, axis=-1)

    ml_qk = jax.nn.silu(causal_depthwise_conv(ml_qk, conv_w))
    ml_q, ml_k = jnp.split(ml_qk, 2, axis=-1)
    shp = (b_, s_, ML_HEADS, ML_HEAD_DIM)
    ig = (ml_i + b_igate).astype(f32)
    lf = jax.nn.log_sigmoid((ml_f + b_fgate).astype(f32))
    h_ml = mlstm_chunkwise(ml_q.reshape(shp).astype(f32), ml_k.reshape(shp).astype(f32),
                           ml_v.reshape(shp).astype(f32), ig, lf)
    h_ml = rmsnorm(h_ml, ml_norm_g.reshape(ML_HEADS, ML_HEAD_DIM)) * jax.nn.sigmoid(ml_o.astype(f32)).reshape(shp)
    h_ml = h_ml.reshape(b_, s_, ML_WIDTH).astype(h.dtype)

    lam_init = 0.8 - 0.6 * math.exp(-0.3 * layer_idx)
    lam = (jnp.exp(jnp.sum(lambda_q1.astype(f32) * lambda_k1.astype(f32)))
           - jnp.exp(jnp.sum(lambda_q2.astype(f32) * lambda_k2.astype(f32))) + lam_init)
    qk_shp = (b_, s_, DA_HEADS, 2, DA_QK_DIM)
    h_da = diff_attention(da_q.reshape(qk_shp), da_k.reshape(qk_shp),
                          da_v.reshape(b_, s_, DA_HEADS, DA_VDIM), lam, lam_init, da_norm_g)
    h_da = h_da.reshape(b_, s_, DA_WIDTH).astype(h.dtype)

    return jnp.concatenate([h_ml, h_da], axis=-1) @ w_out


def cross_attention(h, mem_n, w_cq, w_ck, w_cv, w_co):
    b_, s_, _ = h.shape
    m_ = mem_n.shape[1]
    q = (h @ w_cq).reshape(b_, s_, CA_HEADS, CA_HEAD_DIM)
    k = (mem_n @ w_ck).reshape(b_, m_, CA_HEADS, CA_HEAD_DIM)
    v = (mem_n @ w_cv).reshape(b_, m_, CA_HEADS, CA_HEAD_DIM)
    sc = jnp.einsum('bqhd,bmhd->bhqm', q, k).astype(jnp.float32) * (CA_HEAD_DIM ** -0.5)
    p = jax.nn.softmax(sc, axis=-1).astype(v.dtype)
    o = jnp.einsum('bhqm,bmhd->bqhd', p, v).reshape(b_, s_, D_MODEL)
    return o @ w_co


def peer(h, w_pq, sub_keys, peer_u, peer_v):
    b_, s_, d_ = h.shape
    xt = h.reshape((b_ * s_) // PEER_BLOCK, PEER_BLOCK, d_)

    def block(xb):
        q = (xb @ w_pq).reshape(PEER_BLOCK, PEER_HEADS, 2, PEER_HALF)
        sc = jnp.einsum('thcd,hcnd->thcn', q, sub_keys).astype(jnp.float32)
        top_v, top_i = lax.top_k(sc, PEER_TOPK)
        cand_v = top_v[:, :, 0, :, None] + top_v[:, :, 1, None, :]
        cand_i = top_i[:, :, 0, :, None] * PEER_KEYS + top_i[:, :, 1, None, :]
        cand_v = cand_v.reshape(PEER_BLOCK, PEER_HEADS, PEER_TOPK * PEER_TOPK)
        cand_i = cand_i.reshape(PEER_BLOCK, PEER_HEADS, PEER_TOPK * PEER_TOPK)
        best_v, best_j = lax.top_k(cand_v, PEER_TOPK)
        eidx = jnp.take_along_axis(cand_i, best_j, axis=-1)
        gate = jax.nn.softmax(best_v, axis=-1)
        u = peer_u[eidx]
        act = jax.nn.gelu(jnp.einsum('td,thkd->thk', xb, u).astype(jnp.float32), approximate=False)
        coef = (gate * act).astype(xb.dtype)
        return jnp.einsum('thk,thkd->td', coef, peer_v[eidx])

    return lax.map(block, xt).reshape(b_, s_, d_)


def setup_inputs(seed: int = 0) -> dict:
    key = jax.random.key(seed)
    ks = jax.random.split(key, 32)
    f32 = jnp.float32

    def nrm(k, shape, scale):
        return jax.random.normal(k, shape, f32) * scale

    def gain(k, shape):
        return 1.0 + 0.01 * jax.random.normal(k, shape, f32)

    return {
        "x": nrm(ks[0], (BATCH, SEQ, D_MODEL), 1.0),
        "mem": nrm(ks[1], (BATCH, MEM_LEN, D_MODEL), 1.0),
        "norm_mix_g": gain(ks[2], (DEPTH, D_MODEL)),
        "w_in": nrm(ks[3], (DEPTH, D_MODEL, IN_WIDTH), D_MODEL ** -0.5),
        "conv_w": nrm(ks[4], (DEPTH, CONV_WIDTH, 2 * ML_WIDTH), CONV_WIDTH ** -0.5),
        "b_igate": nrm(ks[5], (DEPTH, ML_HEADS), 0.1),
        "b_fgate": jnp.broadcast_to(jnp.linspace(3.0, 6.0, ML_HEADS, dtype=f32), (DEPTH, ML_HEADS))
                    + nrm(ks[6], (DEPTH, ML_HEADS), 0.01),
        "ml_norm_g": gain(ks[7], (DEPTH, ML_WIDTH)),
        "lambda_q1": nrm(ks[8], (DEPTH, DA_QK_DIM), 0.1),
        "lambda_k1": nrm(ks[9], (DEPTH, DA_QK_DIM), 0.1),
        "lambda_q2": nrm(ks[10], (DEPTH, DA_QK_DIM), 0.1),
        "lambda_k2": nrm(ks[11], (DEPTH, DA_QK_DIM), 0.1),
        "da_norm_g": gain(ks[12], (DEPTH, DA_WIDTH)),
        "w_out": nrm(ks[13], (DEPTH, MIX_WIDTH, D_MODEL), MIX_WIDTH ** -0.5),
        "norm_ca_g": gain(ks[14], (DEPTH, D_MODEL)),
        "norm_mem_g": gain(ks[15], (DEPTH, D_MODEL)),
        "w_cq": nrm(ks[16], (DEPTH, D_MODEL, D_MODEL), D_MODEL ** -0.5),
        "w_ck": nrm(ks[17], (DEPTH, D_MODEL, D_MODEL), D_MODEL ** -0.5),
        "w_cv": nrm(ks[18], (DEPTH, D_MODEL, D_MODEL), D_MODEL ** -0.5),
        "w_co": nrm(ks[19], (DEPTH, D_MODEL, D_MODEL), D_MODEL ** -0.5),
        "norm_ffn_g": gain(ks[20], (DEPTH, D_MODEL)),
        "w_pq": nrm(ks[21], (DEPTH, D_MODEL, PEER_HEADS * PEER_DKEY), D_MODEL ** -0.5),
        "sub_keys": nrm(ks[22], (DEPTH, PEER_HEADS, 2, PEER_KEYS, PEER_HALF), PEER_HALF ** -0.5),
        "peer_u": nrm(ks[23], (DEPTH, PEER_EXPERTS, D_MODEL), D_MODEL ** -0.5),
        "peer_v": nrm(ks[24], (DEPTH, PEER_EXPERTS, D_MODEL), PEER_HEADS ** -0.5),
        "final_norm_g": gain(ks[25], (D_MODEL,)),
    }


def reference(x, mem, norm_mix_g, w_in, conv_w, b_igate, b_fgate, ml_norm_g, lambda_q1, lambda_k1,
              lambda_q2, lambda_k2, da_norm_g, w_out, norm_ca_g, norm_mem_g, w_cq, w_ck, w_cv, w_co,
              norm_ffn_g, w_pq, sub_keys, peer_u, peer_v, final_norm_g):
    for l in range(DEPTH):
        x = x + parallel_mixer(rmsnorm(x, norm_mix_g[l]), w_in[l], conv_w[l], b_igate[l], b_fgate[l],
                               ml_norm_g[l], lambda_q1[l], lambda_k1[l], lambda_q2[l], lambda_k2[l],
                               da_norm_g[l], w_out[l], l)
        x = x + cross_attention(rmsnorm(x, norm_ca_g[l]), rmsnorm(mem, norm_mem_g[l]),
                                w_cq[l], w_ck[l], w_cv[l], w_co[l])
        x = x + peer(rmsnorm(x, norm_ffn_g[l]), w_pq[l], sub_keys[l], peer_u[l], peer_v[l])
    return rmsnorm(x, final_norm_g)
```

```python
import math
from contextlib import ExitStack
import numpy as np
import concourse.bass as bass
import concourse.mybir as mybir
from concourse.bass_utils import run_bass_kernel_spmd

F32 = mybir.dt.float32
BF16 = mybir.dt.bfloat16
I32 = mybir.dt.int32
U32 = mybir.dt.uint32
ALU = mybir.AluOpType
AF = mybir.ActivationFunctionType
AX = mybir.AxisListType

D = 1024
EPS = 1e-6
IN_W = 3592
MEM = 256
NEXP = 16384


class Res:
    __slots__ = ("name", "w", "r")

    def __init__(self, name=""):
        self.name = name
        self.w = None
        self.r = []


class Prog:
    ENGS = ("pe", "act", "dve", "pool", "sp")

    def __init__(self, nc, stack, n_dma_sems=64):
        self.nc = nc
        self.n_dma = n_dma_sems
        self.sems = {}
        self.cnt = {}
        for e in self.ENGS:
            self.sems[e] = stack.enter_context(nc.semaphore("s_" + e))
            self.cnt[e] = 0
        for i in range(n_dma_sems):
            k = "dma%d" % i
            self.sems[k] = stack.enter_context(nc.semaphore("s_" + k))
            self.cnt[k] = 0
        self.lists = {e: [] for e in self.ENGS}
        self.seen = {e: {} for e in self.ENGS}
        self.ninst = 0

    def _deps(self, eng, reads, writes, pe_accum=False):
        need = {}

        def add(dep):
            if dep is None:
                return
            k, v = dep
            if need.get(k, 0) < v:
                need[k] = v
        for r in reads:
            add(r.w)
        for w in writes:
            if not (pe_accum and w.w is not None and w.w[0] == "pe" and eng == "pe"):
                add(w.w)
            for rd in w.r:
                if rd[0] == eng:
                    continue
                add(rd)
        waits = []
        seen = self.seen[eng]
        for k, v in need.items():
            if seen.get(k, 0) >= v:
                continue
            seen[k] = v
            waits.append((k, v))
        return waits

    def op(self, eng, fn, reads=(), writes=(), pe_accum=False):
        waits = self._deps(eng, reads, writes, pe_accum)
        self.cnt[eng] += 1
        v = self.cnt[eng]
        self.lists[eng].append((waits, fn, eng, 1))
        for r in reads:
            r.r.append((eng, v))
        for w in writes:
            w.w = (eng, v)
            w.r = []
        self.ninst += 1

    def dma(self, queue, stream, fn, reads=(), writes=()):
        self.rr = (getattr(self, "rr", -1) + 1) % self.n_dma
        k = "dma%d" % self.rr
        waits = self._deps(queue, reads, writes)
        if self.cnt[k] > 0 and self.seen[queue].get(k, 0) < self.cnt[k]:
            self.seen[queue][k] = self.cnt[k]
            waits.append((k, self.cnt[k]))
        self.cnt[k] += 16
        v = self.cnt[k]
        self.lists[queue].append((waits, fn, k, 16))
        for r in reads:
            r.r.append((k, v))
        for w in writes:
            w.w = (k, v)
            w.r = []
        self.ninst += 1

    def barrier(self):
        for e in self.ENGS:
            waits = []
            for k, v in self.cnt.items():
                if k == e or v == 0:
                    continue
                if self.seen[e].get(k, 0) >= v:
                    continue
                self.seen[e][k] = v
                waits.append((k, v))
            if self.cnt[e] > 0 and self.seen[e].get(e, 0) < self.cnt[e]:
                self.seen[e][e] = self.cnt[e]
                waits.append((e, self.cnt[e]))
            self.lists[e].append((waits, None, None, 0))

    def emit(self, block):
        sems = self.sems
        lists = self.lists

        def mk(ename):
            def body(e):
                for waits, fn, k, inc in lists[ename]:
                    for wk, wv in waits:
                        e.wait_ge(sems[wk], wv)
                    if fn is not None:
                        fn(e).then_inc(sems[k], inc)
            return body
        block.tensor(mk("pe"))
        block.scalar(mk("act"))
        block.vector(mk("dve"))
        block.gpsimd(mk("pool"))
        block.sync(mk("sp"))


class T:
    def __init__(self, nc, stack, name, shape, dt, psum=False):
        if psum:
            self.t = stack.enter_context(nc.psum_tensor(name, shape, dt))
        else:
            self.t = stack.enter_context(nc.sbuf_tensor(name, shape, dt))
        self.r = Res(name)

    def __getitem__(self, idx):
        return self.t[idx]


class View:
    def __init__(self, ap, r):
        self.ap = ap
        self.r = r

    def __getitem__(self, idx):
        return self.ap[idx]


ALIBI_SLOPES = [2.0 ** (-8.0 * (i + 1) / 4) for i in range(4)]
LAM_INIT = 0.8 - 0.6 * math.exp(-0.3 * 0)


def build_nc(S=2048, NB=2, stage=99):
    NT = S // 128
    NCH = S // 64
    nc = bass.Bass("TRN2", target_bir_lowering=False)

    def din(name, shape, dt=F32):
        return nc.dram_tensor(name, shape, dt, kind="ExternalInput").ap()

    x_d = din("x", [NB, S, D])
    mem_d = din("mem", [NB, MEM, D])
    g_mix_d = din("norm_mix_g", [D])
    w_in_d = din("w_in", [D, IN_W])
    conv_w_d = din("conv_w", [4, D])
    b_i_d = din("b_igate", [4])
    b_f_d = din("b_fgate", [4])
    ml_g_d = din("ml_norm_g", [512])
    lq1_d = din("lambda_q1", [64])
    lk1_d = din("lambda_k1", [64])
    lq2_d = din("lambda_q2", [64])
    lk2_d = din("lambda_k2", [64])
    da_g_d = din("da_norm_g", [512])
    w_out_d = din("w_out", [D, D])
    g_ca_d = din("norm_ca_g", [D])
    g_mem_d = din("norm_mem_g", [D])
    w_cq_d = din("w_cq", [D, D])
    w_ck_d = din("w_ck", [D, D])
    w_cv_d = din("w_cv", [D, D])
    w_co_d = din("w_co", [D, D])
    g_ffn_d = din("norm_ffn_g", [D])
    w_pq_d = din("w_pq", [D, D])
    sk_d = din("sub_keys", [8, 2, 128, 64])
    pu_d = din("peer_u", [NEXP, D])
    pv_d = din("peer_v", [NEXP, D])
    g_fin_d = din("final_norm_g", [D])
    ident_d = din("c_ident", [128, 128])
    tri_d = din("c_tri", [128, 128])
    alibi_d = din("c_alibi", [128, 64])
    iota16_d = din("c_iota16", [128, 16])
    out_d = nc.dram_tensor("out", [NB, S, D], F32, kind="ExternalOutput").ap()
    hT_d = nc.dram_tensor("hT_scr", [NB, 8, 128, S], BF16, kind="ExternalOutput" if stage == 0 else "Internal").ap()
    if stage == 0:
        dbg1_d = nc.dram_tensor("dbg_x1", [NB, S, D], F32, kind="ExternalOutput").ap()
        dbg2_d = nc.dram_tensor("dbg_x2", [NB, S, D], F32, kind="ExternalOutput").ap()

    with ExitStack() as top:
        P = Prog(nc, top)
        nc_ = nc

        def mkT(st, name, shape, dt, psum=False):
            return T(nc_, st, name, shape, dt, psum)

        def rl(xs):
            return [t if isinstance(t, Res) else t.r for t in xs]

        def TT(eng, out, in0, in1, op, R, W):
            P.op(eng, lambda e: e.tensor_tensor(out=out, in0=in0, in1=in1, op=op), rl(R), rl(W))

        def TS(eng, out, in0, s1, op0, R, W, s2=None, op1=None):
            if op1 is None:
                P.op(eng, lambda e: e.tensor_scalar(out=out, in0=in0, scalar1=s1, scalar2=None, op0=op0), rl(R), rl(W))
            else:
                P.op(eng, lambda e: e.tensor_scalar(out=out, in0=in0, scalar1=s1, scalar2=s2, op0=op0, op1=op1),
                     rl(R), rl(W))

        def STT(out, in0, scalar, in1, op0, op1, R, W):
            P.op("dve", lambda e: e.scalar_tensor_tensor(out=out, in0=in0, scalar=scalar, in1=in1, op0=op0, op1=op1),
                 rl(R), rl(W))

        def ACT(out, in_, func, R, W, bias=None, scale=None, accum=None):
            kw = {}
            if bias is not None:
                kw["bias"] = bias
            if scale is not None:
                kw["scale"] = scale
            if accum is not None:
                kw["accum_out"] = accum
            P.op("act", lambda e: e.activation(out=out, in_=in_, func=func, **kw), rl(R), rl(W))

        def CP(eng, out, in_, R, W):
            if eng == "act":
                P.op("act", lambda e: e.copy(out=out, in_=in_), rl(R), rl(W))
            else:
                P.op(eng, lambda e: e.tensor_copy(out=out, in_=in_), rl(R), rl(W))

        def MM(out, lhsT, rhs, start, stop, R, W):
            P.op("pe", lambda e: e.matmul(out, lhsT=lhsT, rhs=rhs, start=start, stop=stop), rl(R), rl(W),
                 pe_accum=not start)

        def TR(out, in_, ident, R, W):
            P.op("pe", lambda e: e.transpose(out=out, in_=in_, identity=ident), rl(R), rl(W))

        def RECIP(out, in_, R, W):
            P.op("dve", lambda e: e.reciprocal(out=out, in_=in_), rl(R), rl(W))

        def MEMSET(eng, ap, val, W):
            P.op(eng, lambda e: e.memset(ap, val), [], rl(W))

        def DMA(out, in_, R, W, stream=0, queue="sp", nonc=False):
            if nonc:
                P.dma(queue, stream, lambda e: e.dma_start(out=out, in_=in_, allow_slow_non_contiguous=True),
                      rl(R), rl(W))
            else:
                P.dma(queue, stream, lambda e: e.dma_start(out=out, in_=in_), rl(R), rl(W))

        def DBG(name, ap, shape, dt, R):
            if stage != 0:
                return
            d_ = nc.dram_tensor("dbg_" + name, shape, dt, kind="ExternalOutput").ap()
            DMA(d_, ap, R, [], stream=3)

        identf = mkT(top, "identf", [128, 128], F32)
        identb = mkT(top, "identb", [128, 128], BF16)
        trif = mkT(top, "trif", [128, 128], F32)
        trib = mkT(top, "trib", [128, 128], BF16)
        onesf = mkT(top, "onesf", [64, 128], F32)
        alibi = mkT(top, "alibi", [128, 64], F32)
        iota16 = mkT(top, "iota16", [128, 16], F32)
        gT = {}
        for nm, gd in (("mix", g_mix_d), ("ca", g_ca_d), ("mem", g_mem_d), ("ffn", g_ffn_d)):
            gT[nm] = mkT(top, "gT_" + nm, [128, 8], F32)
            DMA(gT[nm][:], gd.rearrange("(c p) -> p c", p=128), [], [gT[nm]], nonc=True)
        DMA(identf[:], ident_d, [], [identf])
        DMA(trif[:], tri_d, [], [trif])
        DMA(alibi[:], alibi_d, [], [alibi])
        DMA(iota16[:], iota16_d, [], [iota16])
        CP("dve", identb[:], identf[:], [identf], [identb])
        CP("dve", trib[:], trif[:], [trif], [trib])
        MEMSET("dve", onesf[:], 1.0, [onesf])
        ptr = [mkT(top, "ptr%d" % i, [128, 1024], BF16, psum=True) for i in range(2)]
        ptf = mkT(top, "ptf", [128, 512], F32, psum=True)
        pm = [mkT(top, "pm%d" % i, [128, 512], F32, psum=True) for i in range(5)]
        pmi = [0]

        def next_pm():
            pmi[0] = (pmi[0] + 1) % 3
            return pm[pmi[0]]
        ptri = [0]

        def next_ptr():
            ptri[0] ^= 1
            return ptr[ptri[0]]

        ss = mkT(top, "ss", [128, 1], F32)
        rstd = mkT(top, "rstd", [128, 1], F32)
        sqj = mkT(top, "sqj", [128, 1024], F32)
        xs_b = mkT(top, "xs_b", [128, 1024], BF16)

        def norm_T(src, dstT_ap, dstT_res, gbc=None, keep_f32=None):
            ACT(sqj[:], src[:], AF.Square, [src], [sqj, ss], accum=ss[:])
            ACT(rstd[:], ss[:], AF.Sqrt, [ss], [rstd], scale=1.0 / D, bias=EPS)
            RECIP(rstd[:], rstd[:], [rstd], [rstd])
            TS("dve", xs_b[:], src[:], rstd[:, 0:1], ALU.mult, [src, rstd], [xs_b])
            if keep_f32 is not None:
                STT(keep_f32[:], src[:], rstd[:, 0:1], gbc[:], ALU.mult, ALU.mult, [src, rstd, gbc], [keep_f32])
            pt = next_ptr()
            for c in range(8):
                TR(pt[:, c * 128:(c + 1) * 128], xs_b[:, c * 128:(c + 1) * 128], identb[:], [xs_b, identb], [pt])
            CP("act", dstT_ap, pt[:].rearrange("p (c t) -> p c t", t=128), [pt], [dstT_res])

        def load_w(st_f32, dst_ap_fn, dst_res, w_dram, col0, ncols, gTt, stream=1):
            DMA(st_f32[:, :, 0:ncols], w_dram[:, col0:col0 + ncols].rearrange("(c p) n -> p c n", p=128),
                [], [st_f32], stream=stream)
            for c in range(8):
                if gTt is None:
                    CP("pool", dst_ap_fn(c), st_f32[:, c, 0:ncols], [st_f32], [dst_res])
                else:
                    TS("pool", dst_ap_fn(c), st_f32[:, c, 0:ncols], gTt[:, c:c + 1], ALU.mult, [st_f32, gTt], [dst_res])

        with ExitStack() as ph:
            wst = mkT(ph, "wst", [128, 8, 128], F32)
            Wh = mkT(ph, "Wh", [128, 8, 512], BF16)
            Wg8 = mkT(ph, "Wg8", [128, 8, 8], BF16)
            xnT = mkT(ph, "xnT", [128, 8, S], BF16)
            hTs = mkT(ph, "hTs", [128, S], BF16)
            xt = mkT(ph, "xt", [128, 1024], F32)
            convT = mkT(ph, "convT", [128, 8, 4], F32)
            bi_bc = mkT(ph, "bi_bc", [64, 4], F32)
            bf_bc = mkT(ph, "bf_bc", [64, 4], F32)
            mlg_bc = mkT(ph, "mlg_bc", [64, 512], F32)
            dag_bc = mkT(ph, "dag_bc", [128, 512], F32)
            lam4 = mkT(ph, "lam4", [128, 4, 64], F32)
            lamj = mkT(ph, "lamj", [128, 64], F32)
            lams = mkT(ph, "lams", [128, 4], F32)
            neglam = mkT(ph, "neglam", [128, 1], F32)
            gz = mkT(ph, "gz", [64, NCH, 8], F32)
            ig = mkT(ph, "ig", [64, NCH, 4], F32)
            lf = mkT(ph, "lf", [64, NCH, 4], F32)
            ea = mkT(ph, "ea", [64, NCH, 4], F32)
            eb = mkT(ph, "eb", [64, NCH, 4], F32)
            eFL = mkT(ph, "eFL", [128, NCH, 4], F32)
            zbuf = mkT(ph, "zbuf", [128, S + 3], F32)
            ybuf = mkT(ph, "ybuf", [128, S], F32)
            qTs = mkT(ph, "qTs", [128, S], BF16)
            kTs = mkT(ph, "kTs", [128, S], BF16)
            k_tok = mkT(ph, "k_tok", [64, NCH, 128], BF16)
            scm = mkT(ph, "scm", [64, NCH, 64], BF16)
            vea = mkT(ph, "vea", [64, NCH, 129], BF16)
            sig_o = mkT(ph, "sig_o", [64, NCH, 128], BF16)
            acc_all = mkT(ph, "acc_all", [64, NCH, 129], F32)
            hml_full = mkT(ph, "hml", [128, NCH, 128], F32)
            hml = View(hml_full[0:64], hml_full.r)
            hmlb = mkT(ph, "hmlb", [64, NCH, 128], BF16)
            C32 = mkT(ph, "C32", [128, 129], F32)
            C32s = mkT(ph, "C32s", [128, 129], F32)
            Cb = mkT(ph, "Cb", [128, 129], BF16)
            st_a = mkT(ph, "st_a", [64, NCH], F32)
            st_b = mkT(ph, "st_b", [64, NCH], F32)
            vda = mkT(ph, "vda", [128, NT, 129], BF16)
            pT = [mkT(ph, "pT%d" % i, [128, 4, 128], BF16) for i in range(2)]
            oda = mkT(ph, "oda", [128, NT, 128], F32)
            odab = mkT(ph, "odab", [128, NT, 128], BF16)
            rz = mkT(ph, "rz", [128, 2], F32)
            sd_a = mkT(ph, "sd_a", [128, NT], F32)
            sd_b = mkT(ph, "sd_b", [128, NT], F32)

            for j in range(4):
                DMA(convT[:, :, j], conv_w_d[j].rearrange("(c p) -> p c", p=128), [], [convT], nonc=True)
            DMA(bi_bc[:], b_i_d.partition_broadcast(64), [], [bi_bc])
            DMA(bf_bc[:], b_f_d.partition_broadcast(64), [], [bf_bc])
            DMA(mlg_bc[:], ml_g_d.partition_broadcast(64), [], [mlg_bc])
            DMA(dag_bc[:], da_g_d.partition_broadcast(128), [], [dag_bc])
            for i, ld in enumerate((lq1_d, lk1_d, lq2_d, lk2_d)):
                DMA(lam4[:, i, :], ld.partition_broadcast(128), [], [lam4])
            for i in range(2):
                TT("dve", lamj[:], lam4[:, 2 * i, :], lam4[:, 2 * i + 1, :], ALU.mult, [lam4], [lamj])
                P.op("dve", lambda e, i=i: e.tensor_reduce(out=lams[:, i:i + 1], in_=lamj[:], axis=AX.X, op=ALU.add),
                     rl([lamj]), rl([lams]))
            ACT(lams[:, 2:4], lams[:, 0:2], AF.Exp, [lams], [lams])
            TT("dve", neglam[:], lams[:, 3:4], lams[:, 2:3], ALU.subtract, [lams], [neglam])
            TS("dve", neglam[:], neglam[:], -LAM_INIT, ALU.add, [neglam], [neglam])
            load_w(wst, lambda c: Wg8[:, c, :], Wg8, w_in_d, 2048, 8, gT["mix"])
            MEMSET("dve", zbuf[:, 0:3], 0.0, [zbuf])
            MEMSET("dve", vda[:, :, 128:129], 1.0, [vda])

            for b in range(NB):
                for ti in range(NT):
                    DMA(xt[:], x_d[b, ti * 128:(ti + 1) * 128, :], [], [xt], stream=2)
                    norm_T(xt, xnT[:, :, ti * 128:(ti + 1) * 128], xnT)
                if b == 0:
                    DBG("xnT", xnT[:], [128, 8, S], BF16, [xnT])
                pg = pm[3]
                for c in range(NCH):
                    for k in range(8):
                        MM(pg[0:64, c * 8:(c + 1) * 8], xnT[:, k, c * 64:(c + 1) * 64], Wg8[:, k, :], k == 0, k == 7,
                           [xnT, Wg8], [pg])
                CP("act", gz[:], pg[0:64, 0:NCH * 8].rearrange("p (c g) -> p c g", g=8), [pg], [gz])
                TT("dve", ig[:], gz[:, :, 0:4], bi_bc[:, None, :].to_broadcast([64, NCH, 4]), ALU.add, [gz, bi_bc], [ig])
                TT("dve", lf[:], gz[:, :, 4:8], bf_bc[:, None, :].to_broadcast([64, NCH, 4]), ALU.add, [gz, bf_bc], [lf])
                ACT(lf[:], lf[:], AF.Exp, [lf], [lf], scale=-1.0)
                ACT(lf[:], lf[:], AF.Ln, [lf], [lf], bias=1.0)
                TS("dve", lf[:], lf[:], -1.0, ALU.mult, [lf], [lf])
                pF = pm[3]
                pFL = pm[4]
                lf2 = lf[:].rearrange("p c g -> p (c g)")
                MM(pF[0:64, 0:NCH * 4], trif[0:64, 0:64], lf2, True, True, [trif, lf], [pF])
                MM(pFL[:, 0:NCH * 4], onesf[:], lf2, True, True, [onesf, lf], [pFL])
                pF3 = pF[0:64, 0:NCH * 4].rearrange("p (c g) -> p c g", g=4)
                TT("dve", ea[:], ig[:], pF3, ALU.subtract, [ig, pF], [ea])
                ACT(ea[:], ea[:], AF.Exp, [ea], [ea])
                ACT(eb[:], pF3, AF.Exp, [pF], [eb], bias=math.log(128 ** -0.5))
                ACT(eFL[:], pFL[:, 0:NCH * 4].rearrange("p (c g) -> p c g", g=4), AF.Exp, [pFL], [eFL])

                if b == 0:
                    DBG("gz", gz[:], [64, NCH, 8], F32, [gz])
                    DBG("lf", lf[:], [64, NCH, 4], F32, [lf])
                    DBG("ea", ea[:], [64, NCH, 4], F32, [ea])
                    DBG("eb", eb[:], [64, NCH, 4], F32, [eb])
                    DBG("eFL", eFL[:], [128, NCH, 4], F32, [eFL])
                for h in range(4):
                    for jj, c0 in enumerate((h * 128, 512 + h * 128, 1024 + h * 128, 1536 + h * 128)):
                        load_w(wst, lambda c, jj=jj: Wh[:, c, jj * 128:(jj + 1) * 128], Wh, w_in_d, c0, 128, gT["mix"])
                    for j, dstq in enumerate((qTs, kTs)):
                        for tr in range(S // 512):
                            pz = next_pm()
                            for k in range(8):
                                MM(pz[:, :], Wh[:, k, j * 128:(j + 1) * 128], xnT[:, k, tr * 512:(tr + 1) * 512],
                                   k == 0, k == 7, [Wh, xnT], [pz])
                            CP("act", zbuf[:, 3 + tr * 512:3 + (tr + 1) * 512], pz[:, :], [pz], [zbuf])
                        cidx = j * 4 + h
                        TS("dve", ybuf[:], zbuf[:, 0:S], convT[:, cidx, 0:1], ALU.mult, [zbuf, convT], [ybuf])
                        for tap in range(1, 4):
                            STT(ybuf[:], zbuf[:, tap:tap + S], convT[:, cidx, tap:tap + 1], ybuf[:], ALU.mult, ALU.add,
                                [zbuf, convT, ybuf], [ybuf])
                        if b == 0 and h == 0 and j == 0:
                            DBG("zbuf", zbuf[:], [128, S + 3], F32, [zbuf])
                            DBG("ybuf", ybuf[:], [128, S], F32, [ybuf])
                            DBG("convT", convT[:], [128, 8, 4], F32, [convT])
                        ACT(dstq[:], ybuf[:], AF.Silu, [ybuf], [dstq])
                    for c in range(NCH):
                        pv = next_pm()
                        for k in range(8):
                            MM(pv[0:64, 0:256], xnT[:, k, c * 64:(c + 1) * 64], Wh[:, k, 256:512], k == 0, k == 7,
                               [xnT, Wh], [pv])
                        ACT(sig_o[:, c, :], pv[0:64, 128:256], AF.Sigmoid, [pv], [sig_o])
                        TS("dve", vea[:, c, 0:128], pv[0:64, 0:128], ea[:, c, h:h + 1], ALU.mult, [pv, ea, sig_o], [vea])
                    CP("dve", vea[:, :, 128:129], ea[:, :, h:h + 1], [ea], [vea])
                    if b == 0 and h == 0:
                        DBG("qTs", qTs[:], [128, S], BF16, [qTs])
                        DBG("kTs", kTs[:], [128, S], BF16, [kTs])
                        DBG("vea", vea[:], [64, NCH, 129], BF16, [vea])
                        DBG("sig_o", sig_o[:], [64, NCH, 128], BF16, [sig_o])
                    for c0 in range(0, NCH, 8):
                        n8 = min(8, NCH - c0)
                        pt = next_ptr()
                        for cc in range(n8):
                            c = c0 + cc
                            TR(pt[0:64, cc * 128:(cc + 1) * 128], kTs[:, c * 64:(c + 1) * 64], identb[:],
                               [kTs, identb], [pt])
                        CP("act", k_tok[:, c0:c0 + n8, :], pt[0:64, 0:n8 * 128].rearrange("p (c d) -> p c d", d=128), [pt], [k_tok])
                        ps = next_pm()
                        for cc in range(n8):
                            c = c0 + cc
                            MM(ps[0:64, cc * 64:(cc + 1) * 64], kTs[:, c * 64:(c + 1) * 64], qTs[:, c * 64:(c + 1) * 64],
                               True, True, [kTs, qTs], [ps])
                        TT("dve", scm[:, c0:c0 + n8, :], ps[0:64, 0:n8 * 64].rearrange("p (c l) -> p c l", l=64),
                           trif[0:64, None, 0:64].to_broadcast([64, n8, 64]), ALU.mult, [ps, trif], [scm])
                    for c in range(NCH):
                        pa = pm[3]
                        MM(pa[0:64, 0:129], scm[:, c, :], vea[:, c, :], True, c == 0, [scm, vea], [pa])
                        if c > 0:
                            MM(pa[0:64, 0:129], qTs[:, c * 64:(c + 1) * 64], Cb[:, :], False, True, [qTs, Cb], [pa])
                        CP("act", acc_all[:, c, :], pa[0:64, 0:129], [pa], [acc_all])
                        if c < NCH - 1:
                            pu = pm[4]
                            MM(pu[:, 0:129], k_tok[:, c, :], vea[:, c, :], True, True, [k_tok, vea], [pu])
                            if c == 0:
                                TS("dve", C32[:], pu[:, 0:129], eFL[:, c, h:h + 1], ALU.mult, [pu, eFL], [C32])
                            else:
                                TS("dve", C32s[:], C32[:], eFL[:, c, h:h + 1], ALU.mult, [C32, eFL], [C32s])
                                STT(C32[:], pu[:, 0:129], eFL[:, c, h:h + 1], C32s[:], ALU.mult, ALU.add,
                                    [pu, eFL, C32s], [C32])
                            CP("dve", Cb[:], C32[:], [C32], [Cb])
                    if b == 0 and h == 0:
                        DBG("acc_all", acc_all[:], [64, NCH, 129], F32, [acc_all])
                        DBG("scm", scm[:], [64, NCH, 64], BF16, [scm])
                    TT("dve", st_a[:], acc_all[:, :, 128], eb[:, :, h], ALU.mult, [acc_all, eb], [st_a])
                    ACT(st_a[:], st_a[:], AF.Abs, [st_a], [st_a])
                    TS("dve", st_a[:], st_a[:], 1.0, ALU.max, [st_a], [st_a])
                    RECIP(st_a[:], st_a[:], [st_a], [st_a])
                    TT("dve", st_b[:], eb[:, :, h], st_a[:], ALU.mult, [eb, st_a], [st_b])
                    TT("dve", hml[:], acc_all[:, :, 0:128], st_b[:].unsqueeze(2).to_broadcast([64, NCH, 128]), ALU.mult,
                       [acc_all, st_b], [hml])
                    TT("pool", acc_all[:, :, 0:128], hml[:], hml[:], ALU.mult, [hml], [acc_all])
                    P.op("dve", lambda e: e.tensor_reduce(out=st_a[:], in_=acc_all[:, :, 0:128], axis=AX.X, op=ALU.add),
                         rl([acc_all]), rl([st_a]))
                    ACT(st_a[:], st_a[:], AF.Sqrt, [st_a], [st_a], scale=1.0 / 128, bias=EPS)
                    RECIP(st_b[:], st_a[:], [st_a], [st_b])
                    TT("dve", hml[:], hml[:], st_b[:].unsqueeze(2).to_broadcast([64, NCH, 128]), ALU.mult, [hml, st_b], [hml])
                    TT("dve", hml[:], hml[:], mlg_bc[:, None, h * 128:(h + 1) * 128].to_broadcast([64, NCH, 128]), ALU.mult,
                       [hml, mlg_bc], [hml])
                    TT("dve", hmlb[:], hml[:], sig_o[:], ALU.mult, [hml, sig_o], [hmlb])
                    for c0 in range(0, NCH, 16):
                        n16 = min(16, NCH - c0)
                        pt = next_ptr()
                        for cc in range(n16):
                            TR(pt[:, cc * 64:(cc + 1) * 64], hmlb[:, c0 + cc, :], identb[0:64, 0:64], [hmlb, identb], [pt])
                        CP("act", hTs[:, c0 * 64:(c0 + n16) * 64], pt[:, 0:n16 * 64], [pt], [hTs])
                    DMA(hT_d[b, h], hTs[:], [hTs], [], stream=3)

                for h in range(4):
                    for jj, c0 in enumerate((2056 + h * 128, 2568 + h * 128, 3080 + h * 128)):
                        load_w(wst, lambda c, jj=jj: Wh[:, c, jj * 128:(jj + 1) * 128], Wh, w_in_d, c0, 128, gT["mix"])
                    for j, dstq in enumerate((qTs, kTs)):
                        for tr in range(S // 512):
                            pz = next_pm()
                            for k in range(8):
                                MM(pz[:, :], Wh[:, k, j * 128:(j + 1) * 128], xnT[:, k, tr * 512:(tr + 1) * 512],
                                   k == 0, k == 7, [Wh, xnT], [pz])
                            CP("act", dstq[:, tr * 512:(tr + 1) * 512], pz[:, :], [pz], [dstq])
                    for i in range(NT):
                        pv = next_pm()
                        for k in range(8):
                            MM(pv[:, 0:128], xnT[:, k, i * 128:(i + 1) * 128], Wh[:, k, 256:384], k == 0, k == 7,
                               [xnT, Wh], [pv])
                        CP("dve", vda[:, i, 0:128], pv[:, 0:128], [pv], [vda])
                    pti = 0
                    for j in range(NT):
                        pacc = (pm[3], pm[4])
                        for cm in range(2):
                            lo, hi = cm * 64, (cm + 1) * 64
                            for i0 in range(0, j + 1, 4):
                                ii_list = list(range(i0, min(i0 + 4, j + 1)))
                                ps = next_pm()
                                pTt = pT[pti]
                                pti ^= 1
                                for ii, i in enumerate(ii_list):
                                    MM(ps[:, ii * 128:(ii + 1) * 128], kTs[lo:hi, i * 128:(i + 1) * 128],
                                       qTs[lo:hi, j * 128:(j + 1) * 128], True, True, [kTs, qTs], [ps])
                                for ii, i in enumerate(ii_list):
                                    m = j - i
                                    ACT(pTt[:, ii, :], ps[:, ii * 128:(ii + 1) * 128], AF.Exp, [ps, alibi], [pTt],
                                        bias=alibi[:, h * 16 + m:h * 16 + m + 1], scale=0.125)
                                    if i == j:
                                        TT("dve", pTt[:, ii, :], pTt[:, ii, :], trib[:], ALU.mult, [pTt, trib], [pTt])
                                for ii, i in enumerate(ii_list):
                                    MM(pacc[cm][:, 0:129], pTt[:, ii, :], vda[:, i, :], i == 0, i == j, [pTt, vda], [pacc[cm]])
                        RECIP(rz[:, 0:1], pacc[0][:, 128:129], [pacc[0]], [rz])
                        RECIP(rz[:, 1:2], pacc[1][:, 128:129], [pacc[1]], [rz])
                        TT("dve", rz[:, 1:2], rz[:, 1:2], neglam[:], ALU.mult, [rz, neglam], [rz])
                        ACT(oda[:, j, :], pacc[0][:, 0:128], AF.Identity, [pacc[0], rz], [oda], scale=rz[:, 0:1])
                        STT(oda[:, j, :], pacc[1][:, 0:128], rz[:, 1:2], oda[:, j, :], ALU.mult, ALU.add,
                            [pacc[1], rz, oda], [oda])
                    TT("pool", hml_full[:, 0:NT, :], oda[:], oda[:], ALU.mult, [oda], [hml_full])
                    P.op("dve", lambda e: e.tensor_reduce(out=sd_a[:], in_=hml_full[:, 0:NT, :], axis=AX.X, op=ALU.add),
                         rl([hml_full]), rl([sd_a]))
                    ACT(sd_a[:], sd_a[:], AF.Sqrt, [sd_a], [sd_a], scale=1.0 / 128, bias=EPS)
                    RECIP(sd_b[:], sd_a[:], [sd_a], [sd_b])
                    TT("dve", oda[:], oda[:], sd_b[:].unsqueeze(2).to_broadcast([128, NT, 128]), ALU.mult, [oda, sd_b], [oda])
                    STT(odab[:], oda[:], 1.0 - LAM_INIT, dag_bc[:, None, h * 128:(h + 1) * 128].to_broadcast([128, NT, 128]),
                        ALU.mult, ALU.mult, [oda, dag_bc], [odab])
                    for j0 in range(0, NT, 8):
                        nj = min(8, NT - j0)
                        pt = next_ptr()
                        for jj in range(nj):
                            TR(pt[:, jj * 128:(jj + 1) * 128], odab[:, j0 + jj, :], identb[:], [odab, identb], [pt])
                        CP("act", hTs[:, j0 * 128:(j0 + nj) * 128], pt[:, 0:nj * 128], [pt], [hTs])
                    DMA(hT_d[b, 4 + h], hTs[:], [hTs], [], stream=3)
            P.barrier()

        with ExitStack() as ph:
            Wout = mkT(ph, "Wout", [128, 8, 1024], BF16)
            Wcq = mkT(ph, "Wcq", [128, 8, 1024], BF16)
            Wco = mkT(ph, "Wco", [128, 8, 1024], BF16)
            Wpq = mkT(ph, "Wpq", [128, 8, 1024], BF16)
            skT = mkT(ph, "skT", [128, 8, 128], BF16)
            KT = [mkT(ph, "KT%d" % b, [128, 8, MEM], BF16) for b in range(NB)]
            Vb = [mkT(ph, "Vb%d" % b, [128, 2, 4, 257], BF16) for b in range(NB)]
            gffn_bc = mkT(ph, "gffn_bc", [128, 1024], F32)
            gfin_bc = mkT(ph, "gfin_bc", [128, 1024], F32)
            DMA(gffn_bc[:], g_ffn_d.partition_broadcast(128), [], [gffn_bc])
            DMA(gfin_bc[:], g_fin_d.partition_broadcast(128), [], [gfin_bc])
            with ExitStack() as s3:
                wst = mkT(s3, "wst3", [128, 8, 512], F32)
                Wck = mkT(s3, "Wck", [128, 8, 1024], BF16)
                Wcv = mkT(s3, "Wcv", [128, 8, 1024], BF16)
                memT = mkT(s3, "memT", [128, 8, MEM], BF16)
                mt_t = mkT(s3, "mt_t", [128, 1024], F32)
                sk_nat = mkT(s3, "sk_nat", [128, 8, 128], F32)
                for (Wd, wdram, g) in ((Wout, w_out_d, None), (Wcq, w_cq_d, gT["ca"]), (Wco, w_co_d, None),
                                       (Wpq, w_pq_d, gT["ffn"]), (Wck, w_ck_d, gT["mem"]), (Wcv, w_cv_d, gT["mem"])):
                    for half in range(2):
                        load_w(wst, lambda c, Wd=Wd, half=half: Wd[:, c, half * 512:(half + 1) * 512], Wd, wdram,
                               half * 512, 512, g)
                for hh in range(8):
                    for c in range(2):
                        DMA(sk_nat[:, hh, c * 64:(c + 1) * 64], sk_d[hh, c], [], [sk_nat])
                for hh in range(8):
                    TR(ptf[:, 0:128], sk_nat[:, hh, :], identf[:], [sk_nat, identf], [ptf])
                    CP("act", skT[:, hh, :], ptf[:, 0:128], [ptf], [skT])
                for b in range(NB):
                    MEMSET("dve", Vb[b][:, :, :, 256:257], 1.0, [Vb[b]])
                    for mt in range(2):
                        DMA(mt_t[:], mem_d[b, mt * 128:(mt + 1) * 128, :], [], [mt_t], stream=2)
                        norm_T(mt_t, memT[:, :, mt * 128:(mt + 1) * 128], memT)
                    for dch in range(8):
                        pk = next_pm()
                        for k in range(8):
                            MM(pk[:, 0:MEM], Wck[:, k, dch * 128:(dch + 1) * 128], memT[:, k, :], k == 0, k == 7,
                               [Wck, memT], [pk])
                        CP("act", KT[b][:, dch, :], pk[:, 0:MEM], [pk], [KT[b]])
                    for mt in range(2):
                        for half in range(2):
                            pk = next_pm()
                            for k in range(8):
                                MM(pk[:, :], memT[:, k, mt * 128:(mt + 1) * 128], Wcv[:, k, half * 512:(half + 1) * 512],
                                   k == 0, k == 7, [memT, Wcv], [pk])
                            CP("act", Vb[b][:, mt, half * 2:(half + 1) * 2, 0:256],
                               pk[:, :].rearrange("p (h d) -> p h d", d=256), [pk], [Vb[b]])
                P.barrier()

            xt = mkT(ph, "xt3", [128, 1024], F32)
            hTt = mkT(ph, "hTt", [128, 8, 128], BF16)
            x1 = mkT(ph, "x1", [128, 1024], F32)
            x1T = mkT(ph, "x1T", [128, 8, 128], BF16)
            QT = mkT(ph, "QT", [128, 8, 128], BF16)
            pTc = mkT(ph, "pTc", [128, 2, 128], BF16)
            oca = mkT(ph, "oca", [128, 1024], BF16)
            ocaT = mkT(ph, "ocaT", [128, 8, 128], BF16)
            x2 = mkT(ph, "x2", [128, 1024], F32)
            xn2 = mkT(ph, "xn2", [128, 1024], F32)
            x2T = mkT(ph, "x2T", [128, 8, 128], BF16)
            qpT = mkT(ph, "qpT", [128, 8, 128], BF16)
            sc = mkT(ph, "sc", [128, 16, 128], F32)
            work = mkT(ph, "work", [128, 256], F32)
            tv = mkT(ph, "tv", [128, 16, 16], F32)
            tiu = mkT(ph, "tiu", [128, 16, 16], U32)
            tif = mkT(ph, "tif", [128, 16, 16], F32)
            cand = mkT(ph, "cand", [128, 8, 16, 16], F32)
            bv = mkT(ph, "bv", [128, 8, 16], F32)
            bju = mkT(ph, "bju", [128, 8, 16], U32)
            k1u = mkT(ph, "k1u", [128, 8, 16], U32)
            k2u = mkT(ph, "k2u", [128, 8, 16], U32)
            k1f = mkT(ph, "k1f", [128, 8, 16], F32)
            k2f = mkT(ph, "k2f", [128, 8, 16], F32)
            oh = mkT(ph, "oh", [128, 8, 16, 16], F32)
            i1f = mkT(ph, "i1f", [128, 8, 16], F32)
            i2f = mkT(ph, "i2f", [128, 8, 16], F32)
            eidx = mkT(ph, "eidx", [128, 128], I32)
            gate = mkT(ph, "gate", [128, 8, 16], F32)
            gs = mkT(ph, "gs", [128, 8], F32)
            actv = mkT(ph, "actv", [128, 128], F32)
            coef = mkT(ph, "coef", [128, 128], F32)
            pacc_sb = mkT(ph, "pacc_sb", [128, 1024], F32)
            junk = mkT(ph, "junk", [128, 1024], F32)
            rzc = mkT(ph, "rzc", [128, 1], F32)
            NGB = 6
            gbuf = [mkT(ph, "gbuf%d" % i, [128, 1024], F32) for i in range(NGB)]
            gbi = [0]

            for b in range(NB):
                for ti in range(NT):
                    tsl = slice(ti * 128, (ti + 1) * 128)
                    DMA(xt[:], x_d[b, tsl, :], [], [xt], stream=2)
                    DMA(hTt[:], hT_d[b, :, :, tsl].rearrange("c p t -> p c t"), [], [hTt], stream=2)
                    for half in range(2):
                        po = next_pm()
                        for c in range(8):
                            MM(po[:, :], hTt[:, c, :], Wout[:, c, half * 512:(half + 1) * 512], c == 0, c == 7,
                               [hTt, Wout], [po])
                        TT("dve", x1[:, half * 512:(half + 1) * 512], po[:, :], xt[:, half * 512:(half + 1) * 512], ALU.add,
                           [po, xt], [x1])
                    norm_T(x1, x1T[:], x1T)
                    for d0 in range(0, 8, 4):
                        pq = next_pm()
                        for dd in range(4):
                            dch = d0 + dd
                            for k in range(8):
                                MM(pq[:, dd * 128:(dd + 1) * 128], Wcq[:, k, dch * 128:(dch + 1) * 128], x1T[:, k, :],
                                   k == 0, k == 7, [Wcq, x1T], [pq])
                        CP("act", QT[:, d0:d0 + 4, :], pq[:, :].rearrange("p (c t) -> p c t", t=128), [pq], [QT])
                    for h in range(4):
                        ps = next_pm()
                        for mt in range(2):
                            for dd in range(2):
                                MM(ps[:, mt * 128:(mt + 1) * 128], KT[b][:, 2 * h + dd, mt * 128:(mt + 1) * 128],
                                   QT[:, 2 * h + dd, :], dd == 0, dd == 1, [KT[b], QT], [ps])
                        ACT(pTc[:], ps[:, 0:256].rearrange("p (m q) -> p m q", q=128), AF.Exp, [ps], [pTc], scale=1.0 / 16)
                        pa = pm[3]
                        for mt in range(2):
                            MM(pa[:, 0:257], pTc[:, mt, :], Vb[b][:, mt, h, :], mt == 0, mt == 1, [pTc, Vb[b]], [pa])
                        RECIP(rzc[:], pa[:, 256:257], [pa], [rzc])
                        ACT(oca[:, h * 256:(h + 1) * 256], pa[:, 0:256], AF.Identity, [pa, rzc], [oca], scale=rzc[:, 0:1])
                    pt = next_ptr()
                    for c in range(8):
                        TR(pt[:, c * 128:(c + 1) * 128], oca[:, c * 128:(c + 1) * 128], identb[:], [oca, identb], [pt])
                    CP("act", ocaT[:], pt[:].rearrange("p (c t) -> p c t", t=128), [pt], [ocaT])
                    for half in range(2):
                        po = next_pm()
                        for c in range(8):
                            MM(po[:, :], ocaT[:, c, :], Wco[:, c, half * 512:(half + 1) * 512], c == 0, c == 7,
                               [ocaT, Wco], [po])
                        TT("dve", x2[:, half * 512:(half + 1) * 512], po[:, :], x1[:, half * 512:(half + 1) * 512], ALU.add,
                           [po, x1], [x2])
                    if stage == 0:
                        DMA(dbg1_d[b, tsl, :], x1[:], [x1], [], stream=3)
                        DMA(dbg2_d[b, tsl, :], x2[:], [x2], [], stream=3)
                    norm_T(x2, x2T[:], x2T, gbc=gffn_bc, keep_f32=xn2)
                    for d0 in range(0, 8, 4):
                        pq = next_pm()
                        for dd in range(4):
                            dch = d0 + dd
                            for k in range(8):
                                MM(pq[:, dd * 128:(dd + 1) * 128], Wpq[:, k, dch * 128:(dch + 1) * 128], x2T[:, k, :],
                                   k == 0, k == 7, [Wpq, x2T], [pq])
                        CP("act", qpT[:, d0:d0 + 4, :], pq[:, :].rearrange("p (c t) -> p c t", t=128), [pq], [qpT])
                    for s0 in range(0, 16, 4):
                        psc = next_pm()
                        for s_ in range(4):
                            st_ = s0 + s_
                            hp, c = st_ // 2, st_ % 2
                            MM(psc[:, s_ * 128:(s_ + 1) * 128], qpT[c * 64:(c + 1) * 64, hp, :], skT[c * 64:(c + 1) * 64, hp, :],
                               True, True, [qpT, skT], [psc])
                        CP("act", sc[:, s0:s0 + 4, :], psc[:, :].rearrange("p (s n) -> p s n", n=128), [psc], [sc])
                    for st_ in range(16):
                        P.op("dve", lambda e, st_=st_: e.max(out=tv[:, st_, 0:8], in_=sc[:, st_, :]), rl([sc]), rl([tv]))
                        P.op("dve", lambda e, st_=st_: e.max_index(out=tiu[:, st_, 0:8], in_max=tv[:, st_, 0:8],
                                                                  in_values=sc[:, st_, :]), rl([sc, tv]), rl([tiu]))
                        P.op("dve", lambda e, st_=st_: e.match_replace(out=work[:, 0:128], in_to_replace=tv[:, st_, 0:8],
                                                                      in_values=sc[:, st_, :], imm_value=-1e30),
                             rl([sc, tv]), rl([work]))
                        P.op("dve", lambda e, st_=st_: e.max(out=tv[:, st_, 8:16], in_=work[:, 0:128]), rl([work]), rl([tv]))
                        P.op("dve", lambda e, st_=st_: e.max_index(out=tiu[:, st_, 8:16], in_max=tv[:, st_, 8:16],
                                                                  in_values=work[:, 0:128]), rl([work, tv]), rl([tiu]))
                    CP("dve", tif[:], tiu[:], [tiu], [tif])
                    tv4 = tv[:].rearrange("p (h c) k -> p h c k", c=2)
                    tif4 = tif[:].rearrange("p (h c) k -> p h c k", c=2)
                    TT("dve", cand[:], tv4[:, :, 0, :].unsqueeze(3).to_broadcast([128, 8, 16, 16]),
                       tv4[:, :, 1, :].unsqueeze(2).to_broadcast([128, 8, 16, 16]), ALU.add, [tv], [cand])
                    for hp in range(8):
                        cv = cand[:, hp, :, :].rearrange("p a b -> p (a b)")
                        P.op("dve", lambda e, hp=hp, cv=cv: e.max(out=bv[:, hp, 0:8], in_=cv), rl([cand]), rl([bv]))
                        P.op("dve", lambda e, hp=hp, cv=cv: e.max_index(out=bju[:, hp, 0:8], in_max=bv[:, hp, 0:8],
                                                                       in_values=cv), rl([cand, bv]), rl([bju]))
                        P.op("dve", lambda e, hp=hp, cv=cv: e.match_replace(out=work[:], in_to_replace=bv[:, hp, 0:8],
                                                                           in_values=cv, imm_value=-1e30),
                             rl([cand, bv]), rl([work]))
                        P.op("dve", lambda e, hp=hp: e.max(out=bv[:, hp, 8:16], in_=work[:]), rl([work]), rl([bv]))
                        P.op("dve", lambda e, hp=hp: e.max_index(out=bju[:, hp, 8:16], in_max=bv[:, hp, 8:16],
                                                                in_values=work[:]), rl([work, bv]), rl([bju]))
                    P.op("dve", lambda e: e.tensor_single_scalar(out=k1u[:], in_=bju[:], scalar=4,
                                                                 op=ALU.logical_shift_right), rl([bju]), rl([k1u]))
                    P.op("dve", lambda e: e.tensor_single_scalar(out=k2u[:], in_=bju[:], scalar=15,
                                                                 op=ALU.bitwise_and), rl([bju]), rl([k2u]))
                    CP("dve", k1f[:], k1u[:], [k1u], [k1f])
                    CP("dve", k2f[:], k2u[:], [k2u], [k2f])
                    for (kf, cidx, dst) in ((k1f, 0, i1f), (k2f, 1, i2f)):
                        TT("dve", oh[:], kf[:].unsqueeze(3).to_broadcast([128, 8, 16, 16]),
                           iota16[:, None, None, :].to_broadcast([128, 8, 16, 16]), ALU.is_equal, [kf, iota16], [oh])
                        TT("dve", oh[:], oh[:], tif4[:, :, cidx, :].unsqueeze(2).to_broadcast([128, 8, 16, 16]), ALU.mult,
                           [oh, tif], [oh])
                        P.op("dve", lambda e, dst=dst: e.tensor_reduce(out=dst[:], in_=oh[:], axis=AX.X, op=ALU.add),
                             rl([oh]), rl([dst]))
                    STT(i1f[:], i1f[:], 128.0, i2f[:], ALU.mult, ALU.add, [i1f, i2f], [i1f])
                    TS("dve", i1f[:], i1f[:], 0.0, ALU.max, [i1f], [i1f], s2=float(NEXP - 1), op1=ALU.min)
                    CP("dve", eidx[:], i1f[:].rearrange("p h k -> p (h k)"), [i1f], [eidx])
                    TT("dve", gate[:], bv[:], bv[:, :, 0:1].to_broadcast([128, 8, 16]), ALU.subtract, [bv], [gate])
                    ACT(gate[:], gate[:], AF.Exp, [gate], [gate])
                    P.op("dve", lambda e: e.tensor_reduce(out=gs[:], in_=gate[:], axis=AX.X, op=ALU.add), rl([gate]), rl([gs]))
                    RECIP(gs[:], gs[:], [gs], [gs])
                    TT("dve", gate[:], gate[:], gs[:].unsqueeze(2).to_broadcast([128, 8, 16]), ALU.mult, [gate, gs], [gate])
                    for sl in range(128):
                        gb = gbuf[gbi[0]]
                        gbi[0] = (gbi[0] + 1) % NGB
                        P.dma("pool", 4 + (gbi[0] % NGB), lambda e, gb=gb, sl=sl: e.indirect_dma_start(
                            out=gb[:], out_offset=None, in_=pu_d,
                            in_offset=bass.IndirectOffsetOnAxis(ap=eidx[:, sl:sl + 1], axis=0)), rl([eidx]), rl([gb]))
                        P.op("dve", lambda e, gb=gb, sl=sl: e.scalar_tensor_tensor(
                            out=junk[:], in0=gb[:], scalar=1.0, in1=xn2[:], op0=ALU.mult, op1=ALU.mult,
                            accum_out=actv[:, sl:sl + 1]), rl([gb, xn2]), rl([junk, actv]))
                    ACT(coef[:], actv[:], AF.Gelu, [actv], [coef])
                    TT("dve", coef[:], coef[:], gate[:].rearrange("p h k -> p (h k)"), ALU.mult, [coef, gate], [coef])
                    for sl in range(128):
                        gb = gbuf[gbi[0]]
                        gbi[0] = (gbi[0] + 1) % NGB
                        P.dma("pool", 4 + (gbi[0] % NGB), lambda e, gb=gb, sl=sl: e.indirect_dma_start(
                            out=gb[:], out_offset=None, in_=pv_d,
                            in_offset=bass.IndirectOffsetOnAxis(ap=eidx[:, sl:sl + 1], axis=0)), rl([eidx]), rl([gb]))
                        if sl == 0:
                            STT(pacc_sb[:], gb[:], coef[:, 0:1], x2[:], ALU.mult, ALU.add, [gb, coef, x2], [pacc_sb])
                        else:
                            STT(pacc_sb[:], gb[:], coef[:, sl:sl + 1], pacc_sb[:], ALU.mult, ALU.add,
                                [gb, coef, pacc_sb], [pacc_sb])
                    ACT(sqj[:], pacc_sb[:], AF.Square, [pacc_sb], [sqj, ss], accum=ss[:])
                    ACT(rstd[:], ss[:], AF.Sqrt, [ss], [rstd], scale=1.0 / D, bias=EPS)
                    RECIP(rstd[:], rstd[:], [rstd], [rstd])
                    STT(junk[:], pacc_sb[:], rstd[:, 0:1], gfin_bc[:], ALU.mult, ALU.mult, [pacc_sb, rstd, gfin_bc], [junk])
                    out_res = Res("out")
                    DMA(out_d[b, tsl, :], junk[:], [junk], [out_res], stream=3)
            P.barrier()

        with nc.Block() as block:
            P.emit(block)
    return nc, P


def make_consts():
    ident = np.eye(128, dtype=np.float32)
    tri = np.triu(np.ones((128, 128), np.float32))
    al = np.zeros((128, 64), np.float32)
    k = np.arange(128, dtype=np.float32)
    for h in range(4):
        for m in range(16):
            al[:, h * 16 + m] = ALIBI_SLOPES[h] * (k - 128.0 * m)
    iota16 = np.tile(np.arange(16, dtype=np.float32)[None, :], (128, 1))
    return {"c_ident": ident, "c_tri": tri, "c_alibi": al, "c_iota16": iota16}


_CACHE = {}


def kernel(**inputs):
    NC = 8
    x = np.asarray(inputs["x"], np.float32)
    B, S, _ = x.shape
    NB = B // NC
    key = (S, NB)
    if key not in _CACHE:
        _CACHE[key] = build_nc(S, NB)[0]
    nc = _CACHE[key]
    shared = {}
    for k_, v in inputs.items():
        if k_ in ("x", "mem"):
            continue
        a = np.ascontiguousarray(np.asarray(v, np.float32))
        if k_ != "final_norm_g":
            a = a[0]
        shared[k_] = np.ascontiguousarray(a)
    shared.update(make_consts())
    mem = np.asarray(inputs["mem"], np.float32)
    in_maps = []
    for c in range(NC):
        m = dict(shared)
        m["x"] = np.ascontiguousarray(x[c * NB:(c + 1) * NB])
        m["mem"] = np.ascontiguousarray(mem[c * NB:(c + 1) * NB])
        in_maps.append(m)
    res = run_bass_kernel_spmd(nc, in_maps, core_ids=list(range(NC)))
    out = np.concatenate([np.asarray(r["out"]) for r in res.results], axis=0)
    return out.astype(np.float32)
```

```python
import math
from contextlib import ExitStack
import numpy as np
import concourse.bass as bass
import concourse.mybir as mybir
from concourse.bass_utils import run_bass_kernel_spmd

F32 = mybir.dt.float32
BF16 = mybir.dt.bfloat16
I32 = mybir.dt.int32
U32 = mybir.dt.uint32
ALU = mybir.AluOpType
AF = mybir.ActivationFunctionType
AX = mybir.AxisListType

D = 1024
EPS = 1e-6
IN_W = 3592
MEM = 256
NEXP = 16384


class Res:
    __slots__ = ("name", "w", "r")

    def __init__(self, name=""):
        self.name = name
        self.w = None
        self.r = []


class Prog:
    ENGS = ("pe", "act", "dve", "pool", "sp")

    def __init__(self, nc, stack, n_dma_sems=64):
        self.nc = nc
        self.n_dma = n_dma_sems
        self.sems = {}
        self.cnt = {}
        for e in self.ENGS:
            self.sems[e] = stack.enter_context(nc.semaphore("s_" + e))
            self.cnt[e] = 0
        for i in range(n_dma_sems):
            k = "dma%d" % i
            self.sems[k] = stack.enter_context(nc.semaphore("s_" + k))
            self.cnt[k] = 0
        self.lists = {e: [] for e in self.ENGS}
        self.seen = {e: {} for e in self.ENGS}
        self.ninst = 0

    def _deps(self, eng, reads, writes, pe_accum=False):
        need = {}

        def add(dep):
            if dep is None:
                return
            k, v = dep
            if need.get(k, 0) < v:
                need[k] = v
        for r in reads:
            add(r.w)
        for w in writes:
            if not (pe_accum and w.w is not None and w.w[0] == "pe" and eng == "pe"):
                add(w.w)
            for rd in w.r:
                add(rd)
        waits = []
        seen = self.seen[eng]
        for k, v in need.items():
            if seen.get(k, 0) >= v:
                continue
            seen[k] = v
            waits.append((k, v))
        return waits

    def op(self, eng, fn, reads=(), writes=(), pe_accum=False):
        waits = self._deps(eng, reads, writes, pe_accum)
        self.cnt[eng] += 1
        v = self.cnt[eng]
        self.lists[eng].append((waits, fn, eng, 1))
        for r in reads:
            r.r.append((eng, v))
        for w in writes:
            w.w = (eng, v)
            w.r = []
        self.ninst += 1

    def dma(self, queue, stream, fn, reads=(), writes=()):
        self.rr = (getattr(self, "rr", -1) + 1) % self.n_dma
        k = "dma%d" % self.rr
        waits = self._deps(queue, reads, writes)
        if self.cnt[k] > 0 and self.seen[queue].get(k, 0) < self.cnt[k]:
            self.seen[queue][k] = self.cnt[k]
            waits.append((k, self.cnt[k]))
        self.cnt[k] += 16
        v = self.cnt[k]
        self.lists[queue].append((waits, fn, k, 16))
        for r in reads:
            r.r.append((k, v))
        for w in writes:
            w.w = (k, v)
            w.r = []
        self.ninst += 1

    def barrier(self):
        for e in self.ENGS:
            waits = []
            for k, v in self.cnt.items():
                if k == e or v == 0:
                    continue
                if self.seen[e].get(k, 0) >= v:
                    continue
                self.seen[e][k] = v
                waits.append((k, v))
            if self.cnt[e] > 0 and self.seen[e].get(e, 0) < self.cnt[e]:
                self.seen[e][e] = self.cnt[e]
                waits.append((e, self.cnt[e]))
            self.lists[e].append((waits, None, None, 0))

    def emit(self, block):
        sems = self.sems
        lists = self.lists

        def mk(ename):
            def body(e):
                for waits, fn, k, inc in lists[ename]:
                    for wk, wv in waits:
                        e.wait_ge(sems[wk], wv)
                    if fn is not None:
                        fn(e).then_inc(sems[k], inc)
            return body
        block.tensor(mk("pe"))
        block.scalar(mk("act"))
        block.vector(mk("dve"))
        block.gpsimd(mk("pool"))
        block.sync(mk("sp"))


class T:
    def __init__(self, nc, stack, name, shape, dt, psum=False):
        if psum:
            self.t = stack.enter_context(nc.psum_tensor(name, shape, dt))
        else:
            self.t = stack.enter_context(nc.sbuf_tensor(name, shape, dt))
        self.r = Res(name)

    def __getitem__(self, idx):
        return self.t[idx]


class View:
    def __init__(self, ap, r):
        self.ap = ap
        self.r = r

    def __getitem__(self, idx):
        return self.ap[idx]


ALIBI_SLOPES = [2.0 ** (-8.0 * (i + 1) / 4) for i in range(4)]
LAM_INIT = 0.8 - 0.6 * math.exp(-0.3 * 0)


def build_nc(S=2048, NB=2, stage=99):
    NT = S // 128
    NCH = S // 64
    nc = bass.Bass("TRN2", target_bir_lowering=False)

    def din(name, shape, dt=F32):
        return nc.dram_tensor(name, shape, dt, kind="ExternalInput").ap()

    x_d = din("x", [NB, S, D])
    mem_d = din("mem", [NB, MEM, D])
    g_mix_d = din("norm_mix_g", [D])
    w_in_d = din("w_in", [D, IN_W])
    conv_w_d = din("conv_w", [4, D])
    b_i_d = din("b_igate", [4])
    b_f_d = din("b_fgate", [4])
    ml_g_d = din("ml_norm_g", [512])
    lq1_d = din("lambda_q1", [64])
    lk1_d = din("lambda_k1", [64])
    lq2_d = din("lambda_q2", [64])
    lk2_d = din("lambda_k2", [64])
    da_g_d = din("da_norm_g", [512])
    w_out_d = din("w_out", [D, D])
    g_ca_d = din("norm_ca_g", [D])
    g_mem_d = din("norm_mem_g", [D])
    w_cq_d = din("w_cq", [D, D])
    w_ck_d = din("w_ck", [D, D])
    w_cv_d = din("w_cv", [D, D])
    w_co_d = din("w_co", [D, D])
    g_ffn_d = din("norm_ffn_g", [D])
    w_pq_d = din("w_pq", [D, D])
    sk_d = din("sub_keys", [8, 2, 128, 64])
    pu_d = din("peer_u", [NEXP, D])
    pv_d = din("peer_v", [NEXP, D])
    g_fin_d = din("final_norm_g", [D])
    ident_d = din("c_ident", [128, 128])
    tri_d = din("c_tri", [128, 128])
    alibi_d = din("c_alibi", [128, 64])
    iota16_d = din("c_iota16", [128, 16])
    out_d = nc.dram_tensor("out", [NB, S, D], F32, kind="ExternalOutput").ap()
    hT_d = nc.dram_tensor("hT_scr", [NB, 8, 128, S], BF16, kind="ExternalOutput" if stage == 0 else "Internal").ap()
    uvb_d = nc.dram_tensor("uvb_scr", [NEXP, 2 * D], BF16, kind="Internal").ap()
    if stage == 0:
        dbg1_d = nc.dram_tensor("dbg_x1", [NB, S, D], F32, kind="ExternalOutput").ap()
        dbg2_d = nc.dram_tensor("dbg_x2", [NB, S, D], F32, kind="ExternalOutput").ap()

    with ExitStack() as top:
        P = Prog(nc, top)
        nc_ = nc

        def mkT(st, name, shape, dt, psum=False):
            return T(nc_, st, name, shape, dt, psum)

        def rl(xs):
            return [t if isinstance(t, Res) else t.r for t in xs]

        def TT(eng, out, in0, in1, op, R, W):
            P.op(eng, lambda e: e.tensor_tensor(out=out, in0=in0, in1=in1, op=op), rl(R), rl(W))

        def TS(eng, out, in0, s1, op0, R, W, s2=None, op1=None):
            if op1 is None:
                P.op(eng, lambda e: e.tensor_scalar(out=out, in0=in0, scalar1=s1, scalar2=None, op0=op0), rl(R), rl(W))
            else:
                P.op(eng, lambda e: e.tensor_scalar(out=out, in0=in0, scalar1=s1, scalar2=s2, op0=op0, op1=op1),
                     rl(R), rl(W))

        def STT(out, in0, scalar, in1, op0, op1, R, W):
            P.op("dve", lambda e: e.scalar_tensor_tensor(out=out, in0=in0, scalar=scalar, in1=in1, op0=op0, op1=op1),
                 rl(R), rl(W))

        def ACT(out, in_, func, R, W, bias=None, scale=None, accum=None):
            kw = {}
            if bias is not None:
                kw["bias"] = bias
            if scale is not None:
                kw["scale"] = scale
            if accum is not None:
                kw["accum_out"] = accum
            P.op("act", lambda e: e.activation(out=out, in_=in_, func=func, **kw), rl(R), rl(W))

        def CP(eng, out, in_, R, W):
            if eng == "act":
                P.op("act", lambda e: e.copy(out=out, in_=in_), rl(R), rl(W))
            else:
                P.op(eng, lambda e: e.tensor_copy(out=out, in_=in_), rl(R), rl(W))

        def MM(out, lhsT, rhs, start, stop, R, W):
            P.op("pe", lambda e: e.matmul(out, lhsT=lhsT, rhs=rhs, start=start, stop=stop), rl(R), rl(W),
                 pe_accum=not start)

        def TR(out, in_, ident, R, W):
            P.op("pe", lambda e: e.transpose(out=out, in_=in_, identity=ident), rl(R), rl(W))

        def RECIP(out, in_, R, W):
            P.op("dve", lambda e: e.reciprocal(out=out, in_=in_), rl(R), rl(W))

        def MEMSET(eng, ap, val, W):
            P.op(eng, lambda e: e.memset(ap, val), [], rl(W))

        def DMA(out, in_, R, W, stream=0, queue="sp", nonc=False):
            if nonc:
                P.dma(queue, stream, lambda e: e.dma_start(out=out, in_=in_, allow_slow_non_contiguous=True),
                      rl(R), rl(W))
            else:
                P.dma(queue, stream, lambda e: e.dma_start(out=out, in_=in_), rl(R), rl(W))

        def DBG(name, ap, shape, dt, R):
            if stage != 0:
                return
            d_ = nc.dram_tensor("dbg_" + name, shape, dt, kind="ExternalOutput").ap()
            DMA(d_, ap, R, [], stream=3)

        identf = mkT(top, "identf", [128, 128], F32)
        identb = mkT(top, "identb", [128, 128], BF16)
        trif = mkT(top, "trif", [128, 128], F32)
        trib = mkT(top, "trib", [128, 128], BF16)
        onesf = mkT(top, "onesf", [64, 128], F32)
        alibi = mkT(top, "alibi", [128, 64], F32)
        iota16 = mkT(top, "iota16", [128, 16], F32)
        gT = {}
        for nm, gd in (("mix", g_mix_d), ("ca", g_ca_d), ("mem", g_mem_d), ("ffn", g_ffn_d)):
            gT[nm] = mkT(top, "gT_" + nm, [128, 8], F32)
            DMA(gT[nm][:], gd.rearrange("(c p) -> p c", p=128), [], [gT[nm]], nonc=True)
        DMA(identf[:], ident_d, [], [identf])
        DMA(trif[:], tri_d, [], [trif])
        DMA(alibi[:], alibi_d, [], [alibi])
        DMA(iota16[:], iota16_d, [], [iota16])
        CP("dve", identb[:], identf[:], [identf], [identb])
        CP("dve", trib[:], trif[:], [trif], [trib])
        MEMSET("dve", onesf[:], 1.0, [onesf])
        ptr = [mkT(top, "ptr%d" % i, [128, 1024], BF16, psum=True) for i in range(2)]
        ptf = mkT(top, "ptf", [128, 512], F32, psum=True)
        pm = [mkT(top, "pm%d" % i, [128, 512], F32, psum=True) for i in range(5)]
        pmi = [0]

        def next_pm():
            pmi[0] = (pmi[0] + 1) % 3
            return pm[pmi[0]]
        ptri = [0]

        def next_ptr():
            ptri[0] ^= 1
            return ptr[ptri[0]]

        ss = mkT(top, "ss", [128, 1], F32)
        rstd = mkT(top, "rstd", [128, 1], F32)
        sqj = mkT(top, "sqj", [128, 1024], F32)
        xs_b = mkT(top, "xs_b", [128, 1024], BF16)

        def norm_T(src, dstT_ap, dstT_res, gbc=None, keep_f32=None):
            ACT(sqj[:], src[:], AF.Square, [src], [sqj, ss], accum=ss[:])
            ACT(rstd[:], ss[:], AF.Sqrt, [ss], [rstd], scale=1.0 / D, bias=EPS)
            RECIP(rstd[:], rstd[:], [rstd], [rstd])
            TS("dve", xs_b[:], src[:], rstd[:, 0:1], ALU.mult, [src, rstd], [xs_b])
            if keep_f32 is not None:
                STT(keep_f32[:], src[:], rstd[:, 0:1], gbc[:], ALU.mult, ALU.mult, [src, rstd, gbc], [keep_f32])
            pt = next_ptr()
            for c in range(8):
                TR(pt[:, c * 128:(c + 1) * 128], xs_b[:, c * 128:(c + 1) * 128], identb[:], [xs_b, identb], [pt])
            CP("act", dstT_ap, pt[:].rearrange("p (c t) -> p c t", t=128), [pt], [dstT_res])

        def load_w(st_f32, dst_ap_fn, dst_res, w_dram, col0, ncols, gTt, stream=1):
            DMA(st_f32[:, :, 0:ncols], w_dram[:, col0:col0 + ncols].rearrange("(c p) n -> p c n", p=128),
                [], [st_f32], stream=stream)
            for c in range(8):
                if gTt is None:
                    CP("pool", dst_ap_fn(c), st_f32[:, c, 0:ncols], [st_f32], [dst_res])
                else:
                    TS("pool", dst_ap_fn(c), st_f32[:, c, 0:ncols], gTt[:, c:c + 1], ALU.mult, [st_f32, gTt], [dst_res])

        with ExitStack() as ph:
            wst = mkT(ph, "wst", [128, 8, 128], F32)
            Wh = mkT(ph, "Wh", [128, 8, 512], BF16)
            Wg8 = mkT(ph, "Wg8", [128, 8, 8], BF16)
            xnT = mkT(ph, "xnT", [128, 8, S], BF16)
            hTs = mkT(ph, "hTs", [128, S], BF16)
            xt = mkT(ph, "xt", [128, 1024], F32)
            convT = mkT(ph, "convT", [128, 8, 4], F32)
            bi_bc = mkT(ph, "bi_bc", [64, 4], F32)
            bf_bc = mkT(ph, "bf_bc", [64, 4], F32)
            mlg_bc = mkT(ph, "mlg_bc", [64, 512], F32)
            dag_bc = mkT(ph, "dag_bc", [128, 512], F32)
            lam4 = mkT(ph, "lam4", [128, 4, 64], F32)
            lamj = mkT(ph, "lamj", [128, 64], F32)
            lams = mkT(ph, "lams", [128, 4], F32)
            neglam = mkT(ph, "neglam", [128, 1], F32)
            gz = mkT(ph, "gz", [64, NCH, 8], F32)
            ig = mkT(ph, "ig", [64, NCH, 4], F32)
            lf = mkT(ph, "lf", [64, NCH, 4], F32)
            ea = mkT(ph, "ea", [64, NCH, 4], F32)
            eb = mkT(ph, "eb", [64, NCH, 4], F32)
            eFL = mkT(ph, "eFL", [128, NCH, 4], F32)
            zbuf = mkT(ph, "zbuf", [128, S + 3], F32)
            ybuf = mkT(ph, "ybuf", [128, S], F32)
            qTs = mkT(ph, "qTs", [128, S], BF16)
            kTs = mkT(ph, "kTs", [128, S], BF16)
            k_tok = mkT(ph, "k_tok", [64, NCH, 128], BF16)
            scm = mkT(ph, "scm", [64, NCH, 64], BF16)
            vea = mkT(ph, "vea", [64, NCH, 129], BF16)
            sig_o = mkT(ph, "sig_o", [64, NCH, 128], BF16)
            acc_all = mkT(ph, "acc_all", [64, NCH, 129], F32)
            hml_full = mkT(ph, "hml", [128, NCH, 128], F32)
            hml = View(hml_full[0:64], hml_full.r)
            hmlb = mkT(ph, "hmlb", [64, NCH, 128], BF16)
            C32 = mkT(ph, "C32", [128, 129], F32)
            C32s = mkT(ph, "C32s", [128, 129], F32)
            Cb = mkT(ph, "Cb", [128, 129], BF16)
            st_a = mkT(ph, "st_a", [64, NCH], F32)
            st_b = mkT(ph, "st_b", [64, NCH], F32)
            vda = mkT(ph, "vda", [128, NT, 129], BF16)
            pT = [mkT(ph, "pT%d" % i, [128, 4, 128], BF16) for i in range(2)]
            oda = mkT(ph, "oda", [128, NT, 128], F32)
            odab = mkT(ph, "odab", [128, NT, 128], BF16)
            rz = mkT(ph, "rz", [128, 2], F32)
            sd_a = mkT(ph, "sd_a", [128, NT], F32)
            sd_b = mkT(ph, "sd_b", [128, NT], F32)

            for j in range(4):
                DMA(convT[:, :, j], conv_w_d[j].rearrange("(c p) -> p c", p=128), [], [convT], nonc=True)
            DMA(bi_bc[:], b_i_d.partition_broadcast(64), [], [bi_bc])
            DMA(bf_bc[:], b_f_d.partition_broadcast(64), [], [bf_bc])
            DMA(mlg_bc[:], ml_g_d.partition_broadcast(64), [], [mlg_bc])
            DMA(dag_bc[:], da_g_d.partition_broadcast(128), [], [dag_bc])
            for i, ld in enumerate((lq1_d, lk1_d, lq2_d, lk2_d)):
                DMA(lam4[:, i, :], ld.partition_broadcast(128), [], [lam4])
            for i in range(2):
                TT("dve", lamj[:], lam4[:, 2 * i, :], lam4[:, 2 * i + 1, :], ALU.mult, [lam4], [lamj])
                P.op("dve", lambda e, i=i: e.tensor_reduce(out=lams[:, i:i + 1], in_=lamj[:], axis=AX.X, op=ALU.add),
                     rl([lamj]), rl([lams]))
            ACT(lams[:, 2:4], lams[:, 0:2], AF.Exp, [lams], [lams])
            TT("dve", neglam[:], lams[:, 3:4], lams[:, 2:3], ALU.subtract, [lams], [neglam])
            TS("dve", neglam[:], neglam[:], -LAM_INIT, ALU.add, [neglam], [neglam])
            load_w(wst, lambda c: Wg8[:, c, :], Wg8, w_in_d, 2048, 8, gT["mix"])
            MEMSET("dve", zbuf[:, 0:3], 0.0, [zbuf])
            MEMSET("dve", vda[:, :, 128:129], 1.0, [vda])

            for b in range(NB):
                for ti in range(NT):
                    DMA(xt[:], x_d[b, ti * 128:(ti + 1) * 128, :], [], [xt], stream=2)
                    norm_T(xt, xnT[:, :, ti * 128:(ti + 1) * 128], xnT)
                if b == 0:
                    DBG("xnT", xnT[:], [128, 8, S], BF16, [xnT])
                pg = pm[3]
                for c in range(NCH):
                    for k in range(8):
                        MM(pg[0:64, c * 8:(c + 1) * 8], xnT[:, k, c * 64:(c + 1) * 64], Wg8[:, k, :], k == 0, k == 7,
                           [xnT, Wg8], [pg])
                CP("act", gz[:], pg[0:64, 0:NCH * 8].rearrange("p (c g) -> p c g", g=8), [pg], [gz])
                TT("dve", ig[:], gz[:, :, 0:4], bi_bc[:, None, :].to_broadcast([64, NCH, 4]), ALU.add, [gz, bi_bc], [ig])
                TT("dve", lf[:], gz[:, :, 4:8], bf_bc[:, None, :].to_broadcast([64, NCH, 4]), ALU.add, [gz, bf_bc], [lf])
                ACT(lf[:], lf[:], AF.Exp, [lf], [lf], scale=-1.0)
                ACT(lf[:], lf[:], AF.Ln, [lf], [lf], bias=1.0)
                TS("dve", lf[:], lf[:], -1.0, ALU.mult, [lf], [lf])
                pF = pm[3]
                pFL = pm[4]
                lf2 = lf[:].rearrange("p c g -> p (c g)")
                MM(pF[0:64, 0:NCH * 4], trif[0:64, 0:64], lf2, True, True, [trif, lf], [pF])
                MM(pFL[:, 0:NCH * 4], onesf[:], lf2, True, True, [onesf, lf], [pFL])
                pF3 = pF[0:64, 0:NCH * 4].rearrange("p (c g) -> p c g", g=4)
                TT("dve", ea[:], ig[:], pF3, ALU.subtract, [ig, pF], [ea])
                ACT(ea[:], ea[:], AF.Exp, [ea], [ea])
                ACT(eb[:], pF3, AF.Exp, [pF], [eb], bias=math.log(128 ** -0.5))
                ACT(eFL[:], pFL[:, 0:NCH * 4].rearrange("p (c g) -> p c g", g=4), AF.Exp, [pFL], [eFL])

                if b == 0:
                    DBG("gz", gz[:], [64, NCH, 8], F32, [gz])
                    DBG("lf", lf[:], [64, NCH, 4], F32, [lf])
                    DBG("ea", ea[:], [64, NCH, 4], F32, [ea])
                    DBG("eb", eb[:], [64, NCH, 4], F32, [eb])
                    DBG("eFL", eFL[:], [128, NCH, 4], F32, [eFL])
                for h in range(4):
                    for jj, c0 in enumerate((h * 128, 512 + h * 128, 1024 + h * 128, 1536 + h * 128)):
                        load_w(wst, lambda c, jj=jj: Wh[:, c, jj * 128:(jj + 1) * 128], Wh, w_in_d, c0, 128, gT["mix"])
                    for j, dstq in enumerate((qTs, kTs)):
                        for tr in range(S // 512):
                            pz = next_pm()
                            for k in range(8):
                                MM(pz[:, :], Wh[:, k, j * 128:(j + 1) * 128], xnT[:, k, tr * 512:(tr + 1) * 512],
                                   k == 0, k == 7, [Wh, xnT], [pz])
                            CP("act", zbuf[:, 3 + tr * 512:3 + (tr + 1) * 512], pz[:, :], [pz], [zbuf])
                        cidx = j * 4 + h
                        TS("dve", ybuf[:], zbuf[:, 0:S], convT[:, cidx, 0:1], ALU.mult, [zbuf, convT], [ybuf])
                        for tap in range(1, 4):
                            STT(ybuf[:], zbuf[:, tap:tap + S], convT[:, cidx, tap:tap + 1], ybuf[:], ALU.mult, ALU.add,
                                [zbuf, convT, ybuf], [ybuf])
                        if b == 0 and h == 0 and j == 0:
                            DBG("zbuf", zbuf[:], [128, S + 3], F32, [zbuf])
                            DBG("ybuf", ybuf[:], [128, S], F32, [ybuf])
                            DBG("convT", convT[:], [128, 8, 4], F32, [convT])
                        ACT(dstq[:], ybuf[:], AF.Silu, [ybuf], [dstq])
                    for c in range(NCH):
                        pv = next_pm()
                        for k in range(8):
                            MM(pv[0:64, 0:256], xnT[:, k, c * 64:(c + 1) * 64], Wh[:, k, 256:512], k == 0, k == 7,
                               [xnT, Wh], [pv])
                        ACT(sig_o[:, c, :], pv[0:64, 128:256], AF.Sigmoid, [pv], [sig_o])
                        TS("dve", vea[:, c, 0:128], pv[0:64, 0:128], ea[:, c, h:h + 1], ALU.mult, [pv, ea, sig_o], [vea])
                    CP("dve", vea[:, :, 128:129], ea[:, :, h:h + 1], [ea], [vea])
                    if b == 0 and h == 0:
                        DBG("qTs", qTs[:], [128, S], BF16, [qTs])
                        DBG("kTs", kTs[:], [128, S], BF16, [kTs])
                        DBG("vea", vea[:], [64, NCH, 129], BF16, [vea])
                        DBG("sig_o", sig_o[:], [64, NCH, 128], BF16, [sig_o])
                    for c0 in range(0, NCH, 8):
                        n8 = min(8, NCH - c0)
                        pt = next_ptr()
                        for cc in range(n8):
                            c = c0 + cc
                            TR(pt[0:64, cc * 128:(cc + 1) * 128], kTs[:, c * 64:(c + 1) * 64], identb[:],
                               [kTs, identb], [pt])
                        CP("act", k_tok[:, c0:c0 + n8, :], pt[0:64, 0:n8 * 128].rearrange("p (c d) -> p c d", d=128), [pt], [k_tok])
                        ps = next_pm()
                        for cc in range(n8):
                            c = c0 + cc
                            MM(ps[0:64, cc * 64:(cc + 1) * 64], kTs[:, c * 64:(c + 1) * 64], qTs[:, c * 64:(c + 1) * 64],
                               True, True, [kTs, qTs], [ps])
                        TT("dve", scm[:, c0:c0 + n8, :], ps[0:64, 0:n8 * 64].rearrange("p (c l) -> p c l", l=64),
                           trif[0:64, None, 0:64].to_broadcast([64, n8, 64]), ALU.mult, [ps, trif], [scm])
                    for c in range(NCH):
                        pa = pm[3]
                        MM(pa[0:64, 0:129], scm[:, c, :], vea[:, c, :], True, c == 0, [scm, vea], [pa])
                        if c > 0:
                            MM(pa[0:64, 0:129], qTs[:, c * 64:(c + 1) * 64], Cb[:, :], False, True, [qTs, Cb], [pa])
                        CP("act", acc_all[:, c, :], pa[0:64, 0:129], [pa], [acc_all])
                        if c < NCH - 1:
                            pu = pm[4]
                            MM(pu[:, 0:129], k_tok[:, c, :], vea[:, c, :], True, True, [k_tok, vea], [pu])
                            if c == 0:
                                TS("dve", C32[:], pu[:, 0:129], eFL[:, c, h:h + 1], ALU.mult, [pu, eFL], [C32])
                            else:
                                TS("dve", C32s[:], C32[:], eFL[:, c, h:h + 1], ALU.mult, [C32, eFL], [C32s])
                                STT(C32[:], pu[:, 0:129], eFL[:, c, h:h + 1], C32s[:], ALU.mult, ALU.add,
                                    [pu, eFL, C32s], [C32])
                            CP("dve", Cb[:], C32[:], [C32], [Cb])
                    if b == 0 and h == 0:
                        DBG("acc_all", acc_all[:], [64, NCH, 129], F32, [acc_all])
                        DBG("scm", scm[:], [64, NCH, 64], BF16, [scm])
                    TT("dve", st_a[:], acc_all[:, :, 128], eb[:, :, h], ALU.mult, [acc_all, eb], [st_a])
                    ACT(st_a[:], st_a[:], AF.Abs, [st_a], [st_a])
                    TS("dve", st_a[:], st_a[:], 1.0, ALU.max, [st_a], [st_a])
                    RECIP(st_a[:], st_a[:], [st_a], [st_a])
                    TT("dve", st_b[:], eb[:, :, h], st_a[:], ALU.mult, [eb, st_a], [st_b])
                    TT("dve", hml[:], acc_all[:, :, 0:128], st_b[:].unsqueeze(2).to_broadcast([64, NCH, 128]), ALU.mult,
                       [acc_all, st_b], [hml])
                    TT("pool", acc_all[:, :, 0:128], hml[:], hml[:], ALU.mult, [hml], [acc_all])
                    P.op("dve", lambda e: e.tensor_reduce(out=st_a[:], in_=acc_all[:, :, 0:128], axis=AX.X, op=ALU.add),
                         rl([acc_all]), rl([st_a]))
                    ACT(st_a[:], st_a[:], AF.Sqrt, [st_a], [st_a], scale=1.0 / 128, bias=EPS)
                    RECIP(st_b[:], st_a[:], [st_a], [st_b])
                    TT("dve", hml[:], hml[:], st_b[:].unsqueeze(2).to_broadcast([64, NCH, 128]), ALU.mult, [hml, st_b], [hml])
                    TT("dve", hml[:], hml[:], mlg_bc[:, None, h * 128:(h + 1) * 128].to_broadcast([64, NCH, 128]), ALU.mult,
                       [hml, mlg_bc], [hml])
                    TT("dve", hmlb[:], hml[:], sig_o[:], ALU.mult, [hml, sig_o], [hmlb])
                    for c0 in range(0, NCH, 16):
                        n16 = min(16, NCH - c0)
                        pt = next_ptr()
                        for cc in range(n16):
                            TR(pt[:, cc * 64:(cc + 1) * 64], hmlb[:, c0 + cc, :], identb[0:64, 0:64], [hmlb, identb], [pt])
                        CP("act", hTs[:, c0 * 64:(c0 + n16) * 64], pt[:, 0:n16 * 64], [pt], [hTs])
                    DMA(hT_d[b, h], hTs[:], [hTs], [], stream=3)

                for h in range(4):
                    for jj, c0 in enumerate((2056 + h * 128, 2568 + h * 128, 3080 + h * 128)):
                        load_w(wst, lambda c, jj=jj: Wh[:, c, jj * 128:(jj + 1) * 128], Wh, w_in_d, c0, 128, gT["mix"])
                    for j, dstq in enumerate((qTs, kTs)):
                        for tr in range(S // 512):
                            pz = next_pm()
                            for k in range(8):
                                MM(pz[:, :], Wh[:, k, j * 128:(j + 1) * 128], xnT[:, k, tr * 512:(tr + 1) * 512],
                                   k == 0, k == 7, [Wh, xnT], [pz])
                            CP("act", dstq[:, tr * 512:(tr + 1) * 512], pz[:, :], [pz], [dstq])
                    for i in range(NT):
                        pv = next_pm()
                        for k in range(8):
                            MM(pv[:, 0:128], xnT[:, k, i * 128:(i + 1) * 128], Wh[:, k, 256:384], k == 0, k == 7,
                               [xnT, Wh], [pv])
                        CP("dve", vda[:, i, 0:128], pv[:, 0:128], [pv], [vda])
                    pti = 0
                    for j in range(NT):
                        pacc = (pm[3], pm[4])
                        for cm in range(2):
                            lo, hi = cm * 64, (cm + 1) * 64
                            for i0 in range(0, j + 1, 4):
                                ii_list = list(range(i0, min(i0 + 4, j + 1)))
                                ps = next_pm()
                                pTt = pT[pti]
                                pti ^= 1
                                for ii, i in enumerate(ii_list):
                                    MM(ps[:, ii * 128:(ii + 1) * 128], kTs[lo:hi, i * 128:(i + 1) * 128],
                                       qTs[lo:hi, j * 128:(j + 1) * 128], True, True, [kTs, qTs], [ps])
                                for ii, i in enumerate(ii_list):
                                    m = j - i
                                    ACT(pTt[:, ii, :], ps[:, ii * 128:(ii + 1) * 128], AF.Exp, [ps, alibi], [pTt],
                                        bias=alibi[:, h * 16 + m:h * 16 + m + 1], scale=0.125)
                                    if i == j:
                                        TT("dve", pTt[:, ii, :], pTt[:, ii, :], trib[:], ALU.mult, [pTt, trib], [pTt])
                                for ii, i in enumerate(ii_list):
                                    MM(pacc[cm][:, 0:129], pTt[:, ii, :], vda[:, i, :], i == 0, i == j, [pTt, vda], [pacc[cm]])
                        RECIP(rz[:, 0:1], pacc[0][:, 128:129], [pacc[0]], [rz])
                        RECIP(rz[:, 1:2], pacc[1][:, 128:129], [pacc[1]], [rz])
                        TT("dve", rz[:, 1:2], rz[:, 1:2], neglam[:], ALU.mult, [rz, neglam], [rz])
                        ACT(oda[:, j, :], pacc[0][:, 0:128], AF.Identity, [pacc[0], rz], [oda], scale=rz[:, 0:1])
                        STT(oda[:, j, :], pacc[1][:, 0:128], rz[:, 1:2], oda[:, j, :], ALU.mult, ALU.add,
                            [pacc[1], rz, oda], [oda])
                    TT("pool", hml_full[:, 0:NT, :], oda[:], oda[:], ALU.mult, [oda], [hml_full])
                    P.op("dve", lambda e: e.tensor_reduce(out=sd_a[:], in_=hml_full[:, 0:NT, :], axis=AX.X, op=ALU.add),
                         rl([hml_full]), rl([sd_a]))
                    ACT(sd_a[:], sd_a[:], AF.Sqrt, [sd_a], [sd_a], scale=1.0 / 128, bias=EPS)
                    RECIP(sd_b[:], sd_a[:], [sd_a], [sd_b])
                    TT("dve", oda[:], oda[:], sd_b[:].unsqueeze(2).to_broadcast([128, NT, 128]), ALU.mult, [oda, sd_b], [oda])
                    STT(odab[:], oda[:], 1.0 - LAM_INIT, dag_bc[:, None, h * 128:(h + 1) * 128].to_broadcast([128, NT, 128]),
                        ALU.mult, ALU.mult, [oda, dag_bc], [odab])
                    for j0 in range(0, NT, 8):
                        nj = min(8, NT - j0)
                        pt = next_ptr()
                        for jj in range(nj):
                            TR(pt[:, jj * 128:(jj + 1) * 128], odab[:, j0 + jj, :], identb[:], [odab, identb], [pt])
                        CP("act", hTs[:, j0 * 128:(j0 + nj) * 128], pt[:, 0:nj * 128], [pt], [hTs])
                    DMA(hT_d[b, 4 + h], hTs[:], [hTs], [], stream=3)
            P.barrier()

        with ExitStack() as ph:
            Wout = mkT(ph, "Wout", [128, 8, 1024], BF16)
            Wcq = mkT(ph, "Wcq", [128, 8, 1024], BF16)
            Wco = mkT(ph, "Wco", [128, 8, 1024], BF16)
            Wpq = mkT(ph, "Wpq", [128, 8, 1024], BF16)
            skT = mkT(ph, "skT", [128, 8, 128], BF16)
            KT = [mkT(ph, "KT%d" % b, [128, 8, MEM], BF16) for b in range(NB)]
            Vb = [mkT(ph, "Vb%d" % b, [128, 2, 4, 257], BF16) for b in range(NB)]
            gffn_bc = mkT(ph, "gffn_bc", [128, 1024], F32)
            gfin_bc = mkT(ph, "gfin_bc", [128, 1024], F32)
            DMA(gffn_bc[:], g_ffn_d.partition_broadcast(128), [], [gffn_bc])
            DMA(gfin_bc[:], g_fin_d.partition_broadcast(128), [], [gfin_bc])
            with ExitStack() as s3:
                wst = mkT(s3, "wst3", [128, 8, 512], F32)
                Wck = mkT(s3, "Wck", [128, 8, 1024], BF16)
                Wcv = mkT(s3, "Wcv", [128, 8, 1024], BF16)
                memT = mkT(s3, "memT", [128, 8, MEM], BF16)
                mt_t = mkT(s3, "mt_t", [128, 1024], F32)
                sk_nat = mkT(s3, "sk_nat", [128, 8, 128], F32)
                for (Wd, wdram, g) in ((Wout, w_out_d, None), (Wcq, w_cq_d, gT["ca"]), (Wco, w_co_d, None),
                                       (Wpq, w_pq_d, gT["ffn"]), (Wck, w_ck_d, gT["mem"]), (Wcv, w_cv_d, gT["mem"])):
                    for half in range(2):
                        load_w(wst, lambda c, Wd=Wd, half=half: Wd[:, c, half * 512:(half + 1) * 512], Wd, wdram,
                               half * 512, 512, g)
                for hh in range(8):
                    for c in range(2):
                        DMA(sk_nat[:, hh, c * 64:(c + 1) * 64], sk_d[hh, c], [], [sk_nat])
                for hh in range(8):
                    TR(ptf[:, 0:128], sk_nat[:, hh, :], identf[:], [sk_nat, identf], [ptf])
                    CP("act", skT[:, hh, :], ptf[:, 0:128], [ptf], [skT])
                cst = [mkT(s3, "cst%d" % i, [128, 2 * D], F32) for i in range(2)]
                cbf = [mkT(s3, "cbf%d" % i, [128, 2 * D], BF16) for i in range(2)]
                for rt in range(NEXP // 128):
                    cs_, cb_ = cst[rt % 2], cbf[rt % 2]
                    DMA(cs_[:, 0:D], pu_d[rt * 128:(rt + 1) * 128, :], [], [cs_])
                    DMA(cs_[:, D:2 * D], pv_d[rt * 128:(rt + 1) * 128, :], [], [cs_])
                    if rt % 2 == 0:
                        CP("dve", cb_[:], cs_[:], [cs_], [cb_])
                    else:
                        CP("act", cb_[:], cs_[:], [cs_], [cb_])
                    DMA(uvb_d[rt * 128:(rt + 1) * 128, :], cb_[:], [cb_], [])
                for b in range(NB):
                    MEMSET("dve", Vb[b][:, :, :, 256:257], 1.0, [Vb[b]])
                    for mt in range(2):
                        DMA(mt_t[:], mem_d[b, mt * 128:(mt + 1) * 128, :], [], [mt_t], stream=2)
                        norm_T(mt_t, memT[:, :, mt * 128:(mt + 1) * 128], memT)
                    for dch in range(8):
                        pk = next_pm()
                        for k in range(8):
                            MM(pk[:, 0:MEM], Wck[:, k, dch * 128:(dch + 1) * 128], memT[:, k, :], k == 0, k == 7,
                               [Wck, memT], [pk])
                        CP("act", KT[b][:, dch, :], pk[:, 0:MEM], [pk], [KT[b]])
                    for mt in range(2):
                        for half in range(2):
                            pk = next_pm()
                            for k in range(8):
                                MM(pk[:, :], memT[:, k, mt * 128:(mt + 1) * 128], Wcv[:, k, half * 512:(half + 1) * 512],
                                   k == 0, k == 7, [memT, Wcv], [pk])
                            CP("act", Vb[b][:, mt, half * 2:(half + 1) * 2, 0:256],
                               pk[:, :].rearrange("p (h d) -> p h d", d=256), [pk], [Vb[b]])
                P.barrier()

            xt = mkT(ph, "xt3", [128, 1024], F32)
            hTt = mkT(ph, "hTt", [128, 8, 128], BF16)
            x1 = mkT(ph, "x1", [128, 1024], F32)
            x1T = mkT(ph, "x1T", [128, 8, 128], BF16)
            QT = mkT(ph, "QT", [128, 8, 128], BF16)
            pTc = mkT(ph, "pTc", [128, 2, 128], BF16)
            oca = mkT(ph, "oca", [128, 1024], BF16)
            ocaT = mkT(ph, "ocaT", [128, 8, 128], BF16)
            x2 = mkT(ph, "x2", [128, 1024], F32)
            xn2 = mkT(ph, "xn2", [128, 1024], F32)
            x2T = mkT(ph, "x2T", [128, 8, 128], BF16)
            qpT = mkT(ph, "qpT", [128, 8, 128], BF16)
            sc = mkT(ph, "sc", [128, 16, 128], F32)
            work = mkT(ph, "work", [128, 256], F32)
            tv = mkT(ph, "tv", [128, 16, 16], F32)
            tiu = mkT(ph, "tiu", [128, 16, 16], U32)
            tif = mkT(ph, "tif", [128, 16, 16], F32)
            cand = mkT(ph, "cand", [128, 8, 16, 16], F32)
            bv = mkT(ph, "bv", [128, 8, 16], F32)
            bju = mkT(ph, "bju", [128, 8, 16], U32)
            k1u = mkT(ph, "k1u", [128, 8, 16], U32)
            k2u = mkT(ph, "k2u", [128, 8, 16], U32)
            k1f = mkT(ph, "k1f", [128, 8, 16], F32)
            k2f = mkT(ph, "k2f", [128, 8, 16], F32)
            oh = mkT(ph, "oh", [128, 8, 16, 16], F32)
            i1f = mkT(ph, "i1f", [128, 8, 16], F32)
            i2f = mkT(ph, "i2f", [128, 8, 16], F32)
            eidx = mkT(ph, "eidx", [128, 128], I32)
            gate = mkT(ph, "gate", [128, 8, 16], F32)
            gs = mkT(ph, "gs", [128, 8], F32)
            actv = mkT(ph, "actv", [128, 128], F32)
            coef = mkT(ph, "coef", [128, 128], F32)
            pacc_sb = mkT(ph, "pacc_sb", [128, 1024], F32)
            junk = mkT(ph, "junk", [128, 1024], F32)
            rzc = mkT(ph, "rzc", [128, 1], F32)
            NGB = 8
            gbuf = [mkT(ph, "gbuf%d" % i, [128, 2 * D], BF16) for i in range(NGB)]
            Dg = [mkT(ph, "Dg%d" % i, [128, 4, 128], BF16) for i in range(2)]
            junkb = mkT(ph, "junkb", [128, 1024], BF16)
            actg = [mkT(ph, "actg%d" % i, [128, 4], F32) for i in range(2)]
            coefg = [mkT(ph, "coefg%d" % i, [128, 4], F32) for i in range(2)]
            gbi = [0]

            for b in range(NB):
                for ti in range(NT):
                    tsl = slice(ti * 128, (ti + 1) * 128)
                    DMA(xt[:], x_d[b, tsl, :], [], [xt], stream=2)
                    DMA(hTt[:], hT_d[b, :, :, tsl].rearrange("c p t -> p c t"), [], [hTt], stream=2)
                    for half in range(2):
                        po = next_pm()
                        for c in range(8):
                            MM(po[:, :], hTt[:, c, :], Wout[:, c, half * 512:(half + 1) * 512], c == 0, c == 7,
                               [hTt, Wout], [po])
                        TT("dve", x1[:, half * 512:(half + 1) * 512], po[:, :], xt[:, half * 512:(half + 1) * 512], ALU.add,
                           [po, xt], [x1])
                    norm_T(x1, x1T[:], x1T)
                    for d0 in range(0, 8, 4):
                        pq = next_pm()
                        for dd in range(4):
                            dch = d0 + dd
                            for k in range(8):
                                MM(pq[:, dd * 128:(dd + 1) * 128], Wcq[:, k, dch * 128:(dch + 1) * 128], x1T[:, k, :],
                                   k == 0, k == 7, [Wcq, x1T], [pq])
                        CP("act", QT[:, d0:d0 + 4, :], pq[:, :].rearrange("p (c t) -> p c t", t=128), [pq], [QT])
                    for h in range(4):
                        ps = next_pm()
                        for mt in range(2):
                            for dd in range(2):
                                MM(ps[:, mt * 128:(mt + 1) * 128], KT[b][:, 2 * h + dd, mt * 128:(mt + 1) * 128],
                                   QT[:, 2 * h + dd, :], dd == 0, dd == 1, [KT[b], QT], [ps])
                        ACT(pTc[:], ps[:, 0:256].rearrange("p (m q) -> p m q", q=128), AF.Exp, [ps], [pTc], scale=1.0 / 16)
                        pa = pm[3]
                        for mt in range(2):
                            MM(pa[:, 0:257], pTc[:, mt, :], Vb[b][:, mt, h, :], mt == 0, mt == 1, [pTc, Vb[b]], [pa])
                        RECIP(rzc[:], pa[:, 256:257], [pa], [rzc])
                        ACT(oca[:, h * 256:(h + 1) * 256], pa[:, 0:256], AF.Identity, [pa, rzc], [oca], scale=rzc[:, 0:1])
                    pt = next_ptr()
                    for c in range(8):
                        TR(pt[:, c * 128:(c + 1) * 128], oca[:, c * 128:(c + 1) * 128], identb[:], [oca, identb], [pt])
                    CP("act", ocaT[:], pt[:].rearrange("p (c t) -> p c t", t=128), [pt], [ocaT])
                    for half in range(2):
                        po = next_pm()
                        for c in range(8):
                            MM(po[:, :], ocaT[:, c, :], Wco[:, c, half * 512:(half + 1) * 512], c == 0, c == 7,
                               [ocaT, Wco], [po])
                        TT("dve", x2[:, half * 512:(half + 1) * 512], po[:, :], x1[:, half * 512:(half + 1) * 512], ALU.add,
                           [po, x1], [x2])
                    if stage == 0:
                        DMA(dbg1_d[b, tsl, :], x1[:], [x1], [], stream=3)
                        DMA(dbg2_d[b, tsl, :], x2[:], [x2], [], stream=3)
                    norm_T(x2, x2T[:], x2T, gbc=gffn_bc, keep_f32=xn2)
                    for d0 in range(0, 8, 4):
                        pq = next_pm()
                        for dd in range(4):
                            dch = d0 + dd
                            for k in range(8):
                                MM(pq[:, dd * 128:(dd + 1) * 128], Wpq[:, k, dch * 128:(dch + 1) * 128], x2T[:, k, :],
                                   k == 0, k == 7, [Wpq, x2T], [pq])
                        CP("act", qpT[:, d0:d0 + 4, :], pq[:, :].rearrange("p (c t) -> p c t", t=128), [pq], [qpT])
                    for s0 in range(0, 16, 4):
                        psc = next_pm()
                        for s_ in range(4):
                            st_ = s0 + s_
                            hp, c = st_ // 2, st_ % 2
                            MM(psc[:, s_ * 128:(s_ + 1) * 128], qpT[c * 64:(c + 1) * 64, hp, :], skT[c * 64:(c + 1) * 64, hp, :],
                               True, True, [qpT, skT], [psc])
                        CP("act", sc[:, s0:s0 + 4, :], psc[:, :].rearrange("p (s n) -> p s n", n=128), [psc], [sc])
                    for st_ in range(16):
                        P.op("dve", lambda e, st_=st_: e.max(out=tv[:, st_, 0:8], in_=sc[:, st_, :]), rl([sc]), rl([tv]))
                        P.op("dve", lambda e, st_=st_: e.max_index(out=tiu[:, st_, 0:8], in_max=tv[:, st_, 0:8],
                                                                  in_values=sc[:, st_, :]), rl([sc, tv]), rl([tiu]))
                        P.op("dve", lambda e, st_=st_: e.match_replace(out=work[:, 0:128], in_to_replace=tv[:, st_, 0:8],
                                                                      in_values=sc[:, st_, :], imm_value=-1e30),
                             rl([sc, tv]), rl([work]))
                        P.op("dve", lambda e, st_=st_: e.max(out=tv[:, st_, 8:16], in_=work[:, 0:128]), rl([work]), rl([tv]))
                        P.op("dve", lambda e, st_=st_: e.max_index(out=tiu[:, st_, 8:16], in_max=tv[:, st_, 8:16],
                                                                  in_values=work[:, 0:128]), rl([work, tv]), rl([tiu]))
                    CP("dve", tif[:], tiu[:], [tiu], [tif])
                    tv4 = tv[:].rearrange("p (h c) k -> p h c k", c=2)
                    tif4 = tif[:].rearrange("p (h c) k -> p h c k", c=2)
                    TT("dve", cand[:], tv4[:, :, 0, :].unsqueeze(3).to_broadcast([128, 8, 16, 16]),
                       tv4[:, :, 1, :].unsqueeze(2).to_broadcast([128, 8, 16, 16]), ALU.add, [tv], [cand])
                    for hp in range(8):
                        cv = cand[:, hp, :, :].rearrange("p a b -> p (a b)")
                        P.op("dve", lambda e, hp=hp, cv=cv: e.max(out=bv[:, hp, 0:8], in_=cv), rl([cand]), rl([bv]))
                        P.op("dve", lambda e, hp=hp, cv=cv: e.max_index(out=bju[:, hp, 0:8], in_max=bv[:, hp, 0:8],
                                                                       in_values=cv), rl([cand, bv]), rl([bju]))
                        P.op("dve", lambda e, hp=hp, cv=cv: e.match_replace(out=work[:], in_to_replace=bv[:, hp, 0:8],
                                                                           in_values=cv, imm_value=-1e30),
                             rl([cand, bv]), rl([work]))
                        P.op("dve", lambda e, hp=hp: e.max(out=bv[:, hp, 8:16], in_=work[:]), rl([work]), rl([bv]))
                        P.op("dve", lambda e, hp=hp: e.max_index(out=bju[:, hp, 8:16], in_max=bv[:, hp, 8:16],
                                                                in_values=work[:]), rl([work, bv]), rl([bju]))
                    P.op("dve", lambda e: e.tensor_single_scalar(out=k1u[:], in_=bju[:], scalar=4,
                                                                 op=ALU.logical_shift_right), rl([bju]), rl([k1u]))
                    P.op("dve", lambda e: e.tensor_single_scalar(out=k2u[:], in_=bju[:], scalar=15,
                                                                 op=ALU.bitwise_and), rl([bju]), rl([k2u]))
                    CP("dve", k1f[:], k1u[:], [k1u], [k1f])
                    CP("dve", k2f[:], k2u[:], [k2u], [k2f])
                    for (kf, cidx, dst) in ((k1f, 0, i1f), (k2f, 1, i2f)):
                        TT("dve", oh[:], kf[:].unsqueeze(3).to_broadcast([128, 8, 16, 16]),
                           iota16[:, None, None, :].to_broadcast([128, 8, 16, 16]), ALU.is_equal, [kf, iota16], [oh])
                        TT("dve", oh[:], oh[:], tif4[:, :, cidx, :].unsqueeze(2).to_broadcast([128, 8, 16, 16]), ALU.mult,
                           [oh, tif], [oh])
                        P.op("dve", lambda e, dst=dst: e.tensor_reduce(out=dst[:], in_=oh[:], axis=AX.X, op=ALU.add),
                             rl([oh]), rl([dst]))
                    STT(i1f[:], i1f[:], 128.0, i2f[:], ALU.mult, ALU.add, [i1f, i2f], [i1f])
                    TS("dve", i1f[:], i1f[:], 0.0, ALU.max, [i1f], [i1f], s2=float(NEXP - 1), op1=ALU.min)
                    CP("dve", eidx[:], i1f[:].rearrange("p h k -> p (h k)"), [i1f], [eidx])
                    TT("dve", gate[:], bv[:], bv[:, :, 0:1].to_broadcast([128, 8, 16]), ALU.subtract, [bv], [gate])
                    ACT(gate[:], gate[:], AF.Exp, [gate], [gate])
                    P.op("dve", lambda e: e.tensor_reduce(out=gs[:], in_=gate[:], axis=AX.X, op=ALU.add), rl([gate]), rl([gs]))
                    RECIP(gs[:], gs[:], [gs], [gs])
                    TT("dve", gate[:], gate[:], gs[:].unsqueeze(2).to_broadcast([128, 8, 16]), ALU.mult, [gate, gs], [gate])
                    gate2 = gate[:].rearrange("p h k -> p (h k)")
                    pxa = (pm[3], pm[4])
                    for g4 in range(32):
                        ag, cg, dg = actg[g4 % 2], coefg[g4 % 2], Dg[g4 % 2]
                        gbs = []
                        for s_ in range(4):
                            sl = g4 * 4 + s_
                            gb = gbuf[gbi[0]]
                            gbi[0] = (gbi[0] + 1) % NGB
                            gbs.append(gb)
                            P.dma("pool", 0, lambda e, gb=gb, sl=sl: e.indirect_dma_start(
                                out=gb[:], out_offset=None, in_=uvb_d,
                                in_offset=bass.IndirectOffsetOnAxis(ap=eidx[:, sl:sl + 1], axis=0)), rl([eidx]), rl([gb]))
                        for s_ in range(4):
                            gb = gbs[s_]
                            P.op("dve", lambda e, gb=gb, s_=s_, ag=ag: e.scalar_tensor_tensor(
                                out=junkb[:], in0=gb[:, 0:D], scalar=1.0, in1=xn2[:], op0=ALU.mult, op1=ALU.mult,
                                accum_out=ag[:, s_:s_ + 1]), rl([gb, xn2]), rl([junkb, ag]))
                        ACT(cg[:], ag[:], AF.Gelu, [ag], [cg])
                        TT("dve", cg[:], cg[:], gate2[:, g4 * 4:(g4 + 1) * 4], ALU.mult, [cg, gate], [cg])
                        TT("dve", dg[:], identb[:, None, :].to_broadcast([128, 4, 128]),
                           cg[:].unsqueeze(2).to_broadcast([128, 4, 128]), ALU.mult, [identb, cg], [dg])
                        for s_ in range(4):
                            sl = g4 * 4 + s_
                            for half in range(2):
                                MM(pxa[half][:, :], dg[:, s_, :], gbs[s_][:, D + half * 512:D + (half + 1) * 512],
                                   sl == 0, sl == 127, [dg, gbs[s_]], [pxa[half]])
                    for half in range(2):
                        TT("dve", pacc_sb[:, half * 512:(half + 1) * 512], pxa[half][:, :], x2[:, half * 512:(half + 1) * 512],
                           ALU.add, [pxa[half], x2], [pacc_sb])
                    ACT(sqj[:], pacc_sb[:], AF.Square, [pacc_sb], [sqj, ss], accum=ss[:])
                    ACT(rstd[:], ss[:], AF.Sqrt, [ss], [rstd], scale=1.0 / D, bias=EPS)
                    RECIP(rstd[:], rstd[:], [rstd], [rstd])
                    STT(junk[:], pacc_sb[:], rstd[:, 0:1], gfin_bc[:], ALU.mult, ALU.mult, [pacc_sb, rstd, gfin_bc], [junk])
                    out_res = Res("out")
                    DMA(out_d[b, tsl, :], junk[:], [junk], [out_res], stream=3)
            P.barrier()

        with nc.Block() as block:
            P.emit(block)
    return nc, P


def make_consts():
    ident = np.eye(128, dtype=np.float32)
    tri = np.triu(np.ones((128, 128), np.float32))
    al = np.zeros((128, 64), np.float32)
    k = np.arange(128, dtype=np.float32)
    for h in range(4):
        for m in range(16):
            al[:, h * 16 + m] = ALIBI_SLOPES[h] * (k - 128.0 * m)
    iota16 = np.tile(np.arange(16, dtype=np.float32)[None, :], (128, 1))
    return {"c_ident": ident, "c_tri": tri, "c_alibi": al, "c_iota16": iota16}


_CACHE = {}


def kernel(**inputs):
    NC = 8
    x = np.asarray(inputs["x"], np.float32)
    B, S, _ = x.shape
    NB = B // NC
    key = (S, NB)
    if key not in _CACHE:
        _CACHE[key] = build_nc(S, NB)[0]
    nc = _CACHE[key]
    shared = {}
    for k_, v in inputs.items():
        if k_ in ("x", "mem"):
            continue
        a = np.ascontiguousarray(np.asarray(v, np.float32))
        if k_ != "final_norm_g":
            a = a[0]
        shared[k_] = np.ascontiguousarray(a)
    shared.update(make_consts())
    mem = np.asarray(inputs["mem"], np.float32)
    in_maps = []
    for c in range(NC):
        m = dict(shared)
        m["x"] = np.ascontiguousarray(x[c * NB:(c + 1) * NB])
        m["mem"] = np.ascontiguousarray(mem[c * NB:(c + 1) * NB])
        in_maps.append(m)
    res = run_bass_kernel_spmd(nc, in_maps, core_ids=list(range(NC)))
    out = np.concatenate([np.asarray(r["out"]) for r in res.results], axis=0)
    return out.astype(np.float32)
```

```python
import math
from contextlib import ExitStack
import numpy as np
import concourse.bass as bass
import concourse.mybir as mybir
from concourse.bass_utils import run_bass_kernel_spmd

F32 = mybir.dt.float32
BF16 = mybir.dt.bfloat16
I32 = mybir.dt.int32
U32 = mybir.dt.uint32
ALU = mybir.AluOpType
AF = mybir.ActivationFunctionType
AX = mybir.AxisListType

D = 1024
EPS = 1e-6
IN_W = 3592
MEM = 256
NEXP = 16384


class Res:
    __slots__ = ("name", "w", "r")

    def __init__(self, name=""):
        self.name = name
        self.w = None
        self.r = []


class Prog:
    ENGS = ("pe", "act", "dve", "pool", "sp")

    def __init__(self, nc, stack, n_dma_sems=64):
        self.nc = nc
        self.n_dma = n_dma_sems
        self.sems = {}
        self.cnt = {}
        for e in self.ENGS:
            self.sems[e] = stack.enter_context(nc.semaphore("s_" + e))
            self.cnt[e] = 0
        for i in range(n_dma_sems):
            k = "dma%d" % i
            self.sems[k] = stack.enter_context(nc.semaphore("s_" + k))
            self.cnt[k] = 0
        self.lists = {e: [] for e in self.ENGS}
        self.seen = {e: {} for e in self.ENGS}
        self.ninst = 0

    def _deps(self, eng, reads, writes, pe_accum=False):
        need = {}

        def add(dep):
            if dep is None:
                return
            k, v = dep
            if need.get(k, 0) < v:
                need[k] = v
        for r in reads:
            add(r.w)
        for w in writes:
            if not (pe_accum and w.w is not None and w.w[0] == "pe" and eng == "pe"):
                add(w.w)
            for rd in w.r:
                add(rd)
        waits = []
        seen = self.seen[eng]
        for k, v in need.items():
            if seen.get(k, 0) >= v:
                continue
            seen[k] = v
            waits.append((k, v))
        return waits

    def op(self, eng, fn, reads=(), writes=(), pe_accum=False):
        waits = self._deps(eng, reads, writes, pe_accum)
        self.cnt[eng] += 1
        v = self.cnt[eng]
        self.lists[eng].append((waits, fn, eng, 1))
        for r in reads:
            r.r.append((eng, v))
        for w in writes:
            w.w = (eng, v)
            w.r = []
        self.ninst += 1

    def dma(self, queue, stream, fn, reads=(), writes=()):
        self.rr = (getattr(self, "rr", -1) + 1) % self.n_dma
        k = "dma%d" % self.rr
        waits = self._deps(queue, reads, writes)
        if self.cnt[k] > 0 and self.seen[queue].get(k, 0) < self.cnt[k]:
            self.seen[queue][k] = self.cnt[k]
            waits.append((k, self.cnt[k]))
        self.cnt[k] += 16
        v = self.cnt[k]
        self.lists[queue].append((waits, fn, k, 16))
        for r in reads:
            r.r.append((k, v))
        for w in writes:
            w.w = (k, v)
            w.r = []
        self.ninst += 1

    def barrier(self):
        for e in self.ENGS:
            waits = []
            for k, v in self.cnt.items():
                if k == e or v == 0:
                    continue
                if self.seen[e].get(k, 0) >= v:
                    continue
                self.seen[e][k] = v
                waits.append((k, v))
            if self.cnt[e] > 0 and self.seen[e].get(e, 0) < self.cnt[e]:
                self.seen[e][e] = self.cnt[e]
                waits.append((e, self.cnt[e]))
            self.lists[e].append((waits, None, None, 0))

    def emit(self, block):
        sems = self.sems
        lists = self.lists

        def mk(ename):
            def body(e):
                for waits, fn, k, inc in lists[ename]:
                    for wk, wv in waits:
                        e.wait_ge(sems[wk], wv)
                    if fn is not None:
                        fn(e).then_inc(sems[k], inc)
            return body
        block.tensor(mk("pe"))
        block.scalar(mk("act"))
        block.vector(mk("dve"))
        block.gpsimd(mk("pool"))
        block.sync(mk("sp"))


class T:
    def __init__(self, nc, stack, name, shape, dt, psum=False):
        if psum:
            self.t = stack.enter_context(nc.psum_tensor(name, shape, dt))
        else:
            self.t = stack.enter_context(nc.sbuf_tensor(name, shape, dt))
        self.r = Res(name)

    def __getitem__(self, idx):
        return self.t[idx]


class View:
    def __init__(self, ap, r):
        self.ap = ap
        self.r = r

    def __getitem__(self, idx):
        return self.ap[idx]


ALIBI_SLOPES = [2.0 ** (-8.0 * (i + 1) / 4) for i in range(4)]
LAM_INIT = 0.8 - 0.6 * math.exp(-0.3 * 0)


import os
def build_nc(S=2048, NB=2, stage=99):
    SKIP = os.environ.get("KSKIP", "")
    NT = S // 128
    NCH = S // 64
    nc = bass.Bass("TRN2", target_bir_lowering=False)

    def din(name, shape, dt=F32):
        return nc.dram_tensor(name, shape, dt, kind="ExternalInput").ap()

    x_d = din("x", [NB, S, D])
    mem_d = din("mem", [NB, MEM, D])
    g_mix_d = din("norm_mix_g", [D])
    w_in_d = din("w_in", [D, IN_W])
    conv_w_d = din("conv_w", [4, D])
    b_i_d = din("b_igate", [4])
    b_f_d = din("b_fgate", [4])
    ml_g_d = din("ml_norm_g", [512])
    lq1_d = din("lambda_q1", [64])
    lk1_d = din("lambda_k1", [64])
    lq2_d = din("lambda_q2", [64])
    lk2_d = din("lambda_k2", [64])
    da_g_d = din("da_norm_g", [512])
    w_out_d = din("w_out", [D, D])
    g_ca_d = din("norm_ca_g", [D])
    g_mem_d = din("norm_mem_g", [D])
    w_cq_d = din("w_cq", [D, D])
    w_ck_d = din("w_ck", [D, D])
    w_cv_d = din("w_cv", [D, D])
    w_co_d = din("w_co", [D, D])
    g_ffn_d = din("norm_ffn_g", [D])
    w_pq_d = din("w_pq", [D, D])
    sk_d = din("sub_keys", [8, 2, 128, 64])
    pu_d = din("peer_u", [NEXP, D])
    pv_d = din("peer_v", [NEXP, D])
    g_fin_d = din("final_norm_g", [D])
    ident_d = din("c_ident", [128, 128])
    tri_d = din("c_tri", [128, 128])
    alibi_d = din("c_alibi", [128, 64])
    iota16_d = din("c_iota16", [128, 16])
    out_d = nc.dram_tensor("out", [NB, S, D], F32, kind="ExternalOutput").ap()
    hT_d = nc.dram_tensor("hT_scr", [NB, 8, 128, S], BF16, kind="ExternalOutput" if stage == 0 else "Internal").ap()
    uvb_d = nc.dram_tensor("uvb_scr", [NEXP, 2 * D], BF16, kind="Internal").ap()
    if stage == 0:
        dbg1_d = nc.dram_tensor("dbg_x1", [NB, S, D], F32, kind="ExternalOutput").ap()
        dbg2_d = nc.dram_tensor("dbg_x2", [NB, S, D], F32, kind="ExternalOutput").ap()

    with ExitStack() as top:
        P = Prog(nc, top)
        nc_ = nc

        def mkT(st, name, shape, dt, psum=False):
            return T(nc_, st, name, shape, dt, psum)

        def rl(xs):
            return [t if isinstance(t, Res) else t.r for t in xs]

        def TT(eng, out, in0, in1, op, R, W):
            P.op(eng, lambda e: e.tensor_tensor(out=out, in0=in0, in1=in1, op=op), rl(R), rl(W))

        def TS(eng, out, in0, s1, op0, R, W, s2=None, op1=None):
            if op1 is None:
                P.op(eng, lambda e: e.tensor_scalar(out=out, in0=in0, scalar1=s1, scalar2=None, op0=op0), rl(R), rl(W))
            else:
                P.op(eng, lambda e: e.tensor_scalar(out=out, in0=in0, scalar1=s1, scalar2=s2, op0=op0, op1=op1),
                     rl(R), rl(W))

        def STT(out, in0, scalar, in1, op0, op1, R, W):
            P.op("dve", lambda e: e.scalar_tensor_tensor(out=out, in0=in0, scalar=scalar, in1=in1, op0=op0, op1=op1),
                 rl(R), rl(W))

        def ACT(out, in_, func, R, W, bias=None, scale=None, accum=None):
            kw = {}
            if bias is not None:
                kw["bias"] = bias
            if scale is not None:
                kw["scale"] = scale
            if accum is not None:
                kw["accum_out"] = accum
            P.op("act", lambda e: e.activation(out=out, in_=in_, func=func, **kw), rl(R), rl(W))

        def CP(eng, out, in_, R, W):
            if eng == "act":
                P.op("act", lambda e: e.copy(out=out, in_=in_), rl(R), rl(W))
            else:
                P.op(eng, lambda e: e.tensor_copy(out=out, in_=in_), rl(R), rl(W))

        def MM(out, lhsT, rhs, start, stop, R, W, nowaw=False):
            P.op("pe", lambda e: e.matmul(out, lhsT=lhsT, rhs=rhs, start=start, stop=stop), rl(R), rl(W),
                 pe_accum=(not start) or nowaw)

        def TR(out, in_, ident, R, W):
            P.op("pe", lambda e: e.transpose(out=out, in_=in_, identity=ident), rl(R), rl(W))

        def RECIP(out, in_, R, W):
            P.op("dve", lambda e: e.reciprocal(out=out, in_=in_), rl(R), rl(W))

        def MEMSET(eng, ap, val, W):
            P.op(eng, lambda e: e.memset(ap, val), [], rl(W))

        def DMA(out, in_, R, W, stream=0, queue="sp", nonc=False):
            if nonc:
                P.dma(queue, stream, lambda e: e.dma_start(out=out, in_=in_, allow_slow_non_contiguous=True),
                      rl(R), rl(W))
            else:
                P.dma(queue, stream, lambda e: e.dma_start(out=out, in_=in_), rl(R), rl(W))

        def DBG(name, ap, shape, dt, R):
            if stage != 0:
                return
            d_ = nc.dram_tensor("dbg_" + name, shape, dt, kind="ExternalOutput").ap()
            DMA(d_, ap, R, [], stream=3)

        identf = mkT(top, "identf", [128, 128], F32)
        identb = mkT(top, "identb", [128, 128], BF16)
        trif = mkT(top, "trif", [128, 128], F32)
        trib = mkT(top, "trib", [128, 128], BF16)
        onesf = mkT(top, "onesf", [64, 128], F32)
        alibi = mkT(top, "alibi", [128, 64], F32)
        iota16 = mkT(top, "iota16", [128, 16], F32)
        gT = {}
        for nm, gd in (("mix", g_mix_d), ("ca", g_ca_d), ("mem", g_mem_d), ("ffn", g_ffn_d)):
            gT[nm] = mkT(top, "gT_" + nm, [128, 8], F32)
            DMA(gT[nm][:], gd.rearrange("(c p) -> p c", p=128), [], [gT[nm]], nonc=True)
        DMA(identf[:], ident_d, [], [identf])
        DMA(trif[:], tri_d, [], [trif])
        DMA(alibi[:], alibi_d, [], [alibi])
        DMA(iota16[:], iota16_d, [], [iota16])
        CP("dve", identb[:], identf[:], [identf], [identb])
        CP("dve", trib[:], trif[:], [trif], [trib])
        MEMSET("dve", onesf[:], 1.0, [onesf])
        ptr = [mkT(top, "ptr%d" % i, [128, 1024], BF16, psum=True) for i in range(2)]
        ptf = mkT(top, "ptf", [128, 512], F32, psum=True)
        pm = [mkT(top, "pm%d" % i, [128, 512], F32, psum=True) for i in range(5)]
        pmi = [0]

        def next_pm():
            pmi[0] = (pmi[0] + 1) % 3
            return pm[pmi[0]]
        ptri = [0]

        def next_ptr():
            ptri[0] ^= 1
            return ptr[ptri[0]]

        ss = mkT(top, "ss", [128, 1], F32)
        rstd = mkT(top, "rstd", [128, 1], F32)
        sqj = mkT(top, "sqj", [128, 1024], F32)
        xs_b = mkT(top, "xs_b", [128, 1024], BF16)

        def norm_T(src, dstT_ap, dstT_res, gbc=None, keep_f32=None):
            ACT(sqj[:], src[:], AF.Square, [src], [sqj, ss], accum=ss[:])
            ACT(rstd[:], ss[:], AF.Sqrt, [ss], [rstd], scale=1.0 / D, bias=EPS)
            RECIP(rstd[:], rstd[:], [rstd], [rstd])
            TS("dve", xs_b[:], src[:], rstd[:, 0:1], ALU.mult, [src, rstd], [xs_b])
            if keep_f32 is not None:
                STT(keep_f32[:], src[:], rstd[:, 0:1], gbc[:], ALU.mult, ALU.mult, [src, rstd, gbc], [keep_f32])
            pt = next_ptr()
            for c in range(8):
                TR(pt[:, c * 128:(c + 1) * 128], xs_b[:, c * 128:(c + 1) * 128], identb[:], [xs_b, identb], [pt])
            CP("act", dstT_ap, pt[:].rearrange("p (c t) -> p c t", t=128), [pt], [dstT_res])

        def load_w(st_f32, dst_ap_fn, dst_res, w_dram, col0, ncols, gTt, stream=1):
            DMA(st_f32[:, :, 0:ncols], w_dram[:, col0:col0 + ncols].rearrange("(c p) n -> p c n", p=128),
                [], [st_f32], stream=stream)
            for c in range(8):
                if gTt is None:
                    CP("pool", dst_ap_fn(c), st_f32[:, c, 0:ncols], [st_f32], [dst_res])
                else:
                    TS("pool", dst_ap_fn(c), st_f32[:, c, 0:ncols], gTt[:, c:c + 1], ALU.mult, [st_f32, gTt], [dst_res])

        with ExitStack() as ph:
            wst = mkT(ph, "wst", [128, 8, 128], F32)
            Wh = mkT(ph, "Wh", [128, 8, 512], BF16)
            Wg8 = mkT(ph, "Wg8", [128, 8, 8], BF16)
            xnT = mkT(ph, "xnT", [128, 8, S], BF16)
            hTs = mkT(ph, "hTs", [128, S], BF16)
            xt = mkT(ph, "xt", [128, 1024], F32)
            convT = mkT(ph, "convT", [128, 8, 4], F32)
            bi_bc = mkT(ph, "bi_bc", [64, 4], F32)
            bf_bc = mkT(ph, "bf_bc", [64, 4], F32)
            mlg_bc = mkT(ph, "mlg_bc", [64, 512], F32)
            dag_bc = mkT(ph, "dag_bc", [128, 512], F32)
            lam4 = mkT(ph, "lam4", [128, 4, 64], F32)
            lamj = mkT(ph, "lamj", [128, 64], F32)
            lams = mkT(ph, "lams", [128, 4], F32)
            neglam = mkT(ph, "neglam", [128, 1], F32)
            gz = mkT(ph, "gz", [64, NCH, 8], F32)
            ig = mkT(ph, "ig", [64, NCH, 4], F32)
            lf = mkT(ph, "lf", [64, NCH, 4], F32)
            ea = mkT(ph, "ea", [64, NCH, 4], F32)
            eb = mkT(ph, "eb", [64, NCH, 4], F32)
            eFL = mkT(ph, "eFL", [128, NCH, 4], F32)
            zbuf = mkT(ph, "zbuf", [128, S + 3], F32)
            ybuf = mkT(ph, "ybuf", [128, S], F32)
            qTs = mkT(ph, "qTs", [128, S], BF16)
            kTs = mkT(ph, "kTs", [128, S], BF16)
            k_tok = mkT(ph, "k_tok", [64, NCH, 128], BF16)
            scm = mkT(ph, "scm", [64, NCH, 64], BF16)
            vea = mkT(ph, "vea", [64, NCH, 129], BF16)
            sig_o = mkT(ph, "sig_o", [64, NCH, 128], BF16)
            acc_all = mkT(ph, "acc_all", [64, NCH, 129], F32)
            hml_full = mkT(ph, "hml", [128, NCH, 128], F32)
            hml = View(hml_full[0:64], hml_full.r)
            hmlb = mkT(ph, "hmlb", [64, NCH, 128], BF16)
            C32 = mkT(ph, "C32", [128, 129], F32)
            C32s = mkT(ph, "C32s", [128, 129], F32)
            Cb = mkT(ph, "Cb", [128, 129], BF16)
            st_a = mkT(ph, "st_a", [64, NCH], F32)
            st_b = mkT(ph, "st_b", [64, NCH], F32)
            vda = mkT(ph, "vda", [128, NT, 129], BF16)
            pT = [mkT(ph, "pT%d" % i, [128, 4, 128], BF16) for i in range(2)]
            oda = mkT(ph, "oda", [128, NT, 128], F32)
            odab = mkT(ph, "odab", [128, NT, 128], BF16)
            rz = mkT(ph, "rz", [128, 2], F32)
            sd_a = mkT(ph, "sd_a", [128, NT], F32)
            sd_b = mkT(ph, "sd_b", [128, NT], F32)

            for j in range(4):
                DMA(convT[:, :, j], conv_w_d[j].rearrange("(c p) -> p c", p=128), [], [convT], nonc=True)
            DMA(bi_bc[:], b_i_d.partition_broadcast(64), [], [bi_bc])
            DMA(bf_bc[:], b_f_d.partition_broadcast(64), [], [bf_bc])
            DMA(mlg_bc[:], ml_g_d.partition_broadcast(64), [], [mlg_bc])
            DMA(dag_bc[:], da_g_d.partition_broadcast(128), [], [dag_bc])
            for i, ld in enumerate((lq1_d, lk1_d, lq2_d, lk2_d)):
                DMA(lam4[:, i, :], ld.partition_broadcast(128), [], [lam4])
            for i in range(2):
                TT("dve", lamj[:], lam4[:, 2 * i, :], lam4[:, 2 * i + 1, :], ALU.mult, [lam4], [lamj])
                P.op("dve", lambda e, i=i: e.tensor_reduce(out=lams[:, i:i + 1], in_=lamj[:], axis=AX.X, op=ALU.add),
                     rl([lamj]), rl([lams]))
            ACT(lams[:, 2:4], lams[:, 0:2], AF.Exp, [lams], [lams])
            TT("dve", neglam[:], lams[:, 3:4], lams[:, 2:3], ALU.subtract, [lams], [neglam])
            TS("dve", neglam[:], neglam[:], -LAM_INIT, ALU.add, [neglam], [neglam])
            load_w(wst, lambda c: Wg8[:, c, :], Wg8, w_in_d, 2048, 8, gT["mix"])
            MEMSET("dve", zbuf[:, 0:3], 0.0, [zbuf])
            MEMSET("dve", vda[:, :, 128:129], 1.0, [vda])

            for b in range(NB):
                for ti in range(NT):
                    DMA(xt[:], x_d[b, ti * 128:(ti + 1) * 128, :], [], [xt], stream=2)
                    norm_T(xt, xnT[:, :, ti * 128:(ti + 1) * 128], xnT)
                if b == 0:
                    DBG("xnT", xnT[:], [128, 8, S], BF16, [xnT])
                pg = pm[3]
                for c in range(NCH):
                    for k in range(8):
                        MM(pg[0:64, c * 8:(c + 1) * 8], xnT[:, k, c * 64:(c + 1) * 64], Wg8[:, k, :], k == 0, k == 7,
                           [xnT, Wg8], [pg])
                CP("act", gz[:], pg[0:64, 0:NCH * 8].rearrange("p (c g) -> p c g", g=8), [pg], [gz])
                TT("dve", ig[:], gz[:, :, 0:4], bi_bc[:, None, :].to_broadcast([64, NCH, 4]), ALU.add, [gz, bi_bc], [ig])
                TT("dve", lf[:], gz[:, :, 4:8], bf_bc[:, None, :].to_broadcast([64, NCH, 4]), ALU.add, [gz, bf_bc], [lf])
                ACT(lf[:], lf[:], AF.Exp, [lf], [lf], scale=-1.0)
                ACT(lf[:], lf[:], AF.Ln, [lf], [lf], bias=1.0)
                TS("dve", lf[:], lf[:], -1.0, ALU.mult, [lf], [lf])
                pF = pm[3]
                pFL = pm[4]
                lf2 = lf[:].rearrange("p c g -> p (c g)")
                MM(pF[0:64, 0:NCH * 4], trif[0:64, 0:64], lf2, True, True, [trif, lf], [pF])
                MM(pFL[:, 0:NCH * 4], onesf[:], lf2, True, True, [onesf, lf], [pFL])
                pF3 = pF[0:64, 0:NCH * 4].rearrange("p (c g) -> p c g", g=4)
                TT("dve", ea[:], ig[:], pF3, ALU.subtract, [ig, pF], [ea])
                ACT(ea[:], ea[:], AF.Exp, [ea], [ea])
                ACT(eb[:], pF3, AF.Exp, [pF], [eb], bias=math.log(128 ** -0.5))
                ACT(eFL[:], pFL[:, 0:NCH * 4].rearrange("p (c g) -> p c g", g=4), AF.Exp, [pFL], [eFL])

                if b == 0:
                    DBG("gz", gz[:], [64, NCH, 8], F32, [gz])
                    DBG("lf", lf[:], [64, NCH, 4], F32, [lf])
                    DBG("ea", ea[:], [64, NCH, 4], F32, [ea])
                    DBG("eb", eb[:], [64, NCH, 4], F32, [eb])
                    DBG("eFL", eFL[:], [128, NCH, 4], F32, [eFL])
                for h in range(0 if "M" in SKIP else 4):
                    for jj, c0 in enumerate((h * 128, 512 + h * 128, 1024 + h * 128, 1536 + h * 128)):
                        load_w(wst, lambda c, jj=jj: Wh[:, c, jj * 128:(jj + 1) * 128], Wh, w_in_d, c0, 128, gT["mix"])
                    for j, dstq in enumerate((qTs, kTs)):
                        for tr in range(S // 512):
                            pz = next_pm()
                            for k in range(8):
                                MM(pz[:, :], Wh[:, k, j * 128:(j + 1) * 128], xnT[:, k, tr * 512:(tr + 1) * 512],
                                   k == 0, k == 7, [Wh, xnT], [pz])
                            CP("act", zbuf[:, 3 + tr * 512:3 + (tr + 1) * 512], pz[:, :], [pz], [zbuf])
                        cidx = j * 4 + h
                        TS("dve", ybuf[:], zbuf[:, 0:S], convT[:, cidx, 0:1], ALU.mult, [zbuf, convT], [ybuf])
                        for tap in range(1, 4):
                            STT(ybuf[:], zbuf[:, tap:tap + S], convT[:, cidx, tap:tap + 1], ybuf[:], ALU.mult, ALU.add,
                                [zbuf, convT, ybuf], [ybuf])
                        if b == 0 and h == 0 and j == 0:
                            DBG("zbuf", zbuf[:], [128, S + 3], F32, [zbuf])
                            DBG("ybuf", ybuf[:], [128, S], F32, [ybuf])
                            DBG("convT", convT[:], [128, 8, 4], F32, [convT])
                        ACT(dstq[:], ybuf[:], AF.Silu, [ybuf], [dstq])
                    for c in range(NCH):
                        pv = next_pm()
                        for k in range(8):
                            MM(pv[0:64, 0:256], xnT[:, k, c * 64:(c + 1) * 64], Wh[:, k, 256:512], k == 0, k == 7,
                               [xnT, Wh], [pv])
                        ACT(sig_o[:, c, :], pv[0:64, 128:256], AF.Sigmoid, [pv], [sig_o])
                        TS("dve", vea[:, c, 0:128], pv[0:64, 0:128], ea[:, c, h:h + 1], ALU.mult, [pv, ea, sig_o], [vea])
                    CP("dve", vea[:, :, 128:129], ea[:, :, h:h + 1], [ea], [vea])
                    if b == 0 and h == 0:
                        DBG("qTs", qTs[:], [128, S], BF16, [qTs])
                        DBG("kTs", kTs[:], [128, S], BF16, [kTs])
                        DBG("vea", vea[:], [64, NCH, 129], BF16, [vea])
                        DBG("sig_o", sig_o[:], [64, NCH, 128], BF16, [sig_o])
                    for c0 in range(0, NCH, 8):
                        n8 = min(8, NCH - c0)
                        pt = next_ptr()
                        for cc in range(n8):
                            c = c0 + cc
                            TR(pt[0:64, cc * 128:(cc + 1) * 128], kTs[:, c * 64:(c + 1) * 64], identb[:],
                               [kTs, identb], [pt])
                        CP("act", k_tok[:, c0:c0 + n8, :], pt[0:64, 0:n8 * 128].rearrange("p (c d) -> p c d", d=128), [pt], [k_tok])
                        ps = next_pm()
                        for cc in range(n8):
                            c = c0 + cc
                            MM(ps[0:64, cc * 64:(cc + 1) * 64], kTs[:, c * 64:(c + 1) * 64], qTs[:, c * 64:(c + 1) * 64],
                               True, True, [kTs, qTs], [ps])
                        TT("dve", scm[:, c0:c0 + n8, :], ps[0:64, 0:n8 * 64].rearrange("p (c l) -> p c l", l=64),
                           trif[0:64, None, 0:64].to_broadcast([64, n8, 64]), ALU.mult, [ps, trif], [scm])
                    for c in range(NCH):
                        pa = pm[3]
                        MM(pa[0:64, 0:129], scm[:, c, :], vea[:, c, :], True, c == 0, [scm, vea], [pa])
                        if c > 0:
                            MM(pa[0:64, 0:129], qTs[:, c * 64:(c + 1) * 64], Cb[:, :], False, True, [qTs, Cb], [pa])
                        CP("act", acc_all[:, c, :], pa[0:64, 0:129], [pa], [acc_all])
                        if c < NCH - 1:
                            pu = pm[4]
                            MM(pu[:, 0:129], k_tok[:, c, :], vea[:, c, :], True, True, [k_tok, vea], [pu])
                            if c == 0:
                                TS("dve", C32[:], pu[:, 0:129], eFL[:, c, h:h + 1], ALU.mult, [pu, eFL], [C32])
                            else:
                                TS("dve", C32s[:], C32[:], eFL[:, c, h:h + 1], ALU.mult, [C32, eFL], [C32s])
                                STT(C32[:], pu[:, 0:129], eFL[:, c, h:h + 1], C32s[:], ALU.mult, ALU.add,
                                    [pu, eFL, C32s], [C32])
                            CP("dve", Cb[:], C32[:], [C32], [Cb])
                    if b == 0 and h == 0:
                        DBG("acc_all", acc_all[:], [64, NCH, 129], F32, [acc_all])
                        DBG("scm", scm[:], [64, NCH, 64], BF16, [scm])
                    TT("dve", st_a[:], acc_all[:, :, 128], eb[:, :, h], ALU.mult, [acc_all, eb], [st_a])
                    ACT(st_a[:], st_a[:], AF.Abs, [st_a], [st_a])
                    TS("dve", st_a[:], st_a[:], 1.0, ALU.max, [st_a], [st_a])
                    RECIP(st_a[:], st_a[:], [st_a], [st_a])
                    TT("dve", st_b[:], eb[:, :, h], st_a[:], ALU.mult, [eb, st_a], [st_b])
                    TT("dve", hml[:], acc_all[:, :, 0:128], st_b[:].unsqueeze(2).to_broadcast([64, NCH, 128]), ALU.mult,
                       [acc_all, st_b], [hml])
                    TT("pool", acc_all[:, :, 0:128], hml[:], hml[:], ALU.mult, [hml], [acc_all])
                    P.op("dve", lambda e: e.tensor_reduce(out=st_a[:], in_=acc_all[:, :, 0:128], axis=AX.X, op=ALU.add),
                         rl([acc_all]), rl([st_a]))
                    ACT(st_a[:], st_a[:], AF.Sqrt, [st_a], [st_a], scale=1.0 / 128, bias=EPS)
                    RECIP(st_b[:], st_a[:], [st_a], [st_b])
                    TT("dve", hml[:], hml[:], st_b[:].unsqueeze(2).to_broadcast([64, NCH, 128]), ALU.mult, [hml, st_b], [hml])
                    TT("dve", hml[:], hml[:], mlg_bc[:, None, h * 128:(h + 1) * 128].to_broadcast([64, NCH, 128]), ALU.mult,
                       [hml, mlg_bc], [hml])
                    TT("dve", hmlb[:], hml[:], sig_o[:], ALU.mult, [hml, sig_o], [hmlb])
                    for c0 in range(0, NCH, 16):
                        n16 = min(16, NCH - c0)
                        pt = next_ptr()
                        for cc in range(n16):
                            TR(pt[:, cc * 64:(cc + 1) * 64], hmlb[:, c0 + cc, :], identb[0:64, 0:64], [hmlb, identb], [pt])
                        CP("act", hTs[:, c0 * 64:(c0 + n16) * 64], pt[:, 0:n16 * 64], [pt], [hTs])
                    DMA(hT_d[b, h], hTs[:], [hTs], [], stream=3)

                for h in range(0 if "A" in SKIP else 4):
                    for jj, c0 in enumerate((2056 + h * 128, 2568 + h * 128, 3080 + h * 128)):
                        load_w(wst, lambda c, jj=jj: Wh[:, c, jj * 128:(jj + 1) * 128], Wh, w_in_d, c0, 128, gT["mix"])
                    for j, dstq in enumerate((qTs, kTs)):
                        for tr in range(S // 512):
                            pz = next_pm()
                            for k in range(8):
                                MM(pz[:, :], Wh[:, k, j * 128:(j + 1) * 128], xnT[:, k, tr * 512:(tr + 1) * 512],
                                   k == 0, k == 7, [Wh, xnT], [pz])
                            CP("act", dstq[:, tr * 512:(tr + 1) * 512], pz[:, :], [pz], [dstq])
                    for i in range(NT):
                        pv = next_pm()
                        for k in range(8):
                            MM(pv[:, 0:128], xnT[:, k, i * 128:(i + 1) * 128], Wh[:, k, 256:384], k == 0, k == 7,
                               [xnT, Wh], [pv])
                        CP("dve", vda[:, i, 0:128], pv[:, 0:128], [pv], [vda])
                    P.barrier()
                    pacc = (pm[3], pm[4])
                    psr = [[Res("ps%d" % bk)] * 4 for bk in range(3)]
                    pTr = [[Res("pT%d_%d" % (bk, ii)) for ii in range(4)] for bk in range(2)]
                    groups = []
                    for j in range(NT):
                        for cm in range(2):
                            for i0 in range(0, j + 1, 4):
                                groups.append((j, cm, list(range(i0, min(i0 + 4, j + 1)))))

                    def emit_qk(n):
                        j, cm, iis = groups[n]
                        lo, hi = cm * 64, (cm + 1) * 64
                        bk = n % 3
                        for ii, i in enumerate(iis):
                            MM(pm[bk][:, ii * 128:(ii + 1) * 128], kTs[lo:hi, i * 128:(i + 1) * 128],
                               qTs[lo:hi, j * 128:(j + 1) * 128], True, True, [kTs, qTs], [psr[bk][ii]], nowaw=(ii > 0))

                    def emit_act(n):
                        j, cm, iis = groups[n]
                        bk, tb = n % 3, n % 2
                        for ii, i in enumerate(iis):
                            m = j - i
                            ACT(pT[tb][:, ii, :], pm[bk][:, ii * 128:(ii + 1) * 128], AF.Exp, [psr[bk][ii], alibi],
                                [pTr[tb][ii]], bias=alibi[:, h * 16 + m:h * 16 + m + 1], scale=0.125)
                            if i == j:
                                TT("dve", pT[tb][:, ii, :], pT[tb][:, ii, :], trib[:], ALU.mult, [pTr[tb][ii], trib],
                                   [pTr[tb][ii]])

                    def emit_pv(n):
                        j, cm, iis = groups[n]
                        tb = n % 2
                        for ii, i in enumerate(iis):
                            MM(pacc[cm][:, 0:129], pT[tb][:, ii, :], vda[:, i, :], i == 0, i == j, [pTr[tb][ii], vda],
                               [pacc[cm]])
                        if cm == 1 and iis[-1] == j:
                            RECIP(rz[:, 0:1], pacc[0][:, 128:129], [pacc[0]], [rz])
                            RECIP(rz[:, 1:2], pacc[1][:, 128:129], [pacc[1]], [rz])
                            TT("dve", rz[:, 1:2], rz[:, 1:2], neglam[:], ALU.mult, [rz, neglam], [rz])
                            ACT(oda[:, j, :], pacc[0][:, 0:128], AF.Identity, [pacc[0], rz], [oda], scale=rz[:, 0:1])
                            STT(oda[:, j, :], pacc[1][:, 0:128], rz[:, 1:2], oda[:, j, :], ALU.mult, ALU.add,
                                [pacc[1], rz, oda], [oda])

                    emit_qk(0)
                    for n in range(len(groups)):
                        if n + 1 < len(groups):
                            emit_qk(n + 1)
                        emit_act(n)
                        emit_pv(n)
                    P.barrier()
                    TT("pool", hml_full[:, 0:NT, :], oda[:], oda[:], ALU.mult, [oda], [hml_full])
                    P.op("dve", lambda e: e.tensor_reduce(out=sd_a[:], in_=hml_full[:, 0:NT, :], axis=AX.X, op=ALU.add),
                         rl([hml_full]), rl([sd_a]))
                    ACT(sd_a[:], sd_a[:], AF.Sqrt, [sd_a], [sd_a], scale=1.0 / 128, bias=EPS)
                    RECIP(sd_b[:], sd_a[:], [sd_a], [sd_b])
                    TT("dve", oda[:], oda[:], sd_b[:].unsqueeze(2).to_broadcast([128, NT, 128]), ALU.mult, [oda, sd_b], [oda])
                    STT(odab[:], oda[:], 1.0 - LAM_INIT, dag_bc[:, None, h * 128:(h + 1) * 128].to_broadcast([128, NT, 128]),
                        ALU.mult, ALU.mult, [oda, dag_bc], [odab])
                    for j0 in range(0, NT, 8):
                        nj = min(8, NT - j0)
                        pt = next_ptr()
                        for jj in range(nj):
                            TR(pt[:, jj * 128:(jj + 1) * 128], odab[:, j0 + jj, :], identb[:], [odab, identb], [pt])
                        CP("act", hTs[:, j0 * 128:(j0 + nj) * 128], pt[:, 0:nj * 128], [pt], [hTs])
                    DMA(hT_d[b, 4 + h], hTs[:], [hTs], [], stream=3)
            P.barrier()

        with ExitStack() as ph:
            Wout = mkT(ph, "Wout", [128, 8, 1024], BF16)
            Wcq = mkT(ph, "Wcq", [128, 8, 1024], BF16)
            Wco = mkT(ph, "Wco", [128, 8, 1024], BF16)
            Wpq = mkT(ph, "Wpq", [128, 8, 1024], BF16)
            skT = mkT(ph, "skT", [128, 8, 128], BF16)
            KT = [mkT(ph, "KT%d" % b, [128, 8, MEM], BF16) for b in range(NB)]
            Vb = [mkT(ph, "Vb%d" % b, [128, 2, 4, 257], BF16) for b in range(NB)]
            gffn_bc = mkT(ph, "gffn_bc", [128, 1024], F32)
            gfin_bc = mkT(ph, "gfin_bc", [128, 1024], F32)
            DMA(gffn_bc[:], g_ffn_d.partition_broadcast(128), [], [gffn_bc])
            DMA(gfin_bc[:], g_fin_d.partition_broadcast(128), [], [gfin_bc])
            with ExitStack() as s3:
                wst = mkT(s3, "wst3", [128, 8, 512], F32)
                Wck = mkT(s3, "Wck", [128, 8, 1024], BF16)
                Wcv = mkT(s3, "Wcv", [128, 8, 1024], BF16)
                memT = mkT(s3, "memT", [128, 8, MEM], BF16)
                mt_t = mkT(s3, "mt_t", [128, 1024], F32)
                sk_nat = mkT(s3, "sk_nat", [128, 8, 128], F32)
                for (Wd, wdram, g) in ((Wout, w_out_d, None), (Wcq, w_cq_d, gT["ca"]), (Wco, w_co_d, None),
                                       (Wpq, w_pq_d, gT["ffn"]), (Wck, w_ck_d, gT["mem"]), (Wcv, w_cv_d, gT["mem"])):
                    for half in range(2):
                        load_w(wst, lambda c, Wd=Wd, half=half: Wd[:, c, half * 512:(half + 1) * 512], Wd, wdram,
                               half * 512, 512, g)
                for hh in range(8):
                    for c in range(2):
                        DMA(sk_nat[:, hh, c * 64:(c + 1) * 64], sk_d[hh, c], [], [sk_nat])
                for hh in range(8):
                    TR(ptf[:, 0:128], sk_nat[:, hh, :], identf[:], [sk_nat, identf], [ptf])
                    CP("act", skT[:, hh, :], ptf[:, 0:128], [ptf], [skT])
                cst = [mkT(s3, "cst%d" % i, [128, 2 * D], F32) for i in range(2)]
                cbf = [mkT(s3, "cbf%d" % i, [128, 2 * D], BF16) for i in range(2)]
                for rt in range(NEXP // 128):
                    cs_, cb_ = cst[rt % 2], cbf[rt % 2]
                    DMA(cs_[:, 0:D], pu_d[rt * 128:(rt + 1) * 128, :], [], [cs_])
                    DMA(cs_[:, D:2 * D], pv_d[rt * 128:(rt + 1) * 128, :], [], [cs_])
                    if rt % 2 == 0:
                        CP("dve", cb_[:], cs_[:], [cs_], [cb_])
                    else:
                        CP("act", cb_[:], cs_[:], [cs_], [cb_])
                    DMA(uvb_d[rt * 128:(rt + 1) * 128, :], cb_[:], [cb_], [])
                for b in range(NB):
                    MEMSET("dve", Vb[b][:, :, :, 256:257], 1.0, [Vb[b]])
                    for mt in range(2):
                        DMA(mt_t[:], mem_d[b, mt * 128:(mt + 1) * 128, :], [], [mt_t], stream=2)
                        norm_T(mt_t, memT[:, :, mt * 128:(mt + 1) * 128], memT)
                    for dch in range(8):
                        pk = next_pm()
                        for k in range(8):
                            MM(pk[:, 0:MEM], Wck[:, k, dch * 128:(dch + 1) * 128], memT[:, k, :], k == 0, k == 7,
                               [Wck, memT], [pk])
                        CP("act", KT[b][:, dch, :], pk[:, 0:MEM], [pk], [KT[b]])
                    for mt in range(2):
                        for half in range(2):
                            pk = next_pm()
                            for k in range(8):
                                MM(pk[:, :], memT[:, k, mt * 128:(mt + 1) * 128], Wcv[:, k, half * 512:(half + 1) * 512],
                                   k == 0, k == 7, [memT, Wcv], [pk])
                            CP("act", Vb[b][:, mt, half * 2:(half + 1) * 2, 0:256],
                               pk[:, :].rearrange("p (h d) -> p h d", d=256), [pk], [Vb[b]])
                P.barrier()

            xt = mkT(ph, "xt3", [128, 1024], F32)
            hTt = mkT(ph, "hTt", [128, 8, 128], BF16)
            x1 = mkT(ph, "x1", [128, 1024], F32)
            x1T = mkT(ph, "x1T", [128, 8, 128], BF16)
            QT = mkT(ph, "QT", [128, 8, 128], BF16)
            pTc = mkT(ph, "pTc", [128, 2, 128], BF16)
            oca = mkT(ph, "oca", [128, 1024], BF16)
            ocaT = mkT(ph, "ocaT", [128, 8, 128], BF16)
            x2 = mkT(ph, "x2", [128, 1024], F32)
            xn2 = mkT(ph, "xn2", [128, 1024], BF16)
            x2T = mkT(ph, "x2T", [128, 8, 128], BF16)
            qpT = mkT(ph, "qpT", [128, 8, 128], BF16)
            sc = mkT(ph, "sc", [128, 16, 128], F32)
            work = mkT(ph, "work", [128, 256], F32)
            tv = mkT(ph, "tv", [128, 16, 16], F32)
            tiu = mkT(ph, "tiu", [128, 16, 16], U32)
            tif = mkT(ph, "tif", [128, 16, 16], F32)
            cand = mkT(ph, "cand", [128, 8, 16, 16], F32)
            bv = mkT(ph, "bv", [128, 8, 16], F32)
            bju = mkT(ph, "bju", [128, 8, 16], U32)
            k1u = mkT(ph, "k1u", [128, 8, 16], U32)
            k2u = mkT(ph, "k2u", [128, 8, 16], U32)
            k1f = mkT(ph, "k1f", [128, 8, 16], F32)
            k2f = mkT(ph, "k2f", [128, 8, 16], F32)
            oh = mkT(ph, "oh", [128, 8, 16, 16], F32)
            i1f = mkT(ph, "i1f", [128, 8, 16], F32)
            i2f = mkT(ph, "i2f", [128, 8, 16], F32)
            eidx = mkT(ph, "eidx", [128, 128], I32)
            gate = mkT(ph, "gate", [128, 8, 16], F32)
            gs = mkT(ph, "gs", [128, 8], F32)
            actv = mkT(ph, "actv", [128, 128], F32)
            coef = mkT(ph, "coef", [128, 128], F32)
            pacc_sb = mkT(ph, "pacc_sb", [128, 1024], F32)
            junk = mkT(ph, "junk", [128, 1024], F32)
            rzc = mkT(ph, "rzc", [128, 1], F32)
            NGB = 8
            gbuf = [mkT(ph, "gbuf%d" % i, [128, 2 * D], BF16) for i in range(NGB)]
            Dg = [mkT(ph, "Dg%d" % i, [128, 4, 128], BF16) for i in range(2)]
            junkbs = [mkT(ph, "junkb%d" % i, [128, 1024], BF16) for i in range(2)]
            actg = [mkT(ph, "actg%d" % i, [128, 4], F32) for i in range(2)]
            coefg = [mkT(ph, "coefg%d" % i, [128, 4], F32) for i in range(2)]
            gbi = [0]

            for b in range(NB):
                for ti in range(0 if "3" in SKIP else NT):
                    tsl = slice(ti * 128, (ti + 1) * 128)
                    DMA(xt[:], x_d[b, tsl, :], [], [xt], stream=2)
                    DMA(hTt[:], hT_d[b, :, :, tsl].rearrange("c p t -> p c t"), [], [hTt], stream=2)
                    for half in range(2):
                        po = next_pm()
                        for c in range(8):
                            MM(po[:, :], hTt[:, c, :], Wout[:, c, half * 512:(half + 1) * 512], c == 0, c == 7,
                               [hTt, Wout], [po])
                        TT("dve", x1[:, half * 512:(half + 1) * 512], po[:, :], xt[:, half * 512:(half + 1) * 512], ALU.add,
                           [po, xt], [x1])
                    norm_T(x1, x1T[:], x1T)
                    for d0 in range(0, 8, 4):
                        pq = next_pm()
                        for dd in range(4):
                            dch = d0 + dd
                            for k in range(8):
                                MM(pq[:, dd * 128:(dd + 1) * 128], Wcq[:, k, dch * 128:(dch + 1) * 128], x1T[:, k, :],
                                   k == 0, k == 7, [Wcq, x1T], [pq])
                        CP("act", QT[:, d0:d0 + 4, :], pq[:, :].rearrange("p (c t) -> p c t", t=128), [pq], [QT])
                    for h in range(4):
                        ps = next_pm()
                        for mt in range(2):
                            for dd in range(2):
                                MM(ps[:, mt * 128:(mt + 1) * 128], KT[b][:, 2 * h + dd, mt * 128:(mt + 1) * 128],
                                   QT[:, 2 * h + dd, :], dd == 0, dd == 1, [KT[b], QT], [ps])
                        ACT(pTc[:], ps[:, 0:256].rearrange("p (m q) -> p m q", q=128), AF.Exp, [ps], [pTc], scale=1.0 / 16)
                        pa = pm[3]
                        for mt in range(2):
                            MM(pa[:, 0:257], pTc[:, mt, :], Vb[b][:, mt, h, :], mt == 0, mt == 1, [pTc, Vb[b]], [pa])
                        RECIP(rzc[:], pa[:, 256:257], [pa], [rzc])
                        ACT(oca[:, h * 256:(h + 1) * 256], pa[:, 0:256], AF.Identity, [pa, rzc], [oca], scale=rzc[:, 0:1])
                    pt = next_ptr()
                    for c in range(8):
                        TR(pt[:, c * 128:(c + 1) * 128], oca[:, c * 128:(c + 1) * 128], identb[:], [oca, identb], [pt])
                    CP("act", ocaT[:], pt[:].rearrange("p (c t) -> p c t", t=128), [pt], [ocaT])
                    for half in range(2):
                        po = next_pm()
                        for c in range(8):
                            MM(po[:, :], ocaT[:, c, :], Wco[:, c, half * 512:(half + 1) * 512], c == 0, c == 7,
                               [ocaT, Wco], [po])
                        TT("dve", x2[:, half * 512:(half + 1) * 512], po[:, :], x1[:, half * 512:(half + 1) * 512], ALU.add,
                           [po, x1], [x2])
                    if stage == 0:
                        DMA(dbg1_d[b, tsl, :], x1[:], [x1], [], stream=3)
                        DMA(dbg2_d[b, tsl, :], x2[:], [x2], [], stream=3)
                    norm_T(x2, x2T[:], x2T, gbc=gffn_bc, keep_f32=xn2)
                    for d0 in range(0, 8, 4):
                        pq = next_pm()
                        for dd in range(4):
                            dch = d0 + dd
                            for k in range(8):
                                MM(pq[:, dd * 128:(dd + 1) * 128], Wpq[:, k, dch * 128:(dch + 1) * 128], x2T[:, k, :],
                                   k == 0, k == 7, [Wpq, x2T], [pq])
                        CP("act", qpT[:, d0:d0 + 4, :], pq[:, :].rearrange("p (c t) -> p c t", t=128), [pq], [qpT])
                    for s0 in range(0, 16, 4):
                        psc = next_pm()
                        for s_ in range(4):
                            st_ = s0 + s_
                            hp, c = st_ // 2, st_ % 2
                            MM(psc[:, s_ * 128:(s_ + 1) * 128], qpT[c * 64:(c + 1) * 64, hp, :], skT[c * 64:(c + 1) * 64, hp, :],
                               True, True, [qpT, skT], [psc])
                        CP("act", sc[:, s0:s0 + 4, :], psc[:, :].rearrange("p (s n) -> p s n", n=128), [psc], [sc])
                    for st_ in range(16):
                        P.op("dve", lambda e, st_=st_: e.max(out=tv[:, st_, 0:8], in_=sc[:, st_, :]), rl([sc]), rl([tv]))
                        P.op("dve", lambda e, st_=st_: e.max_index(out=tiu[:, st_, 0:8], in_max=tv[:, st_, 0:8],
                                                                  in_values=sc[:, st_, :]), rl([sc, tv]), rl([tiu]))
                        P.op("dve", lambda e, st_=st_: e.match_replace(out=work[:, 0:128], in_to_replace=tv[:, st_, 0:8],
                                                                      in_values=sc[:, st_, :], imm_value=-1e30),
                             rl([sc, tv]), rl([work]))
                        P.op("dve", lambda e, st_=st_: e.max(out=tv[:, st_, 8:16], in_=work[:, 0:128]), rl([work]), rl([tv]))
                        P.op("dve", lambda e, st_=st_: e.max_index(out=tiu[:, st_, 8:16], in_max=tv[:, st_, 8:16],
                                                                  in_values=work[:, 0:128]), rl([work, tv]), rl([tiu]))
                    CP("dve", tif[:], tiu[:], [tiu], [tif])
                    tv4 = tv[:].rearrange("p (h c) k -> p h c k", c=2)
                    tif4 = tif[:].rearrange("p (h c) k -> p h c k", c=2)
                    TT("dve", cand[:], tv4[:, :, 0, :].unsqueeze(3).to_broadcast([128, 8, 16, 16]),
                       tv4[:, :, 1, :].unsqueeze(2).to_broadcast([128, 8, 16, 16]), ALU.add, [tv], [cand])
                    for hp in range(8):
                        cv = cand[:, hp, :, :].rearrange("p a b -> p (a b)")
                        P.op("dve", lambda e, hp=hp, cv=cv: e.max(out=bv[:, hp, 0:8], in_=cv), rl([cand]), rl([bv]))
                        P.op("dve", lambda e, hp=hp, cv=cv: e.max_index(out=bju[:, hp, 0:8], in_max=bv[:, hp, 0:8],
                                                                       in_values=cv), rl([cand, bv]), rl([bju]))
                        P.op("dve", lambda e, hp=hp, cv=cv: e.match_replace(out=work[:], in_to_replace=bv[:, hp, 0:8],
                                                                           in_values=cv, imm_value=-1e30),
                             rl([cand, bv]), rl([work]))
                        P.op("dve", lambda e, hp=hp: e.max(out=bv[:, hp, 8:16], in_=work[:]), rl([work]), rl([bv]))
                        P.op("dve", lambda e, hp=hp: e.max_index(out=bju[:, hp, 8:16], in_max=bv[:, hp, 8:16],
                                                                in_values=work[:]), rl([work, bv]), rl([bju]))
                    P.op("dve", lambda e: e.tensor_single_scalar(out=k1u[:], in_=bju[:], scalar=4,
                                                                 op=ALU.logical_shift_right), rl([bju]), rl([k1u]))
                    P.op("dve", lambda e: e.tensor_single_scalar(out=k2u[:], in_=bju[:], scalar=15,
                                                                 op=ALU.bitwise_and), rl([bju]), rl([k2u]))
                    CP("dve", k1f[:], k1u[:], [k1u], [k1f])
                    CP("dve", k2f[:], k2u[:], [k2u], [k2f])
                    for (kf, cidx, dst) in ((k1f, 0, i1f), (k2f, 1, i2f)):
                        TT("dve", oh[:], kf[:].unsqueeze(3).to_broadcast([128, 8, 16, 16]),
                           iota16[:, None, None, :].to_broadcast([128, 8, 16, 16]), ALU.is_equal, [kf, iota16], [oh])
                        TT("dve", oh[:], oh[:], tif4[:, :, cidx, :].unsqueeze(2).to_broadcast([128, 8, 16, 16]), ALU.mult,
                           [oh, tif], [oh])
                        P.op("dve", lambda e, dst=dst: e.tensor_reduce(out=dst[:], in_=oh[:], axis=AX.X, op=ALU.add),
                             rl([oh]), rl([dst]))
                    STT(i1f[:], i1f[:], 128.0, i2f[:], ALU.mult, ALU.add, [i1f, i2f], [i1f])
                    TS("dve", i1f[:], i1f[:], 0.0, ALU.max, [i1f], [i1f], s2=float(NEXP - 1), op1=ALU.min)
                    CP("dve", eidx[:], i1f[:].rearrange("p h k -> p (h k)"), [i1f], [eidx])
                    TT("dve", gate[:], bv[:], bv[:, :, 0:1].to_broadcast([128, 8, 16]), ALU.subtract, [bv], [gate])
                    ACT(gate[:], gate[:], AF.Exp, [gate], [gate])
                    P.op("dve", lambda e: e.tensor_reduce(out=gs[:], in_=gate[:], axis=AX.X, op=ALU.add), rl([gate]), rl([gs]))
                    RECIP(gs[:], gs[:], [gs], [gs])
                    TT("dve", gate[:], gate[:], gs[:].unsqueeze(2).to_broadcast([128, 8, 16]), ALU.mult, [gate, gs], [gate])
                    gate2 = gate[:].rearrange("p h k -> p (h k)")
                    pxa = (pm[3], pm[4])
                    for g4 in range(0 if "G" in SKIP else 32):
                        ag, cg, dg = actg[g4 % 2], coefg[g4 % 2], Dg[g4 % 2]
                        gbs = []
                        for s_ in range(4):
                            sl = g4 * 4 + s_
                            gb = gbuf[gbi[0]]
                            gbi[0] = (gbi[0] + 1) % NGB
                            gbs.append(gb)
                            P.dma("pool", 0, lambda e, gb=gb, sl=sl: e.indirect_dma_start(
                                out=gb[:], out_offset=None, in_=uvb_d,
                                in_offset=bass.IndirectOffsetOnAxis(ap=eidx[:, sl:sl + 1], axis=0)), rl([eidx]), rl([gb]))
                        for s_ in range(4):
                            gb = gbs[s_]
                            junkb = junkbs[s_ % 2]
                            P.op("dve", lambda e, gb=gb, s_=s_, ag=ag, junkb=junkb: e.scalar_tensor_tensor(
                                out=junkb[:], in0=gb[:, 0:D], scalar=1.0, in1=xn2[:], op0=ALU.mult, op1=ALU.mult,
                                accum_out=ag[:, s_:s_ + 1]), rl([gb, xn2]), rl([junkb, ag]))
                        ACT(cg[:], ag[:], AF.Gelu, [ag], [cg])
                        TT("dve", cg[:], cg[:], gate2[:, g4 * 4:(g4 + 1) * 4], ALU.mult, [cg, gate], [cg])
                        TT("dve", dg[:], identb[:, None, :].to_broadcast([128, 4, 128]),
                           cg[:].unsqueeze(2).to_broadcast([128, 4, 128]), ALU.mult, [identb, cg], [dg])
                        for s_ in range(4):
                            sl = g4 * 4 + s_
                            for half in range(2):
                                MM(pxa[half][:, :], dg[:, s_, :], gbs[s_][:, D + half * 512:D + (half + 1) * 512],
                                   sl == 0, sl == 127, [dg, gbs[s_]], [pxa[half]])
                    for half in range(2):
                        TT("dve", pacc_sb[:, half * 512:(half + 1) * 512], pxa[half][:, :], x2[:, half * 512:(half + 1) * 512],
                           ALU.add, [pxa[half], x2], [pacc_sb])
                    ACT(sqj[:], pacc_sb[:], AF.Square, [pacc_sb], [sqj, ss], accum=ss[:])
                    ACT(rstd[:], ss[:], AF.Sqrt, [ss], [rstd], scale=1.0 / D, bias=EPS)
                    RECIP(rstd[:], rstd[:], [rstd], [rstd])
                    STT(junk[:], pacc_sb[:], rstd[:, 0:1], gfin_bc[:], ALU.mult, ALU.mult, [pacc_sb, rstd, gfin_bc], [junk])
                    out_res = Res("out")
                    DMA(out_d[b, tsl, :], junk[:], [junk], [out_res], stream=3)
            P.barrier()

        with nc.Block() as block:
            P.emit(block)
    return nc, P


def make_consts():
    ident = np.eye(128, dtype=np.float32)
    tri = np.triu(np.ones((128, 128), np.float32))
    al = np.zeros((128, 64), np.float32)
    k = np.arange(128, dtype=np.float32)
    for h in range(4):
        for m in range(16):
            al[:, h * 16 + m] = ALIBI_SLOPES[h] * (k - 128.0 * m)
    iota16 = np.tile(np.arange(16, dtype=np.float32)[None, :], (128, 1))
    return {"c_ident": ident, "c_tri": tri, "c_alibi": al, "c_iota16": iota16}


_CACHE = {}


def kernel(**inputs):
    NC = 8
    x = np.asarray(inputs["x"], np.float32)
    B, S, _ = x.shape
    NB = B // NC
    key = (S, NB)
    if key not in _CACHE:
        _CACHE[key] = build_nc(S, NB)[0]
    nc = _CACHE[key]
    shared = {}
    for k_, v in inputs.items():
        if k_ in ("x", "mem"):
            continue
        a = np.ascontiguousarray(np.asarray(v, np.float32))
        if k_ != "final_norm_g":
            a = a[0]
        shared[k_] = np.ascontiguousarray(a)
    shared.update(make_consts())
    mem = np.asarray(inputs["mem"], np.float32)
    in_maps = []
    for c in range(NC):
        m = dict(shared)
        m["x"] = np.ascontiguousarray(x[c * NB:(c + 1) * NB])
        m["mem"] = np.ascontiguousarray(mem[c * NB:(c + 1) * NB])
        in_maps.append(m)
    res = run_bass_kernel_spmd(nc, in_maps, core_ids=list(range(NC)))
    out = np.concatenate([np.asarray(r["out"]) for r in res.results], axis=0)
    return out.astype(np.float32)
```

```python
import math
from contextlib import ExitStack
import numpy as np
import concourse.bass as bass
import concourse.mybir as mybir
from concourse.bass_utils import run_bass_kernel_spmd

F32 = mybir.dt.float32
BF16 = mybir.dt.bfloat16
I32 = mybir.dt.int32
U32 = mybir.dt.uint32
ALU = mybir.AluOpType
AF = mybir.ActivationFunctionType
AX = mybir.AxisListType

D = 1024
EPS = 1e-6
IN_W = 3592
MEM = 256
NEXP = 16384


class Res:
    __slots__ = ("name", "w", "r")

    def __init__(self, name=""):
        self.name = name
        self.w = None
        self.r = []


class Prog:
    ENGS = ("pe", "act", "dve", "pool", "sp")

    def __init__(self, nc, stack, n_dma_sems=64):
        self.nc = nc
        self.n_dma = n_dma_sems
        self.sems = {}
        self.cnt = {}
        for e in self.ENGS:
            self.sems[e] = stack.enter_context(nc.semaphore("s_" + e))
            self.cnt[e] = 0
        for i in range(n_dma_sems):
            k = "dma%d" % i
            self.sems[k] = stack.enter_context(nc.semaphore("s_" + k))
            self.cnt[k] = 0
        self.lists = {e: [] for e in self.ENGS}
        self.seen = {e: {} for e in self.ENGS}
        self.ninst = 0

    def _deps(self, eng, reads, writes, pe_accum=False):
        need = {}

        def add(dep):
            if dep is None:
                return
            k, v = dep
            if need.get(k, 0) < v:
                need[k] = v
        for r in reads:
            add(r.w)
        for w in writes:
            if not (pe_accum and w.w is not None and w.w[0] == "pe" and eng == "pe"):
                add(w.w)
            for rd in w.r:
                add(rd)
        waits = []
        seen = self.seen[eng]
        for k, v in need.items():
            if seen.get(k, 0) >= v:
                continue
            seen[k] = v
            waits.append((k, v))
        return waits

    def op(self, eng, fn, reads=(), writes=(), pe_accum=False):
        waits = self._deps(eng, reads, writes, pe_accum)
        self.cnt[eng] += 1
        v = self.cnt[eng]
        self.lists[eng].append((waits, fn, eng, 1))
        for r in reads:
            r.r.append((eng, v))
        for w in writes:
            w.w = (eng, v)
            w.r = []
        self.ninst += 1

    def dma(self, queue, stream, fn, reads=(), writes=()):
        self.rr = (getattr(self, "rr", -1) + 1) % self.n_dma
        k = "dma%d" % self.rr
        waits = self._deps(queue, reads, writes)
        if self.cnt[k] > 0 and self.seen[queue].get(k, 0) < self.cnt[k]:
            self.seen[queue][k] = self.cnt[k]
            waits.append((k, self.cnt[k]))
        self.cnt[k] += 16
        v = self.cnt[k]
        self.lists[queue].append((waits, fn, k, 16))
        for r in reads:
            r.r.append((k, v))
        for w in writes:
            w.w = (k, v)
            w.r = []
        self.ninst += 1

    def barrier(self):
        for e in self.ENGS:
            waits = []
            for k, v in self.cnt.items():
                if k == e or v == 0:
                    continue
                if self.seen[e].get(k, 0) >= v:
                    continue
                self.seen[e][k] = v
                waits.append((k, v))
            if self.cnt[e] > 0 and self.seen[e].get(e, 0) < self.cnt[e]:
                self.seen[e][e] = self.cnt[e]
                waits.append((e, self.cnt[e]))
            self.lists[e].append((waits, None, None, 0))

    def emit(self, block):
        sems = self.sems
        lists = self.lists

        def mk(ename):
            def body(e):
                for waits, fn, k, inc in lists[ename]:
                    for wk, wv in waits:
                        e.wait_ge(sems[wk], wv)
                    if fn is not None:
                        fn(e).then_inc(sems[k], inc)
            return body
        block.tensor(mk("pe"))
        block.scalar(mk("act"))
        block.vector(mk("dve"))
        block.gpsimd(mk("pool"))
        block.sync(mk("sp"))


class T:
    def __init__(self, nc, stack, name, shape, dt, psum=False):
        if psum:
            self.t = stack.enter_context(nc.psum_tensor(name, shape, dt))
        else:
            self.t = stack.enter_context(nc.sbuf_tensor(name, shape, dt))
        self.r = Res(name)

    def __getitem__(self, idx):
        return self.t[idx]


class View:
    def __init__(self, ap, r):
        self.ap = ap
        self.r = r

    def __getitem__(self, idx):
        return self.ap[idx]


ALIBI_SLOPES = [2.0 ** (-8.0 * (i + 1) / 4) for i in range(4)]
LAM_INIT = 0.8 - 0.6 * math.exp(-0.3 * 0)


import os
def build_nc(S=2048, NB=2, stage=99):
    SKIP = os.environ.get("KSKIP", "")
    NT = S // 128
    NCH = S // 64
    nc = bass.Bass("TRN2", target_bir_lowering=False)

    def din(name, shape, dt=F32):
        return nc.dram_tensor(name, shape, dt, kind="ExternalInput").ap()

    x_d = din("x", [NB, S, D])
    mem_d = din("mem", [NB, MEM, D])
    g_mix_d = din("norm_mix_g", [D])
    w_in_d = din("w_in", [D, IN_W])
    conv_w_d = din("conv_w", [4, D])
    b_i_d = din("b_igate", [4])
    b_f_d = din("b_fgate", [4])
    ml_g_d = din("ml_norm_g", [512])
    lq1_d = din("lambda_q1", [64])
    lk1_d = din("lambda_k1", [64])
    lq2_d = din("lambda_q2", [64])
    lk2_d = din("lambda_k2", [64])
    da_g_d = din("da_norm_g", [512])
    w_out_d = din("w_out", [D, D])
    g_ca_d = din("norm_ca_g", [D])
    g_mem_d = din("norm_mem_g", [D])
    w_cq_d = din("w_cq", [D, D])
    w_ck_d = din("w_ck", [D, D])
    w_cv_d = din("w_cv", [D, D])
    w_co_d = din("w_co", [D, D])
    g_ffn_d = din("norm_ffn_g", [D])
    w_pq_d = din("w_pq", [D, D])
    sk_d = din("sub_keys", [8, 2, 128, 64])
    pu_d = din("peer_u", [NEXP, D])
    pv_d = din("peer_v", [NEXP, D])
    g_fin_d = din("final_norm_g", [D])
    ident_d = din("c_ident", [128, 128])
    tri_d = din("c_tri", [128, 128])
    alibi_d = din("c_alibi", [128, 64])
    iota16_d = din("c_iota16", [128, 16])
    out_d = nc.dram_tensor("out", [NB, S, D], F32, kind="ExternalOutput").ap()
    hT_d = nc.dram_tensor("hT_scr", [NB, 8, 128, S], BF16, kind="ExternalOutput" if stage == 0 else "Internal").ap()
    uvb_d = nc.dram_tensor("uvb_scr", [NEXP, 2 * D], BF16, kind="Internal").ap()
    if stage == 0:
        dbg1_d = nc.dram_tensor("dbg_x1", [NB, S, D], F32, kind="ExternalOutput").ap()
        dbg2_d = nc.dram_tensor("dbg_x2", [NB, S, D], F32, kind="ExternalOutput").ap()

    with ExitStack() as top:
        P = Prog(nc, top)
        nc_ = nc

        def mkT(st, name, shape, dt, psum=False):
            return T(nc_, st, name, shape, dt, psum)

        def rl(xs):
            return [t if isinstance(t, Res) else t.r for t in xs]

        def TT(eng, out, in0, in1, op, R, W):
            P.op(eng, lambda e: e.tensor_tensor(out=out, in0=in0, in1=in1, op=op), rl(R), rl(W))

        def TS(eng, out, in0, s1, op0, R, W, s2=None, op1=None):
            if op1 is None:
                P.op(eng, lambda e: e.tensor_scalar(out=out, in0=in0, scalar1=s1, scalar2=None, op0=op0), rl(R), rl(W))
            else:
                P.op(eng, lambda e: e.tensor_scalar(out=out, in0=in0, scalar1=s1, scalar2=s2, op0=op0, op1=op1),
                     rl(R), rl(W))

        def STT(out, in0, scalar, in1, op0, op1, R, W):
            P.op("dve", lambda e: e.scalar_tensor_tensor(out=out, in0=in0, scalar=scalar, in1=in1, op0=op0, op1=op1),
                 rl(R), rl(W))

        def ACT(out, in_, func, R, W, bias=None, scale=None, accum=None):
            kw = {}
            if bias is not None:
                kw["bias"] = bias
            if scale is not None:
                kw["scale"] = scale
            if accum is not None:
                kw["accum_out"] = accum
            P.op("act", lambda e: e.activation(out=out, in_=in_, func=func, **kw), rl(R), rl(W))

        def CP(eng, out, in_, R, W):
            if eng == "act":
                P.op("act", lambda e: e.copy(out=out, in_=in_), rl(R), rl(W))
            else:
                P.op(eng, lambda e: e.tensor_copy(out=out, in_=in_), rl(R), rl(W))

        def MM(out, lhsT, rhs, start, stop, R, W, nowaw=False):
            P.op("pe", lambda e: e.matmul(out, lhsT=lhsT, rhs=rhs, start=start, stop=stop), rl(R), rl(W),
                 pe_accum=(not start) or nowaw)

        def TR(out, in_, ident, R, W):
            P.op("pe", lambda e: e.transpose(out=out, in_=in_, identity=ident), rl(R), rl(W))

        def RECIP(out, in_, R, W):
            P.op("dve", lambda e: e.reciprocal(out=out, in_=in_), rl(R), rl(W))

        def MEMSET(eng, ap, val, W):
            P.op(eng, lambda e: e.memset(ap, val), [], rl(W))

        def DMA(out, in_, R, W, stream=0, queue="sp", nonc=False):
            if nonc:
                P.dma(queue, stream, lambda e: e.dma_start(out=out, in_=in_, allow_slow_non_contiguous=True),
                      rl(R), rl(W))
            else:
                P.dma(queue, stream, lambda e: e.dma_start(out=out, in_=in_), rl(R), rl(W))

        def DBG(name, ap, shape, dt, R):
            if stage != 0:
                return
            d_ = nc.dram_tensor("dbg_" + name, shape, dt, kind="ExternalOutput").ap()
            DMA(d_, ap, R, [], stream=3)

        identf = mkT(top, "identf", [128, 128], F32)
        identb = mkT(top, "identb", [128, 128], BF16)
        trif = mkT(top, "trif", [128, 128], F32)
        trib = mkT(top, "trib", [128, 128], BF16)
        onesf = mkT(top, "onesf", [64, 128], F32)
        alibi = mkT(top, "alibi", [128, 64], F32)
        iota16 = mkT(top, "iota16", [128, 16], F32)
        gT = {}
        for nm, gd in (("mix", g_mix_d), ("ca", g_ca_d), ("mem", g_mem_d), ("ffn", g_ffn_d)):
            gT[nm] = mkT(top, "gT_" + nm, [128, 8], F32)
            DMA(gT[nm][:], gd.rearrange("(c p) -> p c", p=128), [], [gT[nm]], nonc=True)
        DMA(identf[:], ident_d, [], [identf])
        DMA(trif[:], tri_d, [], [trif])
        DMA(alibi[:], alibi_d, [], [alibi])
        DMA(iota16[:], iota16_d, [], [iota16])
        CP("dve", identb[:], identf[:], [identf], [identb])
        CP("dve", trib[:], trif[:], [trif], [trib])
        MEMSET("dve", onesf[:], 1.0, [onesf])
        ptr = [mkT(top, "ptr%d" % i, [128, 1024], BF16, psum=True) for i in range(2)]
        ptf = mkT(top, "ptf", [128, 512], F32, psum=True)
        pm = [mkT(top, "pm%d" % i, [128, 512], F32, psum=True) for i in range(5)]
        pmi = [0]

        def next_pm():
            pmi[0] = (pmi[0] + 1) % 3
            return pm[pmi[0]]
        ptri = [0]

        def next_ptr():
            ptri[0] ^= 1
            return ptr[ptri[0]]

        ss = mkT(top, "ss", [128, 1], F32)
        rstd = mkT(top, "rstd", [128, 1], F32)
        sqj = mkT(top, "sqj", [128, 1024], F32)
        xs_b = mkT(top, "xs_b", [128, 1024], BF16)

        def norm_T(src, dstT_ap, dstT_res, gbc=None, keep_f32=None):
            ACT(sqj[:], src[:], AF.Square, [src], [sqj, ss], accum=ss[:])
            ACT(rstd[:], ss[:], AF.Sqrt, [ss], [rstd], scale=1.0 / D, bias=EPS)
            RECIP(rstd[:], rstd[:], [rstd], [rstd])
            TS("dve", xs_b[:], src[:], rstd[:, 0:1], ALU.mult, [src, rstd], [xs_b])
            if keep_f32 is not None:
                STT(keep_f32[:], src[:], rstd[:, 0:1], gbc[:], ALU.mult, ALU.mult, [src, rstd, gbc], [keep_f32])
            pt = next_ptr()
            for c in range(8):
                TR(pt[:, c * 128:(c + 1) * 128], xs_b[:, c * 128:(c + 1) * 128], identb[:], [xs_b, identb], [pt])
            CP("act", dstT_ap, pt[:].rearrange("p (c t) -> p c t", t=128), [pt], [dstT_res])

        def load_w(st_f32, dst_ap_fn, dst_res, w_dram, col0, ncols, gTt, stream=1):
            DMA(st_f32[:, :, 0:ncols], w_dram[:, col0:col0 + ncols].rearrange("(c p) n -> p c n", p=128),
                [], [st_f32], stream=stream)
            for c in range(8):
                if gTt is None:
                    CP("pool", dst_ap_fn(c), st_f32[:, c, 0:ncols], [st_f32], [dst_res])
                else:
                    TS("pool", dst_ap_fn(c), st_f32[:, c, 0:ncols], gTt[:, c:c + 1], ALU.mult, [st_f32, gTt], [dst_res])

        with ExitStack() as ph:
            wst = mkT(ph, "wst", [128, 8, 128], F32)
            Wh = mkT(ph, "Wh", [128, 8, 512], BF16)
            Wg8 = mkT(ph, "Wg8", [128, 8, 8], BF16)
            xnT = mkT(ph, "xnT", [128, 8, S], BF16)
            hTs = mkT(ph, "hTs", [128, S], BF16)
            xt = mkT(ph, "xt", [128, 1024], F32)
            convT = mkT(ph, "convT", [128, 8, 4], F32)
            bi_bc = mkT(ph, "bi_bc", [64, 4], F32)
            bf_bc = mkT(ph, "bf_bc", [64, 4], F32)
            mlg_bc = mkT(ph, "mlg_bc", [64, 512], F32)
            dag_bc = mkT(ph, "dag_bc", [128, 512], F32)
            lam4 = mkT(ph, "lam4", [128, 4, 64], F32)
            lamj = mkT(ph, "lamj", [128, 64], F32)
            lams = mkT(ph, "lams", [128, 4], F32)
            neglam = mkT(ph, "neglam", [128, 1], F32)
            gz = mkT(ph, "gz", [64, NCH, 8], F32)
            ig = mkT(ph, "ig", [64, NCH, 4], F32)
            lf = mkT(ph, "lf", [64, NCH, 4], F32)
            ea = mkT(ph, "ea", [64, NCH, 4], F32)
            eb = mkT(ph, "eb", [64, NCH, 4], F32)
            eFL = mkT(ph, "eFL", [128, NCH, 4], F32)
            zbuf = mkT(ph, "zbuf", [128, S + 3], F32)
            ybuf = mkT(ph, "ybuf", [128, S], F32)
            qTs = mkT(ph, "qTs", [128, S], BF16)
            kTs = mkT(ph, "kTs", [128, S], BF16)
            k_tok = mkT(ph, "k_tok", [64, NCH, 128], BF16)
            scm = mkT(ph, "scm", [64, NCH, 64], BF16)
            vea = mkT(ph, "vea", [64, NCH, 129], BF16)
            sig_o = mkT(ph, "sig_o", [64, NCH, 128], BF16)
            acc_all = mkT(ph, "acc_all", [64, NCH, 129], F32)
            hml_full = mkT(ph, "hml", [128, NCH, 128], F32)
            hml = View(hml_full[0:64], hml_full.r)
            hmlb = mkT(ph, "hmlb", [64, NCH, 128], BF16)
            C32 = mkT(ph, "C32", [128, 129], F32)
            C32s = mkT(ph, "C32s", [128, 129], F32)
            Cb = mkT(ph, "Cb", [128, 129], BF16)
            st_a = mkT(ph, "st_a", [64, NCH], F32)
            st_b = mkT(ph, "st_b", [64, NCH], F32)
            vda = mkT(ph, "vda", [128, NT, 129], BF16)
            pT = [mkT(ph, "pT%d" % i, [128, 4, 128], BF16) for i in range(2)]
            oda = mkT(ph, "oda", [128, NT, 128], F32)
            odab = mkT(ph, "odab", [128, NT, 128], BF16)
            rz = mkT(ph, "rz", [128, 2], F32)
            sd_a = mkT(ph, "sd_a", [128, NT], F32)
            sd_b = mkT(ph, "sd_b", [128, NT], F32)

            for j in range(4):
                DMA(convT[:, :, j], conv_w_d[j].rearrange("(c p) -> p c", p=128), [], [convT], nonc=True)
            DMA(bi_bc[:], b_i_d.partition_broadcast(64), [], [bi_bc])
            DMA(bf_bc[:], b_f_d.partition_broadcast(64), [], [bf_bc])
            DMA(mlg_bc[:], ml_g_d.partition_broadcast(64), [], [mlg_bc])
            DMA(dag_bc[:], da_g_d.partition_broadcast(128), [], [dag_bc])
            for i, ld in enumerate((lq1_d, lk1_d, lq2_d, lk2_d)):
                DMA(lam4[:, i, :], ld.partition_broadcast(128), [], [lam4])
            for i in range(2):
                TT("dve", lamj[:], lam4[:, 2 * i, :], lam4[:, 2 * i + 1, :], ALU.mult, [lam4], [lamj])
                P.op("dve", lambda e, i=i: e.tensor_reduce(out=lams[:, i:i + 1], in_=lamj[:], axis=AX.X, op=ALU.add),
                     rl([lamj]), rl([lams]))
            ACT(lams[:, 2:4], lams[:, 0:2], AF.Exp, [lams], [lams])
            TT("dve", neglam[:], lams[:, 3:4], lams[:, 2:3], ALU.subtract, [lams], [neglam])
            TS("dve", neglam[:], neglam[:], -LAM_INIT, ALU.add, [neglam], [neglam])
            load_w(wst, lambda c: Wg8[:, c, :], Wg8, w_in_d, 2048, 8, gT["mix"])
            MEMSET("dve", zbuf[:, 0:3], 0.0, [zbuf])
            MEMSET("dve", vda[:, :, 128:129], 1.0, [vda])

            for b in range(NB):
                for ti in range(NT):
                    DMA(xt[:], x_d[b, ti * 128:(ti + 1) * 128, :], [], [xt], stream=2)
                    norm_T(xt, xnT[:, :, ti * 128:(ti + 1) * 128], xnT)
                if b == 0:
                    DBG("xnT", xnT[:], [128, 8, S], BF16, [xnT])
                pg = pm[3]
                for c in range(NCH):
                    for k in range(8):
                        MM(pg[0:64, c * 8:(c + 1) * 8], xnT[:, k, c * 64:(c + 1) * 64], Wg8[:, k, :], k == 0, k == 7,
                           [xnT, Wg8], [pg])
                CP("act", gz[:], pg[0:64, 0:NCH * 8].rearrange("p (c g) -> p c g", g=8), [pg], [gz])
                TT("dve", ig[:], gz[:, :, 0:4], bi_bc[:, None, :].to_broadcast([64, NCH, 4]), ALU.add, [gz, bi_bc], [ig])
                TT("dve", lf[:], gz[:, :, 4:8], bf_bc[:, None, :].to_broadcast([64, NCH, 4]), ALU.add, [gz, bf_bc], [lf])
                ACT(lf[:], lf[:], AF.Exp, [lf], [lf], scale=-1.0)
                ACT(lf[:], lf[:], AF.Ln, [lf], [lf], bias=1.0)
                TS("dve", lf[:], lf[:], -1.0, ALU.mult, [lf], [lf])
                pF = pm[3]
                pFL = pm[4]
                lf2 = lf[:].rearrange("p c g -> p (c g)")
                MM(pF[0:64, 0:NCH * 4], trif[0:64, 0:64], lf2, True, True, [trif, lf], [pF])
                MM(pFL[:, 0:NCH * 4], onesf[:], lf2, True, True, [onesf, lf], [pFL])
                pF3 = pF[0:64, 0:NCH * 4].rearrange("p (c g) -> p c g", g=4)
                TT("dve", ea[:], ig[:], pF3, ALU.subtract, [ig, pF], [ea])
                ACT(ea[:], ea[:], AF.Exp, [ea], [ea])
                ACT(eb[:], pF3, AF.Exp, [pF], [eb], bias=math.log(128 ** -0.5))
                ACT(eFL[:], pFL[:, 0:NCH * 4].rearrange("p (c g) -> p c g", g=4), AF.Exp, [pFL], [eFL])

                if b == 0:
                    DBG("gz", gz[:], [64, NCH, 8], F32, [gz])
                    DBG("lf", lf[:], [64, NCH, 4], F32, [lf])
                    DBG("ea", ea[:], [64, NCH, 4], F32, [ea])
                    DBG("eb", eb[:], [64, NCH, 4], F32, [eb])
                    DBG("eFL", eFL[:], [128, NCH, 4], F32, [eFL])
                for h in range(0 if "M" in SKIP else 4):
                    for jj, c0 in enumerate((h * 128, 512 + h * 128, 1024 + h * 128, 1536 + h * 128)):
                        load_w(wst, lambda c, jj=jj: Wh[:, c, jj * 128:(jj + 1) * 128], Wh, w_in_d, c0, 128, gT["mix"])
                    for j, dstq in enumerate((qTs, kTs)):
                        for tr in range(S // 512):
                            pz = next_pm()
                            for k in range(8):
                                MM(pz[:, :], Wh[:, k, j * 128:(j + 1) * 128], xnT[:, k, tr * 512:(tr + 1) * 512],
                                   k == 0, k == 7, [Wh, xnT], [pz])
                            CP("act", zbuf[:, 3 + tr * 512:3 + (tr + 1) * 512], pz[:, :], [pz], [zbuf])
                        cidx = j * 4 + h
                        TS("dve", ybuf[:], zbuf[:, 0:S], convT[:, cidx, 0:1], ALU.mult, [zbuf, convT], [ybuf])
                        for tap in range(1, 4):
                            STT(ybuf[:], zbuf[:, tap:tap + S], convT[:, cidx, tap:tap + 1], ybuf[:], ALU.mult, ALU.add,
                                [zbuf, convT, ybuf], [ybuf])
                        if b == 0 and h == 0 and j == 0:
                            DBG("zbuf", zbuf[:], [128, S + 3], F32, [zbuf])
                            DBG("ybuf", ybuf[:], [128, S], F32, [ybuf])
                            DBG("convT", convT[:], [128, 8, 4], F32, [convT])
                        ACT(dstq[:], ybuf[:], AF.Silu, [ybuf], [dstq])
                    for c in range(NCH):
                        pv = next_pm()
                        for k in range(8):
                            MM(pv[0:64, 0:256], xnT[:, k, c * 64:(c + 1) * 64], Wh[:, k, 256:512], k == 0, k == 7,
                               [xnT, Wh], [pv])
                        ACT(sig_o[:, c, :], pv[0:64, 128:256], AF.Sigmoid, [pv], [sig_o])
                        TS("dve", vea[:, c, 0:128], pv[0:64, 0:128], ea[:, c, h:h + 1], ALU.mult, [pv, ea, sig_o], [vea])
                    CP("dve", vea[:, :, 128:129], ea[:, :, h:h + 1], [ea], [vea])
                    if b == 0 and h == 0:
                        DBG("qTs", qTs[:], [128, S], BF16, [qTs])
                        DBG("kTs", kTs[:], [128, S], BF16, [kTs])
                        DBG("vea", vea[:], [64, NCH, 129], BF16, [vea])
                        DBG("sig_o", sig_o[:], [64, NCH, 128], BF16, [sig_o])
                    for c0 in range(0, NCH, 8):
                        n8 = min(8, NCH - c0)
                        pt = next_ptr()
                        for cc in range(n8):
                            c = c0 + cc
                            TR(pt[0:64, cc * 128:(cc + 1) * 128], kTs[:, c * 64:(c + 1) * 64], identb[:],
                               [kTs, identb], [pt])
                        CP("act", k_tok[:, c0:c0 + n8, :], pt[0:64, 0:n8 * 128].rearrange("p (c d) -> p c d", d=128), [pt], [k_tok])
                        ps = next_pm()
                        for cc in range(n8):
                            c = c0 + cc
                            MM(ps[0:64, cc * 64:(cc + 1) * 64], kTs[:, c * 64:(c + 1) * 64], qTs[:, c * 64:(c + 1) * 64],
                               True, True, [kTs, qTs], [ps])
                        TT("dve", scm[:, c0:c0 + n8, :], ps[0:64, 0:n8 * 64].rearrange("p (c l) -> p c l", l=64),
                           trif[0:64, None, 0:64].to_broadcast([64, n8, 64]), ALU.mult, [ps, trif], [scm])
                    for c in range(NCH):
                        pa = pm[3]
                        MM(pa[0:64, 0:129], scm[:, c, :], vea[:, c, :], True, c == 0, [scm, vea], [pa])
                        if c > 0:
                            MM(pa[0:64, 0:129], qTs[:, c * 64:(c + 1) * 64], Cb[:, :], False, True, [qTs, Cb], [pa])
                        CP("act", acc_all[:, c, :], pa[0:64, 0:129], [pa], [acc_all])
                        if c < NCH - 1:
                            pu = pm[4]
                            MM(pu[:, 0:129], k_tok[:, c, :], vea[:, c, :], True, True, [k_tok, vea], [pu])
                            if c == 0:
                                TS("dve", C32[:], pu[:, 0:129], eFL[:, c, h:h + 1], ALU.mult, [pu, eFL], [C32])
                            else:
                                TS("dve", C32s[:], C32[:], eFL[:, c, h:h + 1], ALU.mult, [C32, eFL], [C32s])
                                STT(C32[:], pu[:, 0:129], eFL[:, c, h:h + 1], C32s[:], ALU.mult, ALU.add,
                                    [pu, eFL, C32s], [C32])
                            CP("dve", Cb[:], C32[:], [C32], [Cb])
                    if b == 0 and h == 0:
                        DBG("acc_all", acc_all[:], [64, NCH, 129], F32, [acc_all])
                        DBG("scm", scm[:], [64, NCH, 64], BF16, [scm])
                    TT("dve", st_a[:], acc_all[:, :, 128], eb[:, :, h], ALU.mult, [acc_all, eb], [st_a])
                    ACT(st_a[:], st_a[:], AF.Abs, [st_a], [st_a])
                    TS("dve", st_a[:], st_a[:], 1.0, ALU.max, [st_a], [st_a])
                    RECIP(st_a[:], st_a[:], [st_a], [st_a])
                    TT("dve", st_b[:], eb[:, :, h], st_a[:], ALU.mult, [eb, st_a], [st_b])
                    TT("dve", hml[:], acc_all[:, :, 0:128], st_b[:].unsqueeze(2).to_broadcast([64, NCH, 128]), ALU.mult,
                       [acc_all, st_b], [hml])
                    TT("pool", acc_all[:, :, 0:128], hml[:], hml[:], ALU.mult, [hml], [acc_all])
                    P.op("dve", lambda e: e.tensor_reduce(out=st_a[:], in_=acc_all[:, :, 0:128], axis=AX.X, op=ALU.add),
                         rl([acc_all]), rl([st_a]))
                    ACT(st_a[:], st_a[:], AF.Sqrt, [st_a], [st_a], scale=1.0 / 128, bias=EPS)
                    RECIP(st_b[:], st_a[:], [st_a], [st_b])
                    TT("dve", hml[:], hml[:], st_b[:].unsqueeze(2).to_broadcast([64, NCH, 128]), ALU.mult, [hml, st_b], [hml])
                    TT("dve", hml[:], hml[:], mlg_bc[:, None, h * 128:(h + 1) * 128].to_broadcast([64, NCH, 128]), ALU.mult,
                       [hml, mlg_bc], [hml])
                    TT("dve", hmlb[:], hml[:], sig_o[:], ALU.mult, [hml, sig_o], [hmlb])
                    for c0 in range(0, NCH, 16):
                        n16 = min(16, NCH - c0)
                        pt = next_ptr()
                        for cc in range(n16):
                            TR(pt[:, cc * 64:(cc + 1) * 64], hmlb[:, c0 + cc, :], identb[0:64, 0:64], [hmlb, identb], [pt])
                        CP("act", hTs[:, c0 * 64:(c0 + n16) * 64], pt[:, 0:n16 * 64], [pt], [hTs])
                    DMA(hT_d[b, h], hTs[:], [hTs], [], stream=3)

                for h in range(0 if "A" in SKIP else 4):
                    for jj, c0 in enumerate((2056 + h * 128, 2568 + h * 128, 3080 + h * 128)):
                        load_w(wst, lambda c, jj=jj: Wh[:, c, jj * 128:(jj + 1) * 128], Wh, w_in_d, c0, 128, gT["mix"])
                    for j, dstq in enumerate((qTs, kTs)):
                        for tr in range(S // 512):
                            pz = next_pm()
                            for k in range(8):
                                MM(pz[:, :], Wh[:, k, j * 128:(j + 1) * 128], xnT[:, k, tr * 512:(tr + 1) * 512],
                                   k == 0, k == 7, [Wh, xnT], [pz])
                            CP("act", dstq[:, tr * 512:(tr + 1) * 512], pz[:, :], [pz], [dstq])
                    for i in range(NT):
                        pv = next_pm()
                        for k in range(8):
                            MM(pv[:, 0:128], xnT[:, k, i * 128:(i + 1) * 128], Wh[:, k, 256:384], k == 0, k == 7,
                               [xnT, Wh], [pv])
                        CP("dve", vda[:, i, 0:128], pv[:, 0:128], [pv], [vda])
                    P.barrier()
                    pacc = (pm[3], pm[4])
                    psr = [[Res("ps%d" % bk)] * 4 for bk in range(3)]
                    pTr = [[Res("pT%d_%d" % (bk, ii)) for ii in range(4)] for bk in range(2)]
                    groups = []
                    for j in range(NT):
                        for cm in range(2):
                            for i0 in range(0, j + 1, 4):
                                groups.append((j, cm, list(range(i0, min(i0 + 4, j + 1)))))

                    def emit_qk(n):
                        j, cm, iis = groups[n]
                        lo, hi = cm * 64, (cm + 1) * 64
                        bk = n % 3
                        for ii, i in enumerate(iis):
                            MM(pm[bk][:, ii * 128:(ii + 1) * 128], kTs[lo:hi, i * 128:(i + 1) * 128],
                               qTs[lo:hi, j * 128:(j + 1) * 128], True, True, [kTs, qTs], [psr[bk][ii]], nowaw=(ii > 0))

                    def emit_act(n):
                        j, cm, iis = groups[n]
                        bk, tb = n % 3, n % 2
                        for ii, i in enumerate(iis):
                            m = j - i
                            ACT(pT[tb][:, ii, :], pm[bk][:, ii * 128:(ii + 1) * 128], AF.Exp, [psr[bk][ii], alibi],
                                [pTr[tb][ii]], bias=alibi[:, h * 16 + m:h * 16 + m + 1], scale=0.125)
                            if i == j:
                                TT("dve", pT[tb][:, ii, :], pT[tb][:, ii, :], trib[:], ALU.mult, [pTr[tb][ii], trib],
                                   [pTr[tb][ii]])

                    def emit_pv(n):
                        j, cm, iis = groups[n]
                        tb = n % 2
                        for ii, i in enumerate(iis):
                            MM(pacc[cm][:, 0:129], pT[tb][:, ii, :], vda[:, i, :], i == 0, i == j, [pTr[tb][ii], vda],
                               [pacc[cm]])
                        if cm == 1 and iis[-1] == j:
                            RECIP(rz[:, 0:1], pacc[0][:, 128:129], [pacc[0]], [rz])
                            RECIP(rz[:, 1:2], pacc[1][:, 128:129], [pacc[1]], [rz])
                            TT("dve", rz[:, 1:2], rz[:, 1:2], neglam[:], ALU.mult, [rz, neglam], [rz])
                            ACT(oda[:, j, :], pacc[0][:, 0:128], AF.Identity, [pacc[0], rz], [oda], scale=rz[:, 0:1])
                            STT(oda[:, j, :], pacc[1][:, 0:128], rz[:, 1:2], oda[:, j, :], ALU.mult, ALU.add,
                                [pacc[1], rz, oda], [oda])

                    emit_qk(0)
                    for n in range(len(groups)):
                        if n + 1 < len(groups):
                            emit_qk(n + 1)
                        emit_act(n)
                        emit_pv(n)
                    P.barrier()
                    TT("pool", hml_full[:, 0:NT, :], oda[:], oda[:], ALU.mult, [oda], [hml_full])
                    P.op("dve", lambda e: e.tensor_reduce(out=sd_a[:], in_=hml_full[:, 0:NT, :], axis=AX.X, op=ALU.add),
                         rl([hml_full]), rl([sd_a]))
                    ACT(sd_a[:], sd_a[:], AF.Sqrt, [sd_a], [sd_a], scale=1.0 / 128, bias=EPS)
                    RECIP(sd_b[:], sd_a[:], [sd_a], [sd_b])
                    TT("dve", oda[:], oda[:], sd_b[:].unsqueeze(2).to_broadcast([128, NT, 128]), ALU.mult, [oda, sd_b], [oda])
                    STT(odab[:], oda[:], 1.0 - LAM_INIT, dag_bc[:, None, h * 128:(h + 1) * 128].to_broadcast([128, NT, 128]),
                        ALU.mult, ALU.mult, [oda, dag_bc], [odab])
                    for j0 in range(0, NT, 8):
                        nj = min(8, NT - j0)
                        pt = next_ptr()
                        for jj in range(nj):
                            TR(pt[:, jj * 128:(jj + 1) * 128], odab[:, j0 + jj, :], identb[:], [odab, identb], [pt])
                        CP("act", hTs[:, j0 * 128:(j0 + nj) * 128], pt[:, 0:nj * 128], [pt], [hTs])
                    DMA(hT_d[b, 4 + h], hTs[:], [hTs], [], stream=3)
            P.barrier()

        with ExitStack() as ph:
            Wout = mkT(ph, "Wout", [128, 8, 1024], BF16)
            Wcq = mkT(ph, "Wcq", [128, 8, 1024], BF16)
            Wco = mkT(ph, "Wco", [128, 8, 1024], BF16)
            Wpq = mkT(ph, "Wpq", [128, 8, 1024], BF16)
            skT = mkT(ph, "skT", [128, 8, 128], BF16)
            KT = [mkT(ph, "KT%d" % b, [128, 8, MEM], BF16) for b in range(NB)]
            Vb = [mkT(ph, "Vb%d" % b, [128, 2, 4, 257], BF16) for b in range(NB)]
            gffn_bc = mkT(ph, "gffn_bc", [128, 1024], F32)
            gfin_bc = mkT(ph, "gfin_bc", [128, 1024], F32)
            DMA(gffn_bc[:], g_ffn_d.partition_broadcast(128), [], [gffn_bc])
            DMA(gfin_bc[:], g_fin_d.partition_broadcast(128), [], [gfin_bc])
            with ExitStack() as s3:
                wst = mkT(s3, "wst3", [128, 8, 512], F32)
                Wck = mkT(s3, "Wck", [128, 8, 1024], BF16)
                Wcv = mkT(s3, "Wcv", [128, 8, 1024], BF16)
                memT = mkT(s3, "memT", [128, 8, MEM], BF16)
                mt_t = mkT(s3, "mt_t", [128, 1024], F32)
                sk_nat = mkT(s3, "sk_nat", [128, 8, 128], F32)
                for (Wd, wdram, g) in ((Wout, w_out_d, None), (Wcq, w_cq_d, gT["ca"]), (Wco, w_co_d, None),
                                       (Wpq, w_pq_d, gT["ffn"]), (Wck, w_ck_d, gT["mem"]), (Wcv, w_cv_d, gT["mem"])):
                    for half in range(2):
                        load_w(wst, lambda c, Wd=Wd, half=half: Wd[:, c, half * 512:(half + 1) * 512], Wd, wdram,
                               half * 512, 512, g)
                for hh in range(8):
                    for c in range(2):
                        DMA(sk_nat[:, hh, c * 64:(c + 1) * 64], sk_d[hh, c], [], [sk_nat])
                for hh in range(8):
                    TR(ptf[:, 0:128], sk_nat[:, hh, :], identf[:], [sk_nat, identf], [ptf])
                    CP("act", skT[:, hh, :], ptf[:, 0:128], [ptf], [skT])
                cst = [mkT(s3, "cst%d" % i, [128, 2 * D], F32) for i in range(2)]
                cbf = [mkT(s3, "cbf%d" % i, [128, 2 * D], BF16) for i in range(2)]
                for rt in range(NEXP // 128):
                    cs_, cb_ = cst[rt % 2], cbf[rt % 2]
                    DMA(cs_[:, 0:D], pu_d[rt * 128:(rt + 1) * 128, :], [], [cs_])
                    DMA(cs_[:, D:2 * D], pv_d[rt * 128:(rt + 1) * 128, :], [], [cs_])
                    if rt % 2 == 0:
                        CP("dve", cb_[:], cs_[:], [cs_], [cb_])
                    else:
                        CP("act", cb_[:], cs_[:], [cs_], [cb_])
                    DMA(uvb_d[rt * 128:(rt + 1) * 128, :], cb_[:], [cb_], [])
                for b in range(NB):
                    MEMSET("dve", Vb[b][:, :, :, 256:257], 1.0, [Vb[b]])
                    for mt in range(2):
                        DMA(mt_t[:], mem_d[b, mt * 128:(mt + 1) * 128, :], [], [mt_t], stream=2)
                        norm_T(mt_t, memT[:, :, mt * 128:(mt + 1) * 128], memT)
                    for dch in range(8):
                        pk = next_pm()
                        for k in range(8):
                            MM(pk[:, 0:MEM], Wck[:, k, dch * 128:(dch + 1) * 128], memT[:, k, :], k == 0, k == 7,
                               [Wck, memT], [pk])
                        CP("act", KT[b][:, dch, :], pk[:, 0:MEM], [pk], [KT[b]])
                    for mt in range(2):
                        for half in range(2):
                            pk = next_pm()
                            for k in range(8):
                                MM(pk[:, :], memT[:, k, mt * 128:(mt + 1) * 128], Wcv[:, k, half * 512:(half + 1) * 512],
                                   k == 0, k == 7, [memT, Wcv], [pk])
                            CP("act", Vb[b][:, mt, half * 2:(half + 1) * 2, 0:256],
                               pk[:, :].rearrange("p (h d) -> p h d", d=256), [pk], [Vb[b]])
                P.barrier()

            xt = mkT(ph, "xt3", [128, 1024], F32)
            hTt = mkT(ph, "hTt", [128, 8, 128], BF16)
            x1 = mkT(ph, "x1", [128, 1024], F32)
            x1T = mkT(ph, "x1T", [128, 8, 128], BF16)
            QT = mkT(ph, "QT", [128, 8, 128], BF16)
            pTc = mkT(ph, "pTc", [128, 2, 128], BF16)
            oca = mkT(ph, "oca", [128, 1024], BF16)
            ocaT = mkT(ph, "ocaT", [128, 8, 128], BF16)
            x2 = mkT(ph, "x2", [128, 1024], F32)
            xn2 = mkT(ph, "xn2", [128, 1024], BF16)
            x2T = mkT(ph, "x2T", [128, 8, 128], BF16)
            qpT = mkT(ph, "qpT", [128, 8, 128], BF16)
            sc = mkT(ph, "sc", [128, 16, 128], F32)
            work = mkT(ph, "work", [128, 256], F32)
            tv = mkT(ph, "tv", [128, 16, 16], F32)
            tiu = mkT(ph, "tiu", [128, 16, 16], U32)
            tif = mkT(ph, "tif", [128, 16, 16], F32)
            cand = mkT(ph, "cand", [128, 8, 16, 16], F32)
            bv = mkT(ph, "bv", [128, 8, 16], F32)
            bju = mkT(ph, "bju", [128, 8, 16], U32)
            k1u = mkT(ph, "k1u", [128, 8, 16], U32)
            k2u = mkT(ph, "k2u", [128, 8, 16], U32)
            k1f = mkT(ph, "k1f", [128, 8, 16], F32)
            k2f = mkT(ph, "k2f", [128, 8, 16], F32)
            oh = cand
            i1f = mkT(ph, "i1f", [128, 8, 16], F32)
            i2f = mkT(ph, "i2f", [128, 8, 16], F32)
            eidx = mkT(ph, "eidx", [128, 128], I32)
            gate = mkT(ph, "gate", [128, 8, 16], F32)
            gs = mkT(ph, "gs", [128, 8], F32)
            actv = mkT(ph, "actv", [128, 128], F32)
            coef = mkT(ph, "coef", [128, 128], F32)
            pacc_sb = View(sc[:, 0:8, :].rearrange("p a b -> p (a b)"), sc.r)
            junk = View(sc[:, 8:16, :].rearrange("p a b -> p (a b)"), sc.r)
            rzc = mkT(ph, "rzc", [128, 1], F32)
            NGB = 12
            gbuf = [mkT(ph, "gbuf%d" % i, [128, 2 * D], BF16) for i in range(NGB)]
            Dg = [mkT(ph, "Dg%d" % i, [128, 4, 128], BF16) for i in range(2)]
            junkbs = [mkT(ph, "junkb%d" % i, [128, 1024], BF16) for i in range(2)]
            actg = [mkT(ph, "actg%d" % i, [128, 4], F32) for i in range(2)]
            coefg = [mkT(ph, "coefg%d" % i, [128, 4], F32) for i in range(2)]
            gbi = [0]

            for b in range(NB):
                for ti in range(0 if "3" in SKIP else NT):
                    tsl = slice(ti * 128, (ti + 1) * 128)
                    DMA(xt[:], x_d[b, tsl, :], [], [xt], stream=2)
                    DMA(hTt[:], hT_d[b, :, :, tsl].rearrange("c p t -> p c t"), [], [hTt], stream=2)
                    for half in range(2):
                        po = next_pm()
                        for c in range(8):
                            MM(po[:, :], hTt[:, c, :], Wout[:, c, half * 512:(half + 1) * 512], c == 0, c == 7,
                               [hTt, Wout], [po])
                        TT("dve", x1[:, half * 512:(half + 1) * 512], po[:, :], xt[:, half * 512:(half + 1) * 512], ALU.add,
                           [po, xt], [x1])
                    norm_T(x1, x1T[:], x1T)
                    for d0 in range(0, 8, 4):
                        pq = next_pm()
                        for dd in range(4):
                            dch = d0 + dd
                            for k in range(8):
                                MM(pq[:, dd * 128:(dd + 1) * 128], Wcq[:, k, dch * 128:(dch + 1) * 128], x1T[:, k, :],
                                   k == 0, k == 7, [Wcq, x1T], [pq])
                        CP("act", QT[:, d0:d0 + 4, :], pq[:, :].rearrange("p (c t) -> p c t", t=128), [pq], [QT])
                    for h in range(4):
                        ps = next_pm()
                        for mt in range(2):
                            for dd in range(2):
                                MM(ps[:, mt * 128:(mt + 1) * 128], KT[b][:, 2 * h + dd, mt * 128:(mt + 1) * 128],
                                   QT[:, 2 * h + dd, :], dd == 0, dd == 1, [KT[b], QT], [ps])
                        ACT(pTc[:], ps[:, 0:256].rearrange("p (m q) -> p m q", q=128), AF.Exp, [ps], [pTc], scale=1.0 / 16)
                        pa = pm[3]
                        for mt in range(2):
                            MM(pa[:, 0:257], pTc[:, mt, :], Vb[b][:, mt, h, :], mt == 0, mt == 1, [pTc, Vb[b]], [pa])
                        RECIP(rzc[:], pa[:, 256:257], [pa], [rzc])
                        ACT(oca[:, h * 256:(h + 1) * 256], pa[:, 0:256], AF.Identity, [pa, rzc], [oca], scale=rzc[:, 0:1])
                    pt = next_ptr()
                    for c in range(8):
                        TR(pt[:, c * 128:(c + 1) * 128], oca[:, c * 128:(c + 1) * 128], identb[:], [oca, identb], [pt])
                    CP("act", ocaT[:], pt[:].rearrange("p (c t) -> p c t", t=128), [pt], [ocaT])
                    for half in range(2):
                        po = next_pm()
                        for c in range(8):
                            MM(po[:, :], ocaT[:, c, :], Wco[:, c, half * 512:(half + 1) * 512], c == 0, c == 7,
                               [ocaT, Wco], [po])
                        TT("dve", x2[:, half * 512:(half + 1) * 512], po[:, :], x1[:, half * 512:(half + 1) * 512], ALU.add,
                           [po, x1], [x2])
                    if stage == 0:
                        DMA(dbg1_d[b, tsl, :], x1[:], [x1], [], stream=3)
                        DMA(dbg2_d[b, tsl, :], x2[:], [x2], [], stream=3)
                    norm_T(x2, x2T[:], x2T, gbc=gffn_bc, keep_f32=xn2)
                    for d0 in range(0, 8, 4):
                        pq = next_pm()
                        for dd in range(4):
                            dch = d0 + dd
                            for k in range(8):
                                MM(pq[:, dd * 128:(dd + 1) * 128], Wpq[:, k, dch * 128:(dch + 1) * 128], x2T[:, k, :],
                                   k == 0, k == 7, [Wpq, x2T], [pq])
                        CP("act", qpT[:, d0:d0 + 4, :], pq[:, :].rearrange("p (c t) -> p c t", t=128), [pq], [qpT])
                    for s0 in range(0, 16, 4):
                        psc = next_pm()
                        for s_ in range(4):
                            st_ = s0 + s_
                            hp, c = st_ // 2, st_ % 2
                            MM(psc[:, s_ * 128:(s_ + 1) * 128], qpT[c * 64:(c + 1) * 64, hp, :], skT[c * 64:(c + 1) * 64, hp, :],
                               True, True, [qpT, skT], [psc])
                        CP("act", sc[:, s0:s0 + 4, :], psc[:, :].rearrange("p (s n) -> p s n", n=128), [psc], [sc])
                    for st_ in range(16):
                        P.op("dve", lambda e, st_=st_: e.max(out=tv[:, st_, 0:8], in_=sc[:, st_, :]), rl([sc]), rl([tv]))
                        P.op("dve", lambda e, st_=st_: e.max_index(out=tiu[:, st_, 0:8], in_max=tv[:, st_, 0:8],
                                                                  in_values=sc[:, st_, :]), rl([sc, tv]), rl([tiu]))
                        P.op("dve", lambda e, st_=st_: e.match_replace(out=work[:, 0:128], in_to_replace=tv[:, st_, 0:8],
                                                                      in_values=sc[:, st_, :], imm_value=-1e30),
                             rl([sc, tv]), rl([work]))
                        P.op("dve", lambda e, st_=st_: e.max(out=tv[:, st_, 8:16], in_=work[:, 0:128]), rl([work]), rl([tv]))
                        P.op("dve", lambda e, st_=st_: e.max_index(out=tiu[:, st_, 8:16], in_max=tv[:, st_, 8:16],
                                                                  in_values=work[:, 0:128]), rl([work, tv]), rl([tiu]))
                    CP("dve", tif[:], tiu[:], [tiu], [tif])
                    tv4 = tv[:].rearrange("p (h c) k -> p h c k", c=2)
                    tif4 = tif[:].rearrange("p (h c) k -> p h c k", c=2)
                    TT("dve", cand[:], tv4[:, :, 0, :].unsqueeze(3).to_broadcast([128, 8, 16, 16]),
                       tv4[:, :, 1, :].unsqueeze(2).to_broadcast([128, 8, 16, 16]), ALU.add, [tv], [cand])
                    for hp in range(8):
                        cv = cand[:, hp, :, :].rearrange("p a b -> p (a b)")
                        P.op("dve", lambda e, hp=hp, cv=cv: e.max(out=bv[:, hp, 0:8], in_=cv), rl([cand]), rl([bv]))
                        P.op("dve", lambda e, hp=hp, cv=cv: e.max_index(out=bju[:, hp, 0:8], in_max=bv[:, hp, 0:8],
                                                                       in_values=cv), rl([cand, bv]), rl([bju]))
                        P.op("dve", lambda e, hp=hp, cv=cv: e.match_replace(out=work[:], in_to_replace=bv[:, hp, 0:8],
                                                                           in_values=cv, imm_value=-1e30),
                             rl([cand, bv]), rl([work]))
                        P.op("dve", lambda e, hp=hp: e.max(out=bv[:, hp, 8:16], in_=work[:]), rl([work]), rl([bv]))
                        P.op("dve", lambda e, hp=hp: e.max_index(out=bju[:, hp, 8:16], in_max=bv[:, hp, 8:16],
                                                                in_values=work[:]), rl([work, bv]), rl([bju]))
                    P.op("dve", lambda e: e.tensor_single_scalar(out=k1u[:], in_=bju[:], scalar=4,
                                                                 op=ALU.logical_shift_right), rl([bju]), rl([k1u]))
                    P.op("dve", lambda e: e.tensor_single_scalar(out=k2u[:], in_=bju[:], scalar=15,
                                                                 op=ALU.bitwise_and), rl([bju]), rl([k2u]))
                    CP("dve", k1f[:], k1u[:], [k1u], [k1f])
                    CP("dve", k2f[:], k2u[:], [k2u], [k2f])
                    for (kf, cidx, dst) in ((k1f, 0, i1f), (k2f, 1, i2f)):
                        TT("dve", oh[:], kf[:].unsqueeze(3).to_broadcast([128, 8, 16, 16]),
                           iota16[:, None, None, :].to_broadcast([128, 8, 16, 16]), ALU.is_equal, [kf, iota16], [oh])
                        TT("dve", oh[:], oh[:], tif4[:, :, cidx, :].unsqueeze(2).to_broadcast([128, 8, 16, 16]), ALU.mult,
                           [oh, tif], [oh])
                        P.op("dve", lambda e, dst=dst: e.tensor_reduce(out=dst[:], in_=oh[:], axis=AX.X, op=ALU.add),
                             rl([oh]), rl([dst]))
                    STT(i1f[:], i1f[:], 128.0, i2f[:], ALU.mult, ALU.add, [i1f, i2f], [i1f])
                    TS("dve", i1f[:], i1f[:], 0.0, ALU.max, [i1f], [i1f], s2=float(NEXP - 1), op1=ALU.min)
                    CP("dve", eidx[:], i1f[:].rearrange("p h k -> p (h k)"), [i1f], [eidx])
                    TT("dve", gate[:], bv[:], bv[:, :, 0:1].to_broadcast([128, 8, 16]), ALU.subtract, [bv], [gate])
                    ACT(gate[:], gate[:], AF.Exp, [gate], [gate])
                    P.op("dve", lambda e: e.tensor_reduce(out=gs[:], in_=gate[:], axis=AX.X, op=ALU.add), rl([gate]), rl([gs]))
                    RECIP(gs[:], gs[:], [gs], [gs])
                    TT("dve", gate[:], gate[:], gs[:].unsqueeze(2).to_broadcast([128, 8, 16]), ALU.mult, [gate, gs], [gate])
                    gate2 = gate[:].rearrange("p h k -> p (h k)")
                    pxa = (pm[3], pm[4])
                    for g4 in range(0 if "G" in SKIP else 32):
                        ag, cg, dg = actg[g4 % 2], coefg[g4 % 2], Dg[g4 % 2]
                        gbs = []
                        for s_ in range(4):
                            sl = g4 * 4 + s_
                            gb = gbuf[gbi[0]]
                            gbi[0] = (gbi[0] + 1) % NGB
                            gbs.append(gb)
                            P.dma("pool", 0, lambda e, gb=gb, sl=sl: e.indirect_dma_start(
                                out=gb[:], out_offset=None, in_=uvb_d,
                                in_offset=bass.IndirectOffsetOnAxis(ap=eidx[:, sl:sl + 1], axis=0)), rl([eidx]), rl([gb]))
                        for s_ in range(4):
                            gb = gbs[s_]
                            junkb = junkbs[s_ % 2]
                            P.op("dve", lambda e, gb=gb, s_=s_, ag=ag, junkb=junkb: e.scalar_tensor_tensor(
                                out=junkb[:], in0=gb[:, 0:D], scalar=1.0, in1=xn2[:], op0=ALU.mult, op1=ALU.mult,
                                accum_out=ag[:, s_:s_ + 1]), rl([gb, xn2]), rl([junkb, ag]))
                        ACT(cg[:], ag[:], AF.Gelu, [ag], [cg])
                        TT("dve", cg[:], cg[:], gate2[:, g4 * 4:(g4 + 1) * 4], ALU.mult, [cg, gate], [cg])
                        TT("dve", dg[:], identb[:, None, :].to_broadcast([128, 4, 128]),
                           cg[:].unsqueeze(2).to_broadcast([128, 4, 128]), ALU.mult, [identb, cg], [dg])
                        for s_ in range(4):
                            sl = g4 * 4 + s_
                            for half in range(2):
                                MM(pxa[half][:, :], dg[:, s_, :], gbs[s_][:, D + half * 512:D + (half + 1) * 512],
                                   sl == 0, sl == 127, [dg, gbs[s_]], [pxa[half]])
                    for half in range(2):
                        TT("dve", pacc_sb[:, half * 512:(half + 1) * 512], pxa[half][:, :], x2[:, half * 512:(half + 1) * 512],
                           ALU.add, [pxa[half], x2], [pacc_sb])
                    ACT(sqj[:], pacc_sb[:], AF.Square, [pacc_sb], [sqj, ss], accum=ss[:])
                    ACT(rstd[:], ss[:], AF.Sqrt, [ss], [rstd], scale=1.0 / D, bias=EPS)
                    RECIP(rstd[:], rstd[:], [rstd], [rstd])
                    STT(junk[:], pacc_sb[:], rstd[:, 0:1], gfin_bc[:], ALU.mult, ALU.mult, [pacc_sb, rstd, gfin_bc], [junk])
                    out_res = Res("out")
                    DMA(out_d[b, tsl, :], junk[:], [junk], [out_res], stream=3)
            P.barrier()

        with nc.Block() as block:
            P.emit(block)
    return nc, P


def make_consts():
    ident = np.eye(128, dtype=np.float32)
    tri = np.triu(np.ones((128, 128), np.float32))
    al = np.zeros((128, 64), np.float32)
    k = np.arange(128, dtype=np.float32)
    for h in range(4):
        for m in range(16):
            al[:, h * 16 + m] = ALIBI_SLOPES[h] * (k - 128.0 * m)
    iota16 = np.tile(np.arange(16, dtype=np.float32)[None, :], (128, 1))
    return {"c_ident": ident, "c_tri": tri, "c_alibi": al, "c_iota16": iota16}


_CACHE = {}


def kernel(**inputs):
    NC = 8
    x = np.asarray(inputs["x"], np.float32)
    B, S, _ = x.shape
    NB = B // NC
    key = (S, NB)
    if key not in _CACHE:
        _CACHE[key] = build_nc(S, NB)[0]
    nc = _CACHE[key]
    shared = {}
    for k_, v in inputs.items():
        if k_ in ("x", "mem"):
            continue
        a = np.ascontiguousarray(np.asarray(v, np.float32))
        if k_ != "final_norm_g":
            a = a[0]
        shared[k_] = np.ascontiguousarray(a)
    shared.update(make_consts())
    mem = np.asarray(inputs["mem"], np.float32)
    in_maps = []
    for c in range(NC):
        m = dict(shared)
        m["x"] = np.ascontiguousarray(x[c * NB:(c + 1) * NB])
        m["mem"] = np.ascontiguousarray(mem[c * NB:(c + 1) * NB])
        in_maps.append(m)
    res = run_bass_kernel_spmd(nc, in_maps, core_ids=list(range(NC)))
    out = np.concatenate([np.asarray(r["out"]) for r in res.results], axis=0)
    return out.astype(np.float32)
```
